# Optimizing a Trainium2 kernel written in Bass

```python
import jax
import jax.numpy as jnp
from jax import lax
import numpy as np


D_MODEL = 1024
BATCH = 16
SEQ = 4096
DEPTH = 4

CTX_LEN = 256
GRID_W = 64
POOL_WINDOWS = (2, 4, 8, 16)
POOL_GROUP = D_MODEL // 8
POOL_WIDTH = POOL_GROUP * len(POOL_WINDOWS)
RET_HEADS = 4
RET_QK_DIM = D_MODEL // 8
RET_V_DIM = 2 * RET_QK_DIM
RET_QK_WIDTH = RET_HEADS * RET_QK_DIM
RET_V_WIDTH = RET_HEADS * RET_V_DIM
RET_CHUNK = 128
ROPE_BASE = 10000.0
OFF_Q = POOL_WIDTH
OFF_K = OFF_Q + RET_QK_WIDTH
OFF_V = OFF_K + RET_QK_WIDTH
OFF_G = OFF_V + RET_V_WIDTH
OFF_GATE_POOL = OFF_G + RET_V_WIDTH
OFF_GATE_RET = OFF_GATE_POOL + D_MODEL
IN_WIDTH = OFF_GATE_RET + D_MODEL
D_FF = 2816
N_EXPERTS = 8
TOP_K = 2
EXPERT_FF = 3584
MOE_BLOCK = 256
N_DENSE = (DEPTH + 1) // 2
N_MOE = DEPTH // 2
DEEPNORM_ALPHA = (2 * DEPTH) ** 0.25
DEEPNORM_BETA = (8 * DEPTH) ** -0.25
LN_EPS = 1e-5

kernel_name = 'hybrid_pool_retention_moe_dit'


def _layer_norm(x, g, b):
    xf = x.astype(jnp.float32)
    xc = xf - jnp.mean(xf, -1, keepdims=True)
    var = jnp.mean(xc * xc, -1, keepdims=True)
    return (xc * lax.rsqrt(var + LN_EPS) * g + b).astype(x.dtype)


def _box_mean(u, axis, w):
    n = u.shape[axis]
    left = w // 2
    right = w - 1 - left
    cs = jnp.cumsum(u.astype(jnp.float32), axis=axis)
    pad = [(0, 0)] * u.ndim
    pad[axis] = (1, 0)
    cs = jnp.pad(cs, pad)
    t = jnp.arange(n)
    hi = jnp.minimum(t + right + 1, n)
    lo = jnp.maximum(t - left, 0)
    shape = [1] * u.ndim
    shape[axis] = n
    cnt = (hi - lo).astype(jnp.float32).reshape(shape)
    return ((jnp.take(cs, hi, axis=axis) - jnp.take(cs, lo, axis=axis)) / cnt).astype(u.dtype)


def _pool_mixer(u, rows, w_group, scale):
    B, T, _ = u.shape
    ug = u.reshape(B, T, len(POOL_WINDOWS), POOL_GROUP)
    pooled = []
    for gi, w in enumerate(POOL_WINDOWS):
        g = ug[:, :, gi]
        if rows is None:
            p = _box_mean(g, 1, w)
        else:
            gg = g.reshape(B, rows, GRID_W, POOL_GROUP)
            p = _box_mean(_box_mean(gg, 1, w), 2, w).reshape(B, T, POOL_GROUP)
        pooled.append(p)
    d = jnp.stack(pooled, axis=2) - ug
    y = jnp.einsum('btgc,gcd->btgd', d, w_group).reshape(B, T, POOL_WIDTH)
    return y * scale


def _grid_rope(n_tokens):
    t = jnp.arange(n_tokens)
    row = (t // GRID_W).astype(jnp.float32)
    col = (t % GRID_W).astype(jnp.float32)
    n_freq = RET_QK_DIM // 4
    inv = jnp.exp(-jnp.log(ROPE_BASE) * jnp.arange(n_freq, dtype=jnp.float32) / n_freq)
    ang = jnp.concatenate([row[:, None] * inv, col[:, None] * inv], -1)
    return jnp.cos(ang), jnp.sin(ang)


def _rotate(t, cos, sin):
    half = t.shape[-1] // 2
    t1, t2 = t[..., :half], t[..., half:]
    c = cos[None, :, None, :]
    s = sin[None, :, None, :]
    return jnp.concatenate([t1 * c - t2 * s, t1 * s + t2 * c], -1).astype(t.dtype)


def _heads(t, d):
    B, T, _ = t.shape
    return t.reshape(B, T, RET_HEADS, d)


def _seq_flip(t, rev):
    return jnp.flip(t, axis=2) if rev else t


def _chunk_retention(q, k, v, log_gamma, state0):
    B, H, T, _ = q.shape
    dv = v.shape[-1]
    C = RET_CHUNK
    n = T // C
    f32 = jnp.float32
    pos = jnp.arange(C, dtype=f32)
    lg = log_gamma[:, None]
    diff = pos[:, None] - pos[None, :]
    inner = jnp.where(diff >= 0, jnp.exp(lg[:, :, None] * jnp.maximum(diff, 0.0)), 0.0)
    q_dec = jnp.exp(lg * (pos + 1.0))[:, :, None]
    k_dec = jnp.exp(lg * (C - 1.0 - pos))[:, :, None]
    c_dec = jnp.exp(log_gamma * C)[:, None, None]

    def chunks(t):
        return t.astype(f32).reshape(B, H, n, C, t.shape[-1]).transpose(2, 0, 1, 3, 4)

    def step(s, blk):
        qb, kb, vb = blk
        scores = jnp.einsum('bhid,bhjd->bhij', qb, kb) * inner
        o = jnp.einsum('bhij,bhjv->bhiv', scores, vb) + jnp.einsum('bhid,bhdv->bhiv', qb * q_dec, s)
        s = s * c_dec + jnp.einsum('bhjd,bhjv->bhdv', kb * k_dec, vb)
        return s, o

    s, o = lax.scan(step, state0, (chunks(q), chunks(k), chunks(v)))
    return o.transpose(1, 2, 0, 3, 4).reshape(B, H, T, dv), s


def _final_state(k, v, log_gamma):
    T = k.shape[2]
    w = jnp.exp(log_gamma[:, None] * (T - 1.0 - jnp.arange(T, dtype=jnp.float32)))
    return jnp.einsum('bhtd,bhtv->bhdv', k.astype(jnp.float32) * w[None, :, :, None], v.astype(jnp.float32))


def _head_norm(o, dtype):
    oc = o - jnp.mean(o, -1, keepdims=True)
    var = jnp.mean(oc * oc, -1, keepdims=True)
    on = oc * lax.rsqrt(var + LN_EPS)
    B, H, T, dv = o.shape
    return on.transpose(0, 2, 1, 3).reshape(B, T, H * dv).astype(dtype)


def _token_mixer(h, hc, cos, sin, rows, w_in, pool_w, pool_scale, w_pool_out,
                 w_ret_out, decay_logit, w_out, need_ctx):
    B = h.shape[0]
    log_gamma = jax.nn.log_sigmoid(decay_logit.astype(jnp.float32))
    k_scale = RET_QK_DIM ** -0.5

    p = h @ w_in
    q = _rotate(_heads(p[..., OFF_Q:OFF_K], RET_QK_DIM), cos, sin).transpose(0, 2, 1, 3)
    k = (_rotate(_heads(p[..., OFF_K:OFF_V], RET_QK_DIM), cos, sin) * k_scale).transpose(0, 2, 1, 3)
    v = _heads(p[..., OFF_V:OFF_G], RET_V_DIM).transpose(0, 2, 1, 3)
    if need_ctx:
        pc = hc @ w_in
        qc = _heads(pc[..., OFF_Q:OFF_K], RET_QK_DIM).transpose(0, 2, 1, 3)
        kc_src = pc[..., OFF_K:OFF_V]
        vc_src = pc[..., OFF_V:OFF_G]
    else:
        pc_kv = hc @ w_in[:, OFF_K:OFF_G]
        kc_src = pc_kv[..., :RET_QK_WIDTH]
        vc_src = pc_kv[..., RET_QK_WIDTH:]
    kc = (_heads(kc_src, RET_QK_DIM) * k_scale).transpose(0, 2, 1, 3)
    vc = _heads(vc_src, RET_V_DIM).transpose(0, 2, 1, 3)

    zero_state = jnp.zeros((B, RET_HEADS, RET_QK_DIM, RET_V_DIM), jnp.float32)
    o_lat = 0.0
    o_ctx = 0.0
    for d in range(2):
        rev = d == 1
        if need_ctx:
            oc, s = _chunk_retention(_seq_flip(qc, rev), _seq_flip(kc, rev), _seq_flip(vc, rev),
                                     log_gamma[d], zero_state)
            o_ctx = o_ctx + _seq_flip(oc, rev)
        else:
            s = _final_state(_seq_flip(kc, rev), _seq_flip(vc, rev), log_gamma[d])
        ol, _ = _chunk_retention(_seq_flip(q, rev), _seq_flip(k, rev), _seq_flip(v, rev), log_gamma[d], s)
        o_lat = o_lat + _seq_flip(ol, rev)

    def merge(pz, o, grid_rows):
        y_pool = _pool_mixer(pz[..., :OFF_Q], grid_rows, pool_w, pool_scale) @ w_pool_out
        y_ret = (jax.nn.silu(pz[..., OFF_G:OFF_GATE_POOL]) * _head_norm(o, pz.dtype)) @ w_ret_out
        mix = (jax.nn.sigmoid(pz[..., OFF_GATE_POOL:OFF_GATE_RET]) * y_pool
               + jax.nn.sigmoid(pz[..., OFF_GATE_RET:]) * y_ret)
        return mix @ w_out

    y = merge(p, o_lat, rows)
    yc = merge(pc, o_ctx, None) if need_ctx else None
    return y, yc


def _swiglu(t, w1, w3, w2):
    return (jax.nn.silu(t @ w1) * (t @ w3)) @ w2


def _moe_swiglu(t, w_router, w1, w3, w2):
    N, D = t.shape
    logits = (t @ w_router).astype(jnp.float32)
    top_logit, top_idx = lax.top_k(logits, TOP_K)
    top_w = jax.nn.softmax(top_logit, axis=-1)
    n_assign = N * TOP_K
    flat_e = top_idx.reshape(-1).astype(jnp.int32)
    flat_tok = jnp.arange(n_assign, dtype=jnp.int32) // TOP_K
    flat_w = top_w.reshape(-1)
    order = jnp.argsort(flat_e, stable=True)
    se = flat_e[order]
    counts = jax.ops.segment_sum(jnp.ones_like(flat_e), flat_e, num_segments=N_EXPERTS)
    padded = (counts + MOE_BLOCK - 1) // MOE_BLOCK * MOE_BLOCK
    pad_end = jnp.cumsum(padded)
    pad_start = pad_end - padded
    start = jnp.cumsum(counts) - counts
    dest = pad_start[se] + (jnp.arange(n_assign, dtype=jnp.int32) - start[se])
    n_blocks = (n_assign + MOE_BLOCK - 1) // MOE_BLOCK + N_EXPERTS
    n_rows = n_blocks * MOE_BLOCK
    row_tok = jnp.full((n_rows,), N, jnp.int32).at[dest].set(flat_tok[order])
    row_w = jnp.zeros((n_rows,), jnp.float32).at[dest].set(flat_w[order])
    block_start = jnp.arange(n_blocks, dtype=jnp.int32) * MOE_BLOCK
    block_e = jnp.minimum(jnp.searchsorted(pad_end, block_start, side='right'), N_EXPERTS - 1)
    t_pad = jnp.concatenate([t, jnp.zeros((1, D), t.dtype)], 0)

    def expert_block(args):
        tok, e = args
        xb = t_pad[tok]
        return (jax.nn.silu(xb @ w1[e]) * (xb @ w3[e])) @ w2[e]

    y = lax.map(expert_block, (row_tok.reshape(n_blocks, MOE_BLOCK), block_e))
    y = y.reshape(n_rows, D) * row_w[:, None].astype(y.dtype)
    return jax.ops.segment_sum(y, row_tok, num_segments=N + 1)[:N]


def setup_inputs(seed: int = 0) -> dict:
    key = jax.random.key(seed)
    ks = jax.random.split(key, 24)
    f32 = jnp.float32
    D = D_MODEL

    def nrm(k, shape, s):
        return jax.random.normal(k, shape, f32) * s

    base_logit = jnp.log(2.0 ** (5.0 + jnp.arange(RET_HEADS, dtype=f32)) - 1.0)
    return {
        'x': nrm(ks[0], (BATCH, SEQ, D), 1.0),
        'c': nrm(ks[1], (BATCH, D), 1.0),
        'ctx': nrm(ks[2], (BATCH, CTX_LEN, D), 1.0),
        'c_ctx': nrm(ks[3], (D,), 1.0),
        'ada_w': nrm(ks[4], (DEPTH, D, 6 * D), 0.5 * D ** -0.5),
        'ada_b': nrm(ks[5], (DEPTH, 6 * D), 0.01),
        'w_in': nrm(ks[6], (DEPTH, D, IN_WIDTH), D ** -0.5),
        'pool_w': nrm(ks[7], (DEPTH, len(POOL_WINDOWS), POOL_GROUP, POOL_GROUP), POOL_GROUP ** -0.5),
        'pool_scale': 1.0 + nrm(ks[8], (DEPTH, POOL_WIDTH), 0.02),
        'w_pool_out': nrm(ks[9], (DEPTH, POOL_WIDTH, D), POOL_WIDTH ** -0.5),
        'w_ret_out': nrm(ks[10], (DEPTH, RET_V_WIDTH, D), RET_V_WIDTH ** -0.5),
        'ret_decay_logit': base_logit + nrm(ks[11], (DEPTH, 2, RET_HEADS), 0.05),
        'w_out': nrm(ks[12], (DEPTH, D, D), DEEPNORM_BETA * D ** -0.5),
        'ln_mix_g': 1.0 + nrm(ks[13], (DEPTH, D), 0.02),
        'ln_mix_b': nrm(ks[14], (DEPTH, D), 0.01),
        'ln_ffn_g': 1.0 + nrm(ks[15], (DEPTH, D), 0.02),
        'ln_ffn_b': nrm(ks[16], (DEPTH, D), 0.01),
        'ffn_w1': nrm(ks[17], (N_DENSE, D, D_FF), D ** -0.5),
        'ffn_w3': nrm(ks[18], (N_DENSE, D, D_FF), D ** -0.5),
        'ffn_w2': nrm(ks[19], (N_DENSE, D_FF, D), DEEPNORM_BETA * D_FF ** -0.5),
        'moe_router': nrm(ks[20], (N_MOE, D, N_EXPERTS), D ** -0.5),
        'moe_w1': nrm(ks[21], (N_MOE, N_EXPERTS, D, EXPERT_FF), D ** -0.5),
        'moe_w3': nrm(ks[22], (N_MOE, N_EXPERTS, D, EXPERT_FF), D ** -0.5),
        'moe_w2': nrm(ks[23], (N_MOE, N_EXPERTS, EXPERT_FF, D), DEEPNORM_BETA * EXPERT_FF ** -0.5),
    }


def reference(x, c, ctx, c_ctx, ada_w, ada_b, w_in, pool_w, pool_scale, w_pool_out, w_ret_out,
              ret_decay_logit, w_out, ln_mix_g, ln_mix_b, ln_ffn_g, ln_ffn_b, ffn_w1, ffn_w3, ffn_w2,
              moe_router, moe_w1, moe_w3, moe_w2):
    B, L, D = x.shape
    rows = L // GRID_W
    n_ctx = ctx.shape[1]
    n_lat = B * L
    cos, sin = _grid_rope(L)
    s_lat = jax.nn.silu(c)
    s_ctx = jax.nn.silu(c_ctx)
    for l in range(DEPTH):
        need_ctx = l < DEPTH - 1
        mod = jnp.split((s_lat @ ada_w[l] + ada_b[l])[:, None, :], 6, axis=-1)
        mod_c = jnp.split(s_ctx @ ada_w[l] + ada_b[l], 6, axis=-1)

        h = x * (1.0 + mod[1]) + mod[0]
        hc = ctx * (1.0 + mod_c[1]) + mod_c[0]
        y, yc = _token_mixer(h, hc, cos, sin, rows, w_in[l], pool_w[l], pool_scale[l], w_pool_out[l],
                             w_ret_out[l], ret_decay_logit[l], w_out[l], need_ctx)
        x = _layer_norm(DEEPNORM_ALPHA * x + mod[2] * y, ln_mix_g[l], ln_mix_b[l])
        h = x * (1.0 + mod[4]) + mod[3]
        if need_ctx:
            ctx = _layer_norm(DEEPNORM_ALPHA * ctx + mod_c[2] * yc, ln_mix_g[l], ln_mix_b[l])
            hc = ctx * (1.0 + mod_c[4]) + mod_c[3]
            tokens = jnp.concatenate([h.reshape(-1, D), hc.reshape(-1, D)], 0)
        else:
            tokens = h.reshape(-1, D)

        if l % 2 == 0:
            f = _swiglu(tokens, ffn_w1[l // 2], ffn_w3[l // 2], ffn_w2[l // 2])
        else:
            f = _moe_swiglu(tokens, moe_router[l // 2], moe_w1[l // 2], moe_w3[l // 2], moe_w2[l // 2])
        x = _layer_norm(DEEPNORM_ALPHA * x + mod[5] * f[:n_lat].reshape(B, L, D), ln_ffn_g[l], ln_ffn_b[l])
        if need_ctx:
            ctx = _layer_norm(DEEPNORM_ALPHA * ctx + mod_c[5] * f[n_lat:].reshape(B, n_ctx, D),
                              ln_ffn_g[l], ln_ffn_b[l])
    return x
```

```python
import numpy as np
import ml_dtypes
from contextlib import ExitStack
import concourse.bass as bass
import concourse.mybir as mybir
from concourse.bass_utils import run_bass_kernel_spmd

F32 = mybir.dt.float32
BF16 = mybir.dt.bfloat16
AF = mybir.ActivationFunctionType
ALU = mybir.AluOpType

D = 1024
L = 4096
NCTX = 256
T = L + NCTX
NT = T // 128
DEPTH = 4
GRID = 64
WINS = (2, 4, 8, 16)
DFF = 2816
EFF = 3584
NEXP = 8
ALPHA = (2 * DEPTH) ** 0.25
EPS = 1e-5
NCORES = 8
SEQS = 2


class Prog:
    ENGS = ("pe", "dve", "act", "pool", "sp")

    def __init__(self, nc, es):
        self.nc = nc
        self.es = es
        self.streams = {e: [] for e in self.ENGS}
        self.cnt = {e: 0 for e in self.ENGS}
        self.sem = {e: es.enter_context(nc.semaphore("s_" + e)) for e in ("pe", "dve", "act", "pool")}
        self.dsem = {}
        self.res = {}
        self.known = {e: {} for e in self.ENGS}

    def _dma_sem(self, key):
        if key not in self.dsem:
            self.dsem[key] = [self.es.enter_context(self.nc.semaphore("d%d" % len(self.dsem))), 0]
        return self.dsem[key]

    def _need(self, eng, toks):
        need = {}
        for kind, (teng, sem, val) in toks:
            if teng == eng and eng in ("pe", "sp"):
                continue
            k = id(sem)
            if k not in need or need[k][1] < val:
                need[k] = (sem, val)
        out = []
        kn = self.known[eng]
        for k, (sem, val) in need.items():
            if kn.get(k, 0) >= val:
                continue
            kn[k] = val
            out.append((sem, val))
        return out

    def _deps(self, eng, reads, writes):
        toks = []
        for r in reads:
            st = self.res.get(r)
            if st and st["w"] is not None:
                toks.append(("raw", st["w"]))
        for w in writes:
            st = self.res.get(w)
            if st:
                if st["w"] is not None:
                    toks.append(("waw", st["w"]))
                for t in st["r"]:
                    toks.append(("war", t))
        return self._need(eng, toks)

    def _commit(self, tok, reads, writes):
        for r in reads:
            st = self.res.setdefault(r, {"w": None, "r": []})
            st["r"].append(tok)
        for w in writes:
            self.res[w] = {"w": tok, "r": []}

    def op(self, eng, fn, reads=(), writes=()):
        waits = self._deps(eng, reads, writes)
        self.cnt[eng] += 1
        sem = self.sem[eng]
        self.streams[eng].append((waits, fn, sem, 1))
        self._commit((eng, sem, self.cnt[eng]), reads, writes)

    def dma(self, q, out, in_, reads, writes, key, **kw):
        waits = self._deps(q, reads, writes)
        ds = self._dma_sem(key)
        ds[1] += 16
        sem, val = ds[0], ds[1]

        def fn(e, out=out, in_=in_, kw=kw):
            return e.dma_start(out=out, in_=in_, **kw)

        self.streams[q].append((waits, fn, sem, 16))
        self._commit(("dma", sem, val), reads, writes)

    def barrier(self):
        toks = []
        for e in ("pe", "dve", "act", "pool"):
            if self.cnt[e]:
                toks.append(("raw", (e, self.sem[e], self.cnt[e])))
        for key, (sem, val) in self.dsem.items():
            if val:
                toks.append(("raw", ("dma", sem, val)))
        for e in self.ENGS:
            waits = self._need(e, [t for t in toks if t[1][0] != e or e == "sp"])
            if waits:
                self.streams[e].append((waits, None, None, 0))
        self.res = {}

    def emit(self, block):
        engs = {"pe": block.tensor, "dve": block.vector, "act": block.scalar, "pool": block.gpsimd, "sp": block.sync}
        for name, deco in engs.items():
            stream = self.streams[name]
            if not stream:
                continue

            def body(e, stream=stream):
                for waits, fn, sem, inc in stream:
                    for ws, wv in waits:
                        e.wait_ge(ws, wv)
                    if fn is not None:
                        fn(e).then_inc(sem, inc)

            deco(body)
            self.streams[name] = []


def _box_matrix(n, w):
    left = w // 2
    right = w - 1 - left
    A = np.zeros((n, n), np.float64)
    for t in range(n):
        lo = max(t - left, 0)
        hi = min(t + right + 1, n)
        A[t, lo:hi] = 1.0 / (hi - lo)
    return A


def _pool_constants():
    sets = []
    lists = []
    mats = []
    for si, ob in enumerate((0, 3, 7)):
        lst_g = []
        for g, w in enumerate(WINS):
            A = _box_matrix(GRID, w)
            lst = []
            rows_out = np.arange(8 * ob, 8 * ob + 8)
            for tin in range(32):
                rows_in = np.arange(2 * tin, 2 * tin + 2)
                Ar = A[np.ix_(rows_out, rows_in)]
                if not np.any(Ar):
                    continue
                M = np.kron(Ar, A)
                if tin // 4 == ob:
                    o0 = (tin - 4 * ob) * 128
                    M[o0:o0 + 128, :] -= np.eye(128)
                lst.append((tin - 4 * ob, len(mats)))
                mats.append(M.T.astype(np.float32))
            lst_g.append(lst)
        lists.append(lst_g)
    out_sets = []
    out_lists = []
    for si in range(3):
        slots = []
        lg = []
        for g in range(4):
            l2 = []
            for rel, mi in lists[si][g]:
                l2.append((rel, len(slots)))
                slots.append(mats[mi])
            lg.append(l2)
        out_sets.append(np.stack(slots))
        out_lists.append(lg)
    nmax = max(s.shape[0] for s in out_sets)
    PM = np.zeros((3, nmax, 128, 512), np.float32)
    for si in range(3):
        PM[si, :out_sets[si].shape[0]] = out_sets[si]
    PMC = np.zeros((4, 2, 128, 256), np.float32)
    for g, w in enumerate(WINS):
        A = _box_matrix(NCTX, w) - np.eye(NCTX)
        for tin in range(2):
            PMC[g, tin] = A[:, tin * 128:(tin + 1) * 128].T
    return PM.astype(ml_dtypes.bfloat16), out_lists, PMC.astype(ml_dtypes.bfloat16)


def _rope_tables():
    t = np.arange(L)
    row = (t // GRID).astype(np.float32)
    col = (t % GRID).astype(np.float32)
    n_freq = 32
    inv = np.exp(-np.log(np.float32(10000.0)) * np.arange(n_freq, dtype=np.float32) / n_freq).astype(np.float32)
    ang = np.concatenate([row[:, None] * inv, col[:, None] * inv], -1).astype(np.float32)
    cos = np.ones((T, 64), np.float32)
    sin = np.zeros((T, 64), np.float32)
    cos[:L] = np.cos(ang)
    sin[:L] = np.sin(ang)
    cosT = np.ascontiguousarray(cos.reshape(NT, 128, 64).transpose(1, 0, 2))
    sinT = np.ascontiguousarray(sin.reshape(NT, 128, 64).transpose(1, 0, 2))
    return cosT, sinT


def _misc_constants():
    j = np.arange(128, dtype=np.float32)
    E = np.zeros((128, 16), np.float32)
    for h in range(4):
        E[:, 0 + h] = j - 127.0
        E[:, 4 + h] = -j
        E[:, 8 + h] = 127.0 - j
        E[:, 12 + h] = j
    jj = np.arange(128)[:, None]
    ii = np.arange(128)[None, :]
    masks = np.zeros((128, 2, 128), np.float32)
    masks[:, 0, :] = (ii >= jj)
    masks[:, 1, :] = (ii <= jj)
    return E, masks


def build(layers=(0, 1, 2, 3), stop_after=None, pm_lists=None, pm_slots=31):
    nc = bass.Bass("TRN2", target_bir_lowering=False)
    dt = nc.dram_tensor

    def inp(name, shape, dtype=F32):
        return dt(name, list(shape), dtype, kind="ExternalInput").ap()

    def scr(name, shape, dtype):
        return dt(name, list(shape), dtype).ap()

    xT_in = inp("xT", [SEQS, 8, 128, T])
    cT_in = inp("cT", [128, 8, 3])
    cos_in = inp("cosT", [128, NT, 64])
    sin_in = inp("sinT", [128, NT, 64])
    etab_in = inp("etab", [128, 16])
    mask_in = inp("masks", [128, 2, 128])
    pm_in = inp("pm", [3, pm_slots, 128, 512], BF16)
    pmc_in = inp("pmc", [4, 2, 128, 256], BF16)
    ident_in = inp("ident", [128, 128])
    W = {}
    for l in layers:
        W[l] = dict(
            ada_w=inp("ada_w%d" % l, [D, 6 * D]),
            ada_b=inp("ada_bT%d" % l, [128, 48]),
            w_in=inp("w_in%d" % l, [D, 5632]),
            pool_w=inp("pool_w%d" % l, [4, 128, 128]),
            psc=inp("pscT%d" % l, [128, 4]),
            wpo=inp("w_pool_out%d" % l, [512, D]),
            wro=inp("w_ret_out%d" % l, [D, D]),
            logit=inp("logit_bc%d" % l, [128, 8]),
            wo=inp("w_out%d" % l, [D, D]),
            lnp=inp("lnT%d" % l, [128, 4, 8]),
        )
        if l % 2 == 0:
            W[l].update(
                w1=inp("ffn_w1_%d" % l, [D, DFF]),
                w3=inp("ffn_w3_%d" % l, [D, DFF]),
                w2=inp("ffn_w2_%d" % l, [DFF, D]),
            )
        else:
            W[l].update(
                wr=inp("moe_router%d" % l, [D, NEXP]),
                w1=inp("moe_w1_%d" % l, [NEXP, D, EFF]),
                w3=inp("moe_w3_%d" % l, [NEXP, D, EFF]),
                w2=inp("moe_w2_%d" % l, [NEXP, EFF, D]),
            )
    outT = dt("outT", [SEQS, 8, 128, L], F32, kind="ExternalOutput").ap()

    XS = scr("XS", [SEQS, 8, 128, T], F32)
    U_d = scr("U_d", [SEQS, T, 512], BF16)
    V_d = scr("V_d", [SEQS, T, 1024], BF16)
    KF_d = scr("KF_d", [SEQS, T, 512], BF16)
    KB_d = scr("KB_d", [SEQS, T, 512], BF16)
    QKT_d = scr("QKT_d", [SEQS, 16, 128, T], BF16)
    G_d = [scr("G%d_d" % i, [SEQS, 8, 128, T], BF16) for i in range(3)]
    DST_d = scr("DST_d", [SEQS, 2, NT, 4, 128, 256], BF16)
    ZR_d = scr("ZR_d", [SEQS, 8, 128, T], BF16)

    blocks = [(i * 512, 512, False) for i in range(8)] + [(L, NCTX, True)]

    with ExitStack() as es:
        P = Prog(nc, es)
        block = es.enter_context(nc.Block())
        sb = lambda name, shape, dtype=F32: es.enter_context(nc.sbuf_tensor(name, list(shape), dtype))
        ident_f = sb("ident_f", [128, 128])
        ident_b = sb("ident_b", [128, 128], BF16)
        ones_b = sb("ones_b", [128, 128], BF16)
        ones_f = sb("ones_f", [128, 128])
        masks = sb("masks_s", [128, 2, 128])
        etab = sb("etab_s", [128, 16])
        cT = sb("cT_s", [128, 8, 3])
        sT = sb("sT_s", [128, 8, 3])
        MOD = sb("MOD", [128, 48, 3])
        lnp = sb("lnp", [128, 4, 8])
        psc = sb("psc", [128, 4])
        lgt = sb("lgt", [128, 8])
        lg16 = sb("lg16", [128, 16])
        DEC = sb("DEC", [128, 16])
        GC = sb("GC", [128, 8])
        epsb = sb("epsb", [128, 2])
        PS = [es.enter_context(nc.psum_tensor("ps%d" % i, [128, 512], F32)) for i in range(8)]

        P.dma("sp", ident_f[:], ident_in, [], ["ident_f"], "c0")
        P.dma("sp", masks[:], mask_in, [], ["masks"], "c1")
        P.dma("sp", etab[:], etab_in, [], ["etab"], "c2")
        P.dma("sp", cT[:], cT_in, [], ["cT"], "c3")
        P.op("dve", lambda e: e.tensor_copy(out=ident_b[:], in_=ident_f[:]), ["ident_f"], ["ident_b"])
        P.op("dve", lambda e: e.memset(ones_b[:], 1.0 / 1024.0), [], ["ones_b"])
        P.op("dve", lambda e: e.memset(ones_f[:], 1.0 / 256.0), [], ["ones_f"])
        P.op("dve", lambda e: e.memset(epsb[:, 0:1], EPS), [], ["epsb0"])
        P.op("dve", lambda e: e.memset(epsb[:, 1:2], EPS / (ALPHA * ALPHA)), [], ["epsb1"])
        P.op("act", lambda e: e.activation(out=sT[:], in_=cT[:], func=AF.Silu), ["cT"], ["sT"])

        def stage_end():
            P.barrier()
            P.emit(block)

        def ln_block(ss, xb, n, gk, bk, epscol, pm_i, pe_i, XK="xb"):
            zb, sq, ms, m2, rs = ss["zb"], ss["sq"], ss["ms"], ss["m2"], ss["rs"]
            P.op("act", lambda e: e.activation(out=zb[:, :, :n], in_=xb[:, :, :n], func=AF.Copy), [XK], ["zb"])
            P.op("act", lambda e: e.activation(out=sq[:, :, :n], in_=xb[:, :, :n], func=AF.Square), [XK], ["sq"])

            def mm1(e):
                for k in range(8):
                    r = e.matmul(PS[pm_i][:, :n], lhsT=ones_b[:], rhs=zb[:, k, :n], start=(k == 0), stop=(k == 7))
                return r

            def mm2(e):
                for k in range(8):
                    r = e.matmul(PS[pe_i][:, :n], lhsT=ones_b[:], rhs=sq[:, k, :n], start=(k == 0), stop=(k == 7))
                return r

            P.op("pe", mm1, ["zb", "ones_b"], [("ps", pm_i)])
            P.op("pe", mm2, ["sq", "ones_b"], [("ps", pe_i)])
            P.op("act", lambda e: e.activation(out=ms[:, :n], in_=PS[pm_i][:, :n], func=AF.Copy), [("ps", pm_i)], ["ms"])
            P.op("act", lambda e: e.activation(out=m2[:, :n], in_=PS[pm_i][:, :n], func=AF.Square), [("ps", pm_i)], ["m2"])
            P.op("dve", lambda e: e.tensor_tensor(out=rs[:, :n], in0=PS[pe_i][:, :n], in1=m2[:, :n], op=ALU.subtract),
                 [("ps", pe_i), "m2"], ["rs"])
            P.op("act", lambda e: e.activation(out=rs[:, :n], in_=rs[:, :n], func=AF.Ln, bias=epsb[:, epscol:epscol + 1]),
                 ["rs", "epsb%d" % epscol], ["rs"])
            P.op("act", lambda e: e.activation(out=rs[:, :n], in_=rs[:, :n], func=AF.Exp, scale=-0.5), ["rs"], ["rs"])
            P.op("dve", lambda e: e.tensor_tensor(out=xb[:, :, :n], in0=xb[:, :, :n],
                                                  in1=ms[:, :n].unsqueeze(1).to_broadcast([128, 8, n]), op=ALU.subtract),
                 [XK, "ms"], [XK])
            P.op("dve", lambda e: e.tensor_tensor(out=xb[:, :, :n], in0=xb[:, :, :n],
                                                  in1=rs[:, :n].unsqueeze(1).to_broadcast([128, 8, n]), op=ALU.mult),
                 [XK, "rs"], [XK])
            for k in range(8):
                P.op("act", lambda e, k=k: e.activation(out=xb[:, k, :n], in_=xb[:, k, :n], func=AF.Identity,
                                                         scale=lnp[:, gk, k:k + 1], bias=lnp[:, bk, k:k + 1]),
                     [XK, "lnp"], [XK])

        def load_cast(dst, src_ap, nk, ncols, stage, tag, piece=512):
            srcv = src_ap.rearrange("(k p) n -> p k n", p=128)
            i = 0
            for c0 in range(0, ncols, piece):
                cw = min(piece, ncols - c0)
                for k0 in range(0, nk, 8):
                    kw = min(8, nk - k0)
                    st = stage[i % 2]
                    P.dma("sp", st[:, :kw, :cw], srcv[:, k0:k0 + kw, c0:c0 + cw], [], [("wst", i % 2)], ("wst", i % 2))
                    P.op("pool", lambda e, st=st, kw=kw, cw=cw, k0=k0, c0=c0: e.tensor_copy(
                        out=dst[:, k0:k0 + kw, c0:c0 + cw], in_=st[:, :kw, :cw]), [("wst", i % 2)], [tag])
                    i += 1

        for li, l in enumerate(layers):
            w = W[l]
            xsrc = xT_in if li == 0 else XS
            need_ctx = l < DEPTH - 1
            with ExitStack() as ls:
                lsb = lambda name, shape, dtype=F32: ls.enter_context(nc.sbuf_tensor(name + "_L%d" % l, list(shape), dtype))
                adst = [lsb("adst%d" % i, [128, 8, 768]) for i in range(2)]
                adb = lsb("adb", [128, 48])
                P.dma("sp", adb[:], w["ada_b"], [], ["adb"], "p0")
                P.dma("sp", lnp[:], w["lnp"], [], ["lnp"], "p1")
                P.dma("sp", psc[:], w["psc"], [], ["psc"], "p2")
                P.dma("sp", lgt[:], w["logit"], [], ["lgt"], "p3")
                adv = w["ada_w"].rearrange("(k p) n -> p k n", p=128)
                for pc in range(8):
                    st = adst[pc % 2]
                    P.dma("sp", st[:], adv[:, :, pc * 768:(pc + 1) * 768], [], [("adst", pc % 2)], ("adst", pc % 2))

                    def mm(e, st=st, pc=pc):
                        for oc in range(6):
                            og = pc * 6 + oc
                            for k in range(8):
                                r = e.matmul(PS[0][:, og * 3:og * 3 + 3], lhsT=st[:, k, oc * 128:(oc + 1) * 128],
                                             rhs=sT[:, k, :], start=(k == 0), stop=(k == 7))
                        return r

                    P.op("pe", mm, [("adst", pc % 2), "sT"], [("ps", 0)])
                P.op("dve", lambda e: e.tensor_tensor(out=MOD[:], in0=PS[0][:, 0:144].rearrange("p (a b) -> p a b", b=3),
                                                      in1=adb[:].unsqueeze(2).to_broadcast([128, 48, 3]), op=ALU.add),
                     [("ps", 0), "adb"], ["MOD"])
                P.op("dve", lambda e: e.tensor_scalar(out=MOD[:, 8:16, :], in0=MOD[:, 8:16, :], scalar1=1.0, scalar2=None, op0=ALU.add), ["MOD"], ["MOD"])
                P.op("dve", lambda e: e.tensor_scalar(out=MOD[:, 16:24, :], in0=MOD[:, 16:24, :], scalar1=1.0 / ALPHA, scalar2=None, op0=ALU.mult), ["MOD"], ["MOD"])
                P.op("dve", lambda e: e.tensor_scalar(out=MOD[:, 32:40, :], in0=MOD[:, 32:40, :], scalar1=1.0, scalar2=None, op0=ALU.add), ["MOD"], ["MOD"])
                P.op("dve", lambda e: e.tensor_scalar(out=MOD[:, 40:48, :], in0=MOD[:, 40:48, :], scalar1=1.0 / ALPHA, scalar2=None, op0=ALU.mult), ["MOD"], ["MOD"])
                P.op("act", lambda e: e.activation(out=lgt[:], in_=lgt[:], func=AF.Exp, scale=-1.0), ["lgt"], ["lgt"])
                P.op("act", lambda e: e.activation(out=lgt[:], in_=lgt[:], func=AF.Ln, bias=1.0), ["lgt"], ["lgt"])
                P.op("dve", lambda e: e.tensor_scalar(out=lg16[:, 0:8], in0=lgt[:], scalar1=-1.0, scalar2=None, op0=ALU.mult), ["lgt"], ["lg16"])
                P.op("dve", lambda e: e.tensor_scalar(out=lg16[:, 8:16], in0=lgt[:], scalar1=-1.0, scalar2=None, op0=ALU.mult), ["lgt", "lg16"], ["lg16"])
                P.op("act", lambda e: e.activation(out=GC[:], in_=lg16[:, 0:8], func=AF.Exp, scale=128.0), ["lg16"], ["GC"])
                P.op("dve", lambda e: e.tensor_tensor(out=DEC[:], in0=lg16[:], in1=etab[:], op=ALU.mult), ["lg16", "etab"], ["DEC"])
                P.op("act", lambda e: e.activation(out=DEC[:], in_=DEC[:], func=AF.Exp), ["DEC"], ["DEC"])
                P.op("dve", lambda e: e.tensor_scalar(out=DEC[:, 8:16], in0=DEC[:, 8:16], scalar1=128.0 ** -0.5, scalar2=None, op0=ALU.mult), ["DEC"], ["DEC"])
                stage_end()
            if stop_after == ("params", l):
                break

            def modp(m, k, tc):
                return MOD[:, m * 8 + k, tc:tc + 1]

            with ExitStack() as ls:
                lsb = lambda name, shape, dtype=F32: ls.enter_context(nc.sbuf_tensor(name + "_L%d" % l, list(shape), dtype))
                WB = lsb("WB", [128, 8, 5632], BF16)
                with ExitStack() as ls2:
                    wst = [ls2.enter_context(nc.sbuf_tensor("wst%d_L%d" % (i, l), [128, 8, 512], F32)) for i in range(2)]
                    load_cast(WB, w["w_in"], 8, 5632, wst, "WB")
                    stage_end()
                cosT = lsb("cosT_s", [128, NT, 64])
                sinT = lsb("sinT_s", [128, NT, 64])
                P.dma("sp", cosT[:], cos_in, [], ["cosT"], "c4")
                P.dma("sp", sinT[:], sin_in, [], ["sinT"], "c5")
                xbs = [lsb("xa%d" % i, [128, 8, 512]) for i in range(2)]
                hb = [lsb("ha%d" % i, [128, 8, 512], BF16) for i in range(1)]
                ub = lsb("ub", [128, 512], BF16)
                vb = lsb("vb", [128, 1024], BF16)
                rt = [lsb("rt%d" % i, [128, 4, 64]) for i in range(4)]
                rot = lsb("rot", [128, 4, 2, 64])
                var_tm = lsb("var_tm", [128, 4, 512], BF16)
                qkT = lsb("qkT", [128, 16, 512], BF16)
                gb = [lsb("gb%d" % i, [128, 8, 512], BF16) for i in range(3)]
                bi = 0
                for s in range(SEQS):
                    for (t0, n, isctx) in blocks:
                        tc = 2 if isctx else s
                        xb = xbs[bi % 2]
                        h = hb[0]
                        xk, hk = ("xa", bi % 2), ("ha", 0)
                        P.dma("sp", xb[:, :, :n], xsrc[s, :, :, t0:t0 + n].rearrange("k p t -> p k t"),
                              [("XS", s, t0)], [xk], xk)
                        for k in range(8):
                            P.op("act", lambda e, k=k, xb=xb, h=h, n=n, tc=tc: e.activation(
                                out=h[:, k, :n], in_=xb[:, k, :n], func=AF.Identity,
                                scale=modp(1, k, tc), bias=modp(0, k, tc)), [xk, "MOD"], [hk])
                        for ti in range(n // 128):
                            gt = (t0 // 128) + ti
                            tsl = slice(ti * 128, (ti + 1) * 128)
                            rows = slice(t0 + ti * 128, t0 + ti * 128 + 128)
                            for bnk, c0 in enumerate((0, 512, 1024, 1536, 2048)):
                                def mm(e, bnk=bnk, c0=c0, h=h, tsl=tsl):
                                    for k in range(8):
                                        r = e.matmul(PS[bnk][:, :], lhsT=h[:, k, tsl], rhs=WB[:, k, c0:c0 + 512],
                                                     start=(k == 0), stop=(k == 7))
                                    return r
                                P.op("pe", mm, [hk, "WB"], [("ps", bnk)])
                            P.op("act", lambda e: e.activation(out=ub[:], in_=PS[0][:, :], func=AF.Copy), [("ps", 0)], ["ub"])
                            P.dma("sp", U_d[s, rows, :], ub[:], ["ub"], [("U", s, gt)], "ub")
                            P.op("act", lambda e: e.activation(out=vb[:, 0:512], in_=PS[3][:, :], func=AF.Copy), [("ps", 3)], ["vb0"])
                            P.op("dve", lambda e: e.tensor_copy(out=vb[:, 512:1024], in_=PS[4][:, :]), [("ps", 4)], ["vb1"])
                            P.dma("sp", V_d[s, rows, :], vb[:], ["vb0", "vb1"], [("V", s, gt)], "vb")
                            for qi, bnk in enumerate((1, 2)):
                                pv = PS[bnk][:, :].rearrange("p (h two d) -> p h two d", two=2, d=64)
                                Cb = cosT[:, gt, :].unsqueeze(1).to_broadcast([128, 4, 64])
                                Sb = sinT[:, gt, :].unsqueeze(1).to_broadcast([128, 4, 64])
                                P.op("dve", lambda e, pv=pv, Cb=Cb: e.tensor_tensor(out=rt[0][:], in0=pv[:, :, 0, :], in1=Cb, op=ALU.mult), [("ps", bnk), "cosT"], ["rt0"])
                                P.op("dve", lambda e, pv=pv, Sb=Sb: e.tensor_tensor(out=rt[1][:], in0=pv[:, :, 1, :], in1=Sb, op=ALU.mult), [("ps", bnk), "sinT"], ["rt1"])
                                P.op("dve", lambda e, pv=pv, Sb=Sb: e.tensor_tensor(out=rt[2][:], in0=pv[:, :, 0, :], in1=Sb, op=ALU.mult), [("ps", bnk), "sinT"], ["rt2"])
                                P.op("dve", lambda e, pv=pv, Cb=Cb: e.tensor_tensor(out=rt[3][:], in0=pv[:, :, 1, :], in1=Cb, op=ALU.mult), [("ps", bnk), "cosT"], ["rt3"])
                                P.op("dve", lambda e: e.tensor_tensor(out=rot[:, :, 0, :], in0=rt[0][:], in1=rt[1][:], op=ALU.subtract), ["rt0", "rt1"], ["rot0"])
                                P.op("dve", lambda e: e.tensor_tensor(out=rot[:, :, 1, :], in0=rt[2][:], in1=rt[3][:], op=ALU.add), ["rt2", "rt3"], ["rot1"])
                                for dr in range(2):
                                    vi = qi * 2 + dr
                                    for hh in range(4):
                                        P.op("act", lambda e, vi=vi, hh=hh, dr=dr, qi=qi: e.activation(
                                            out=var_tm[:, vi, hh * 128:(hh + 1) * 128],
                                            in_=rot[:, hh, :, :].rearrange("p a b -> p (a b)"), func=AF.Identity,
                                            scale=DEC[:, qi * 8 + dr * 4 + hh:qi * 8 + dr * 4 + hh + 1]),
                                            ["rot0", "rot1", "DEC"], [("var", vi)])
                            P.dma("sp", KF_d[s, rows, :], var_tm[:, 2, :], [("var", 2)], [("KF", s, gt)], "kf")
                            P.dma("sp", KB_d[s, rows, :], var_tm[:, 3, :], [("var", 3)], [("KB", s, gt)], "kb")
                            p5 = PS[5][:, :].bitcast(BF16).rearrange("p (a b) -> p a b", b=128)[:, 0:8, :]
                            for half in range(2):
                                def tr(e, half=half):
                                    for j in range(8):
                                        vi = half * 2 + j // 4
                                        hh = j % 4
                                        r = e.transpose(p5[:, j, :], var_tm[:, vi, hh * 128:(hh + 1) * 128], ident_b[:])
                                    return r
                                P.op("pe", tr, [("var", half * 2), ("var", half * 2 + 1), "ident_b"], [("ps", 5)])
                                P.op("dve", lambda e, half=half, tsl=tsl: e.tensor_copy(out=qkT[:, half * 8:(half + 1) * 8, tsl], in_=p5),
                                     [("ps", 5)], [("qkT", half)])
                        P.dma("sp", QKT_d[s, :, :, t0:t0 + n].rearrange("a p t -> p a t"), qkT[:, :, :n],
                              [("qkT", 0), ("qkT", 1)], [("QKT", s, t0)], "qkT")
                        for gi in range(3):
                            func = AF.Silu if gi == 0 else AF.Sigmoid
                            for oc in range(8):
                                bnk = 6 + (oc % 2)
                                c0 = 2560 + gi * 1024 + oc * 128
                                def mm(e, bnk=bnk, c0=c0, h=h, n=n):
                                    for k in range(8):
                                        r = e.matmul(PS[bnk][:, :n], lhsT=WB[:, k, c0:c0 + 128], rhs=h[:, k, :n],
                                                     start=(k == 0), stop=(k == 7))
                                    return r
                                P.op("pe", mm, [hk, "WB"], [("ps", bnk)])
                                P.op("act", lambda e, bnk=bnk, gi=gi, oc=oc, n=n, func=func: e.activation(
                                    out=gb[gi][:, oc, :n], in_=PS[bnk][:, :n], func=func), [("ps", bnk)], [("gb", gi, oc)])
                            P.dma("sp", G_d[gi][s, :, :, t0:t0 + n].rearrange("k p t -> p k t"), gb[gi][:, :, :n],
                                  [("gb", gi, oc) for oc in range(8)], [("G", gi, s, t0)], ("gb", gi))
                        bi += 1
                stage_end()
            if stop_after == ("A", l):
                break

            with ExitStack() as ls:
                lsb = lambda name, shape, dtype=F32: ls.enter_context(nc.sbuf_tensor(name + "_L%d" % l, list(shape), dtype))
                Sst = lsb("Sst", [128, 1024])
                Df = lsb("Df", [128, 1024])
                Dbf = [lsb("Dbf%d" % i, [128, 1024], BF16) for i in range(2)]
                kt = [lsb("kt%d" % i, [128, 512], BF16) for i in range(2)]
                vt = [lsb("vt%d" % i, [128, 1024], BF16) for i in range(2)]
                it = 0
                for s in range(SEQS):
                    for dr in range(2):
                        order = [32, 33] + list(range(32)) if dr == 0 else [33, 32] + list(range(31, -1, -1))
                        Ksrc = KF_d if dr == 0 else KB_d
                        P.op("dve", lambda e: e.memset(Sst[:], 0.0), [], ["S"])
                        for c in order:
                            j = it % 2
                            rows = slice(c * 128, (c + 1) * 128)
                            P.dma("sp", kt[j][:], Ksrc[s, rows, :], [("KF", s), ("KB", s)], [("kt", j)], ("kt", j))
                            P.dma("sp", vt[j][:], V_d[s, rows, :], [("V", s)], [("vt", j)], ("vt", j))
                            for hh in range(4):
                                P.op("dve", lambda e, hh=hh, dr=dr: e.tensor_scalar(
                                    out=Df[:, hh * 256:(hh + 1) * 256], in0=Sst[:, hh * 256:(hh + 1) * 256],
                                    scalar1=GC[:, dr * 4 + hh:dr * 4 + hh + 1], scalar2=None, op0=ALU.mult), ["S", "GC"], [("Df", hh)])
                            P.op("act", lambda e, j=j: e.activation(out=Dbf[j][:], in_=Df[:], func=AF.Copy),
                                 [("Df", hh) for hh in range(4)], [("Dbf", j)])
                            P.dma("sp", DST_d[s, dr, c].rearrange("h p v -> p h v"),
                                  Dbf[j][:].rearrange("p (h v) -> p h v", v=256), [("Dbf", j)], [("DST", s, dr, c)], ("Dbf", j))

                            def mm(e, j=j):
                                for hh in range(4):
                                    r = e.matmul(PS[hh // 2][:, (hh % 2) * 256:(hh % 2) * 256 + 256],
                                                 lhsT=kt[j][:, hh * 128:(hh + 1) * 128], rhs=vt[j][:, hh * 256:(hh + 1) * 256],
                                                 start=True, stop=True)
                                return r
                            P.op("pe", mm, [("kt", j), ("vt", j)], [("ps", 0), ("ps", 1)])
                            for b2 in range(2):
                                P.op("dve", lambda e, b2=b2: e.tensor_tensor(
                                    out=Sst[:, b2 * 512:(b2 + 1) * 512], in0=PS[b2][:, :], in1=Df[:, b2 * 512:(b2 + 1) * 512], op=ALU.add),
                                    [("ps", b2), ("Df", 2 * b2), ("Df", 2 * b2 + 1)], ["S"])
                            it += 1
                stage_end()

            with ExitStack() as ls:
                lsb = lambda name, shape, dtype=F32: ls.enter_context(nc.sbuf_tensor(name + "_L%d" % l, list(shape), dtype))
                qk = [lsb("qk%d" % i, [128, 16, 128], BF16) for i in range(2)]
                vt = [lsb("vc%d" % i, [128, 1024], BF16) for i in range(2)]
                Dt = [lsb("Dt%d" % i, [128, 2, 4, 256], BF16) for i in range(2)]
                sg = [lsb("sg%d" % i, [128, 8, 128], BF16) for i in range(2)]
                PT = lsb("PT", [128, 8, 128], BF16)
                of = lsb("of", [128, 8, 128])
                osq = lsb("osq", [128, 8, 128])
                msr = lsb("msr", [128, 4, 128])
                m2r = lsb("m2r", [128, 4, 128])
                rsr = lsb("rsr", [128, 4, 128])
                zrt = [lsb("zrt%d" % i, [128, 8, 128], BF16) for i in range(2)]
                it = 0
                ntl = NT if need_ctx else 32
                for s in range(SEQS):
                    for c in range(ntl):
                        j = it % 2
                        cs = slice(c * 128, (c + 1) * 128)
                        P.dma("sp", qk[j][:], QKT_d[s, :, :, cs].rearrange("a p t -> p a t"), [("QKT", s)], [("qk", j)], ("qk", j))
                        P.dma("sp", vt[j][:], V_d[s, cs, :], [("V", s)], [("vc", j)], ("vc", j))
                        for dr in range(2):
                            P.dma("sp", Dt[j][:, dr], DST_d[s, dr, c].rearrange("h p v -> p h v"), [("DST", s)], [("Dt", j)], ("Dt", j))
                        P.dma("sp", sg[j][:], G_d[0][s, :, :, cs].rearrange("k p t -> p k t"), [("G", 0, s)], [("sg", j)], ("sg", j))
                        for dr in range(2):
                            def mm(e, dr=dr, j=j):
                                for hh in range(4):
                                    r = e.matmul(PS[dr][:, hh * 128:(hh + 1) * 128], lhsT=qk[j][:, (2 + dr) * 4 + hh, :],
                                                 rhs=qk[j][:, dr * 4 + hh, :], start=True, stop=True)
                                return r
                            P.op("pe", mm, [("qk", j)], [("ps", dr)])
                            P.op("dve", lambda e, dr=dr: e.tensor_tensor(
                                out=PT[:, dr * 4:(dr + 1) * 4, :], in0=PS[dr][:, :].rearrange("p (h i) -> p h i", i=128),
                                in1=masks[:, dr, :].unsqueeze(1).to_broadcast([128, 4, 128]), op=ALU.mult),
                                [("ps", dr), "masks"], [("PT", dr)])
                        for b2 in range(2):
                            def mm(e, b2=b2, j=j):
                                for q in range(4):
                                    ch = b2 * 4 + q
                                    hh, m = ch // 2, ch % 2
                                    o = PS[2 + b2][:, q * 128:(q + 1) * 128]
                                    for dr in range(2):
                                        e.matmul(o, lhsT=vt[j][:, hh * 256 + m * 128:hh * 256 + m * 128 + 128],
                                                 rhs=PT[:, dr * 4 + hh, :], start=(dr == 0), stop=False)
                                        r = e.matmul(o, lhsT=Dt[j][:, dr, hh, m * 128:(m + 1) * 128],
                                                     rhs=qk[j][:, dr * 4 + hh, :], start=False, stop=(dr == 1))
                                return r
                            P.op("pe", mm, [("vc", j), ("PT", 0), ("PT", 1), ("Dt", j), ("qk", j)], [("ps", 2 + b2)])
                            P.op("act", lambda e, b2=b2: e.activation(out=of[:, b2 * 4:(b2 + 1) * 4, :].rearrange("p a b -> p (a b)"),
                                                                     in_=PS[2 + b2][:, :], func=AF.Copy), [("ps", 2 + b2)], [("of", b2)])
                            P.op("act", lambda e, b2=b2: e.activation(out=osq[:, b2 * 4:(b2 + 1) * 4, :].rearrange("p a b -> p (a b)"),
                                                                     in_=PS[2 + b2][:, :], func=AF.Square), [("ps", 2 + b2)], [("osq", b2)])
                        def mmst(e):
                            for hh in range(4):
                                for m in range(2):
                                    e.matmul(PS[4][:, hh * 128:(hh + 1) * 128], lhsT=ones_f[:], rhs=of[:, hh * 2 + m, :],
                                             start=(m == 0), stop=(m == 1))
                            for hh in range(4):
                                for m in range(2):
                                    r = e.matmul(PS[5][:, hh * 128:(hh + 1) * 128], lhsT=ones_f[:], rhs=osq[:, hh * 2 + m, :],
                                                 start=(m == 0), stop=(m == 1))
                            return r
                        P.op("pe", mmst, [("of", 0), ("of", 1), ("osq", 0), ("osq", 1), "ones_f"], [("ps", 4), ("ps", 5)])
                        fl = lambda t: t[:].rearrange("p a b -> p (a b)")
                        P.op("act", lambda e: e.activation(out=fl(msr), in_=PS[4][:, :], func=AF.Copy), [("ps", 4)], ["msr"])
                        P.op("act", lambda e: e.activation(out=fl(m2r), in_=PS[4][:, :], func=AF.Square), [("ps", 4)], ["m2r"])
                        P.op("dve", lambda e: e.tensor_tensor(out=fl(rsr), in0=PS[5][:, :], in1=fl(m2r), op=ALU.subtract), [("ps", 5), "m2r"], ["rsr"])
                        P.op("act", lambda e: e.activation(out=fl(rsr), in_=fl(rsr), func=AF.Ln, bias=epsb[:, 0:1]), ["rsr", "epsb0"], ["rsr"])
                        P.op("act", lambda e: e.activation(out=fl(rsr), in_=fl(rsr), func=AF.Exp, scale=-0.5), ["rsr"], ["rsr"])
                        ov = of[:].rearrange("p (h m) t -> p h m t", m=2)
                        P.op("dve", lambda e, ov=ov: e.tensor_tensor(out=ov, in0=ov, in1=msr[:].unsqueeze(2).to_broadcast([128, 4, 2, 128]), op=ALU.subtract),
                             [("of", 0), ("of", 1), "msr"], [("of", 0), ("of", 1)])
                        P.op("dve", lambda e, ov=ov: e.tensor_tensor(out=ov, in0=ov, in1=rsr[:].unsqueeze(2).to_broadcast([128, 4, 2, 128]), op=ALU.mult),
                             [("of", 0), ("of", 1), "rsr"], [("of", 0), ("of", 1)])
                        P.op("dve", lambda e, j=j: e.tensor_tensor(out=zrt[j][:], in0=of[:], in1=sg[j][:], op=ALU.mult),
                             [("of", 0), ("of", 1), ("sg", j)], [("zrt", j)])
                        P.dma("sp", ZR_d[s, :, :, cs].rearrange("k p t -> p k t"), zrt[j][:], [("zrt", j)], [("ZR", s, c)], ("zrt", j))
                        it += 1
                stage_end()
            if stop_after == ("C1", l):
                break

            with ExitStack() as ls:
                lsb = lambda name, shape, dtype=F32: ls.enter_context(nc.sbuf_tensor(name + "_L%d" % l, list(shape), dtype))
                wro = lsb("wro", [128, 8, 1024], BF16)
                wo = lsb("wo", [128, 8, 1024], BF16)
                wpo = lsb("wpo", [128, 4, 1024], BF16)
                plw = lsb("plw", [128, 4, 128], BF16)
                with ExitStack() as ls2:
                    wst = [ls2.enter_context(nc.sbuf_tensor("wstc%d_L%d" % (i, l), [128, 8, 512], F32)) for i in range(2)]
                    load_cast(wro, w["wro"], 8, 1024, wst, "wro")
                    load_cast(wo, w["wo"], 8, 1024, wst, "wo")
                    load_cast(wpo, w["wpo"], 4, 1024, wst, "wpo")
                    P.dma("sp", wst[0][:, 0:4, 0:128], w["pool_w"].rearrange("g c d -> c g d"), [], [("wst", 0)], ("wst", 0))
                    P.op("pool", lambda e: e.tensor_copy(out=plw[:], in_=wst[0][:, 0:4, 0:128]), [("wst", 0)], ["plw"])
                    stage_end()
                Ures = lsb("Ures", [128, NT, 512], BF16)
                PMb = lsb("PMb", [128, pm_slots, 512], BF16)
                PMc = lsb("PMc", [128, 8, 256], BF16)
                P.dma("sp", PMc[:], pmc_in.rearrange("g t p o -> p (g t) o"), [], ["PMc"], "pmc")
                zr = lsb("zr", [128, 8, 512], BF16)
                spb = lsb("spb", [128, 8, 512], BF16)
                srb = lsb("srb", [128, 8, 512], BF16)
                dTb = lsb("dTb", [128, 4, 512], BF16)
                ygb = lsb("ygb", [128, 4, 512], BF16)
                mixb = lsb("mixb", [128, 8, 512], BF16)
                t1 = lsb("t1", [128, 512])
                t2 = lsb("t2", [128, 512])
                xb = lsb("xc", [128, 8, 512])
                ss = dict(zb=lsb("zb", [128, 8, 512], BF16), sq=lsb("sq", [128, 8, 512], BF16),
                          ms=lsb("ms", [128, 512]), m2=lsb("m2", [128, 512]), rs=lsb("rs", [128, 512]))
                for s in range(SEQS):
                    P.dma("sp", Ures[:], U_d[s].rearrange("(t p) c -> p t c", p=128), [("U", s)], ["Ures"], "Ures")
                    for ob, (t0, n, isctx) in enumerate(blocks):
                        if isctx and not need_ctx:
                            continue
                        tc = 2 if isctx else s
                        bsl = (slice(None), slice(None), slice(t0, t0 + n))
                        P.dma("sp", zr[:, :, :n], ZR_d[s][bsl].rearrange("k p t -> p k t"), [("ZR", s)], ["zr"], "zr")
                        P.dma("sp", spb[:, :, :n], G_d[1][s][bsl].rearrange("k p t -> p k t"), [("G", 1, s)], ["spb"], "spb")
                        P.dma("sp", srb[:, :, :n], G_d[2][s][bsl].rearrange("k p t -> p k t"), [("G", 2, s)], ["srb"], "srb")
                        P.dma("sp", xb[:, :, :n], xsrc[s][bsl].rearrange("k p t -> p k t"), [("XS", s, t0)], ["xb"], "xb")
                        if not isctx:
                            si = 0 if ob == 0 else (2 if ob == 7 else 1)
                            if ob in (0, 1, 7):
                                P.dma("sp", PMb[:], pm_in[si].rearrange("a p o -> p a o"), [], ["PMb"], "PMb")
                        for g in range(4):
                            bnk = g % 2
                            if isctx:
                                lst = [(32 + tt, PMc[:, g * 2 + tt, :]) for tt in range(2)]
                            else:
                                lst = [(4 * ob + rel, PMb[:, slot, :]) for rel, slot in pm_lists[si][g]]
                            def mm(e, lst=lst, g=g, bnk=bnk, n=n):
                                for i2, (tin, pmv) in enumerate(lst):
                                    r = e.matmul(PS[bnk][:, :n], lhsT=Ures[:, tin, g * 128:(g + 1) * 128], rhs=pmv[:, :n],
                                                 start=(i2 == 0), stop=(i2 == len(lst) - 1))
                                return r
                            P.op("pe", mm, ["Ures", "PMb", "PMc"], [("ps", bnk)])
                            P.op("act", lambda e, g=g, bnk=bnk, n=n: e.activation(out=dTb[:, g, :n], in_=PS[bnk][:, :n], func=AF.Copy),
                                 [("ps", bnk)], [("dTb", g)])
                            P.op("pe", lambda e, g=g, bnk=bnk, n=n: e.matmul(PS[2 + bnk][:, :n], lhsT=plw[:, g, :], rhs=dTb[:, g, :n], start=True, stop=True),
                                 [("dTb", g), "plw"], [("ps", 2 + bnk)])
                            P.op("act", lambda e, g=g, bnk=bnk, n=n: e.activation(out=ygb[:, g, :n], in_=PS[2 + bnk][:, :n], func=AF.Identity,
                                                                                   scale=psc[:, g:g + 1]), [("ps", 2 + bnk), "psc"], [("ygb", g)])
                        for oc in range(8):
                            ocs = slice(oc * 128, (oc + 1) * 128)
                            def mmp(e, ocs=ocs, n=n):
                                for g in range(4):
                                    r = e.matmul(PS[4][:, :n], lhsT=wpo[:, g, ocs], rhs=ygb[:, g, :n], start=(g == 0), stop=(g == 3))
                                return r
                            def mmr(e, ocs=ocs, n=n):
                                for k in range(8):
                                    r = e.matmul(PS[5][:, :n], lhsT=wro[:, k, ocs], rhs=zr[:, k, :n], start=(k == 0), stop=(k == 7))
                                return r
                            P.op("pe", mmp, [("ygb", g) for g in range(4)] + ["wpo"], [("ps", 4)])
                            P.op("pe", mmr, ["zr", "wro"], [("ps", 5)])
                            P.op("dve", lambda e, oc=oc, n=n: e.tensor_tensor(out=t1[:, :n], in0=PS[4][:, :n], in1=spb[:, oc, :n], op=ALU.mult),
                                 [("ps", 4), "spb"], ["t1"])
                            P.op("dve", lambda e, oc=oc, n=n: e.tensor_tensor(out=t2[:, :n], in0=PS[5][:, :n], in1=srb[:, oc, :n], op=ALU.mult),
                                 [("ps", 5), "srb"], ["t2"])
                            P.op("dve", lambda e, oc=oc, n=n: e.tensor_tensor(out=mixb[:, oc, :n], in0=t1[:, :n], in1=t2[:, :n], op=ALU.add),
                                 ["t1", "t2"], [("mixb", oc)])
                        for oc2 in range(8):
                            bnk = 6 + oc2 % 2
                            def mmo(e, oc2=oc2, bnk=bnk, n=n):
                                for oc in range(8):
                                    r = e.matmul(PS[bnk][:, :n], lhsT=wo[:, oc, oc2 * 128:(oc2 + 1) * 128], rhs=mixb[:, oc, :n],
                                                 start=(oc == 0), stop=(oc == 7))
                                return r
                            P.op("pe", mmo, [("mixb", oc) for oc in range(8)] + ["wo"], [("ps", bnk)])
                            P.op("dve", lambda e, oc2=oc2, bnk=bnk, n=n, tc=tc: e.scalar_tensor_tensor(
                                out=xb[:, oc2, :n], in0=PS[bnk][:, :n], scalar=modp(2, oc2, tc), in1=xb[:, oc2, :n],
                                op0=ALU.mult, op1=ALU.add), [("ps", bnk), "xb", "MOD"], ["xb"])
                        ln_block(ss, xb, n, 0, 1, 1, 0, 1)
                        P.dma("sp", XS[s][bsl].rearrange("k p t -> p k t"), xb[:, :, :n], ["xb"], [("XS", s, t0)], "xb_st")
                stage_end()
            xsrc = XS
            if stop_after == ("C2", l):
                break

            last = (li == len(layers) - 1)
            if l % 2 == 0:
                with ExitStack() as ls:
                    lsb = lambda name, shape, dtype=F32: ls.enter_context(nc.sbuf_tensor(name + "_L%d" % l, list(shape), dtype))
                    w1 = lsb("w1", [128, 8, DFF], BF16)
                    w3 = lsb("w3", [128, 8, DFF], BF16)
                    w2 = lsb("w2", [128, 22, D], BF16)
                    with ExitStack() as ls2:
                        wst = [ls2.enter_context(nc.sbuf_tensor("wstd%d_L%d" % (i, l), [128, 8, 512], F32)) for i in range(2)]
                        load_cast(w1, w["w1"], 8, DFF, wst, "w1")
                        load_cast(w3, w["w3"], 8, DFF, wst, "w3")
                        load_cast(w2, w["w2"], 22, D, wst, "w2")
                        stage_end()
                    NB = 256
                    xds = [lsb("xd%d" % i, [128, 8, NB]) for i in range(2)]
                    h2 = lsb("h2", [128, 8, NB], BF16)
                    hid = lsb("hid", [128, 22, NB], BF16)
                    sl = [lsb("sl%d" % i, [128, NB]) for i in range(2)]
                    ss = dict(zb=lsb("zbd", [128, 8, NB], BF16), sq=lsb("sqd", [128, 8, NB], BF16),
                              ms=lsb("msd", [128, NB]), m2=lsb("m2d", [128, NB]), rs=lsb("rsd", [128, NB]))
                    bi = 0
                    for s in range(SEQS):
                        ntok = T if need_ctx else L
                        for t0 in range(0, ntok, NB):
                            isctx = t0 >= L
                            tc = 2 if isctx else s
                            xb = xds[bi % 2]
                            xk = ("xd", bi % 2)
                            bsl = (slice(None), slice(None), slice(t0, t0 + NB))
                            P.dma("sp", xb[:], XS[s][bsl].rearrange("k p t -> p k t"), [("XS", s, t0)], [xk], xk)
                            for k in range(8):
                                P.op("act", lambda e, k=k, xb=xb, tc=tc: e.activation(out=h2[:, k, :], in_=xb[:, k, :], func=AF.Identity,
                                                                                      scale=modp(4, k, tc), bias=modp(3, k, tc)), [xk, "MOD"], ["h2"])
                            for ff in range(22):
                                fs = slice(ff * 128, (ff + 1) * 128)
                                b1, b3 = (ff % 2) * 2, (ff % 2) * 2 + 1
                                def mm13(e, fs=fs, b1=b1, b3=b3):
                                    for k in range(8):
                                        e.matmul(PS[b1][:, :NB], lhsT=w1[:, k, fs], rhs=h2[:, k, :], start=(k == 0), stop=(k == 7))
                                    for k in range(8):
                                        r = e.matmul(PS[b3][:, :NB], lhsT=w3[:, k, fs], rhs=h2[:, k, :], start=(k == 0), stop=(k == 7))
                                    return r
                                P.op("pe", mm13, ["h2", "w1", "w3"], [("ps", b1), ("ps", b3)])
                                P.op("act", lambda e, ff=ff, b1=b1: e.activation(out=sl[ff % 2][:], in_=PS[b1][:, :NB], func=AF.Silu), [("ps", b1)], [("sl", ff % 2)])
                                P.op("dve", lambda e, ff=ff, b3=b3: e.tensor_tensor(out=hid[:, ff, :], in0=PS[b3][:, :NB], in1=sl[ff % 2][:], op=ALU.mult),
                                     [("ps", b3), ("sl", ff % 2)], [("hid", ff)])
                            for oc in range(8):
                                bnk = 4 + oc % 4
                                def mm2_(e, oc=oc, bnk=bnk):
                                    for ff in range(22):
                                        r = e.matmul(PS[bnk][:, :NB], lhsT=w2[:, ff, oc * 128:(oc + 1) * 128], rhs=hid[:, ff, :],
                                                     start=(ff == 0), stop=(ff == 21))
                                    return r
                                P.op("pe", mm2_, [("hid", ff) for ff in range(22)] + ["w2"], [("ps", bnk)])
                                P.op("dve", lambda e, oc=oc, bnk=bnk, xb=xb, tc=tc: e.scalar_tensor_tensor(
                                    out=xb[:, oc, :], in0=PS[bnk][:, :NB], scalar=modp(5, oc, tc), in1=xb[:, oc, :],
                                    op0=ALU.mult, op1=ALU.add), [("ps", bnk), xk, "MOD"], [xk])
                            ln_block(ss, xb, NB, 2, 3, 1, 0, 1, XK=xk)
                            if last:
                                if not isctx:
                                    P.dma("sp", outT[s][bsl].rearrange("k p t -> p k t"), xb[:], [xk], [("OUT",)], ("xd_st", bi % 2))
                            else:
                                P.dma("sp", XS[s][bsl].rearrange("k p t -> p k t"), xb[:], [xk], [("XS", s, t0)], ("xd_st", bi % 2))
                            bi += 1
                    stage_end()
            else:
                with ExitStack() as ls:
                    lsb = lambda name, shape, dtype=F32: ls.enter_context(nc.sbuf_tensor(name + "_L%d" % l, list(shape), dtype))
                    NB = 256
                    wrb = lsb("wrb", [128, 8, NEXP], BF16)
                    wrf = lsb("wrf", [128, 8, NEXP])
                    P.dma("sp", wrf[:], w["wr"].rearrange("(k p) e -> p k e", p=128), [], ["wrf"], "wrf")
                    P.op("dve", lambda e: e.tensor_copy(out=wrb[:], in_=wrf[:]), ["wrf"], ["wrb"])
                    wst = [lsb("wste%d" % i, [128, 4096]) for i in range(2)]
                    wsl = [[lsb("wsl%d_%d" % (i, j), [128, 4096], BF16) for j in range(3)] for i in range(2)]
                    xe = lsb("xe", [128, 8, 1024])
                    h2 = lsb("h2e", [128, 8, 1024], BF16)
                    gw = lsb("gw", [128, NEXP, 1024])
                    hid = [lsb("hide%d" % i, [128, 4, NB], BF16) for i in range(2)]
                    sl = [lsb("sle%d" % i, [128, NB]) for i in range(2)]
                    tg = [lsb("tge%d" % i, [128, NB]) for i in range(2)]
                    lgs = lsb("lgs", [128, 8])
                    mx8 = lsb("mx8", [128, 8])
                    dd = lsb("dd", [128, 4])
                    gte = lsb("gte", [128, 2, 8])
                    gbc = lsb("gbc", [128, 8, 128])
                    ss = dict(zb=lsb("zbe", [128, 8, NB], BF16), sq=lsb("sqe", [128, 8, NB], BF16),
                              ms=lsb("mse", [128, NB]), m2=lsb("m2e", [128, NB]), rs=lsb("rse", [128, NB]))
                    sbs = [[(s, q * 1024 + b * NB, s) for b in range(4)] for s in range(SEQS) for q in range(4)]
                    if need_ctx:
                        sbs.append([(0, L, 2), (1, L, 2)])
                    wi = 0
                    hi = 0
                    for sbi, blks in enumerate(sbs):
                        nblk = len(blks)
                        for bix, (s, t0, tc) in enumerate(blks):
                            bs = slice(bix * NB, (bix + 1) * NB)
                            P.dma("sp", xe[:, :, bs], XS[s, :, :, t0:t0 + NB].rearrange("k p t -> p k t"), [("XS", s, t0)], [("xe", bix)], ("xe", bix))
                            for k in range(8):
                                P.op("act", lambda e, k=k, bs=bs, tc=tc: e.activation(out=h2[:, k, bs], in_=xe[:, k, bs], func=AF.Identity,
                                                                                      scale=modp(4, k, tc), bias=modp(3, k, tc)),
                                     [("xe", bix), "MOD"], [("h2e", bix)])
                            for tt in range(2):
                                ts_ = slice(bix * NB + tt * 128, bix * NB + tt * 128 + 128)
                                def mmr(e, ts_=ts_):
                                    for k in range(8):
                                        r = e.matmul(PS[6][:, 0:8], lhsT=h2[:, k, ts_], rhs=wrb[:, k, :], start=(k == 0), stop=(k == 7))
                                    return r
                                P.op("pe", mmr, [("h2e", bix), "wrb"], [("ps", 6)])
                                P.op("act", lambda e: e.activation(out=lgs[:], in_=PS[6][:, 0:8], func=AF.Copy), [("ps", 6)], ["lgs"])
                                P.op("dve", lambda e: e.max(out=mx8[:], in_=lgs[:]), ["lgs"], ["mx8"])
                                P.op("dve", lambda e: e.tensor_tensor(out=dd[:, 0:1], in0=mx8[:, 0:1], in1=mx8[:, 1:2], op=ALU.subtract), ["mx8"], ["dd0"])
                                P.op("act", lambda e: e.activation(out=dd[:, 1:2], in_=dd[:, 0:1], func=AF.Sigmoid), ["dd0"], ["dd1"])
                                P.op("act", lambda e: e.activation(out=dd[:, 2:3], in_=dd[:, 0:1], func=AF.Sigmoid, scale=-1.0), ["dd0"], ["dd2"])
                                P.op("dve", lambda e: e.tensor_scalar(out=gte[:, 0, :], in0=lgs[:], scalar1=mx8[:, 0:1], scalar2=dd[:, 1:2],
                                                                      op0=ALU.is_equal, op1=ALU.mult), ["lgs", "mx8", "dd1"], ["gte0"])
                                P.op("dve", lambda e: e.tensor_scalar(out=gte[:, 1, :], in0=lgs[:], scalar1=mx8[:, 1:2], scalar2=dd[:, 2:3],
                                                                      op0=ALU.is_equal, op1=ALU.mult), ["lgs", "mx8", "dd2"], ["gte1"])
                                P.op("dve", lambda e: e.tensor_tensor(out=gte[:, 0, :], in0=gte[:, 0, :], in1=gte[:, 1, :], op=ALU.add), ["gte0", "gte1"], ["gte0"])
                                P.op("dve", lambda e: e.tensor_copy(out=gbc[:], in_=gte[:, 0, :].unsqueeze(2).to_broadcast([128, 8, 128])), ["gte0"], ["gbc"])
                                for hb2 in range(2):
                                    def mmb(e, hb2=hb2):
                                        for q in range(4):
                                            r = e.matmul(PS[4 + hb2][:, q * 128:(q + 1) * 128], lhsT=gbc[:, hb2 * 4 + q, :], rhs=ident_f[:], start=True, stop=True)
                                        return r
                                    P.op("pe", mmb, ["gbc", "ident_f"], [("ps", 4 + hb2)])
                                    P.op("act", lambda e, hb2=hb2, ts_=ts_: e.activation(out=gw[:, hb2 * 4:(hb2 + 1) * 4, ts_],
                                                                                        in_=PS[4 + hb2][:, :].rearrange("p (a b) -> p a b", b=128), func=AF.Copy),
                                         [("ps", 4 + hb2)], [("gw", bix)])
                        for ex in range(NEXP):
                            for fsl in range(7):
                                j = wi % 2
                                srcs = (w["w1"][ex].rearrange("(k p) n -> p k n", p=128)[:, :, fsl * 512:(fsl + 1) * 512],
                                        w["w3"][ex].rearrange("(k p) n -> p k n", p=128)[:, :, fsl * 512:(fsl + 1) * 512],
                                        w["w2"][ex, fsl * 512:(fsl + 1) * 512, :].rearrange("(c p) n -> p c n", p=128))
                                for m3 in range(3):
                                    sti = (wi * 3 + m3) % 2
                                    shp = "p (a b) -> p a b"
                                    bdim = 512 if m3 < 2 else 1024
                                    P.dma("sp", wst[sti][:].rearrange(shp, b=bdim), srcs[m3], [], [("wste", sti)], ("wste", sti))
                                    P.op("pool", lambda e, sti=sti, j=j, m3=m3: e.tensor_copy(out=wsl[j][m3][:], in_=wst[sti][:]),
                                         [("wste", sti)], [("wsl", j, m3)])
                                w1s = wsl[j][0][:].rearrange("p (a b) -> p a b", b=512)
                                w3s = wsl[j][1][:].rearrange("p (a b) -> p a b", b=512)
                                w2s = wsl[j][2][:].rearrange("p (a b) -> p a b", b=1024)
                                for bix, (s, t0, tc) in enumerate(blks):
                                    bs = slice(bix * NB, (bix + 1) * NB)
                                    hj = hi % 2
                                    for c4 in range(4):
                                        cs = slice(c4 * 128, (c4 + 1) * 128)
                                        b1, b3 = (c4 % 2) * 2, (c4 % 2) * 2 + 1
                                        def mm13(e, cs=cs, b1=b1, b3=b3, bs=bs, w1s=w1s, w3s=w3s):
                                            for k in range(8):
                                                e.matmul(PS[b1][:, :NB], lhsT=w1s[:, k, cs], rhs=h2[:, k, bs], start=(k == 0), stop=(k == 7))
                                            for k in range(8):
                                                r = e.matmul(PS[b3][:, :NB], lhsT=w3s[:, k, cs], rhs=h2[:, k, bs], start=(k == 0), stop=(k == 7))
                                            return r
                                        P.op("pe", mm13, [("h2e", bix), ("wsl", j, 0), ("wsl", j, 1)], [("ps", b1), ("ps", b3)])
                                        P.op("act", lambda e, c4=c4, b1=b1: e.activation(out=sl[c4 % 2][:], in_=PS[b1][:, :NB], func=AF.Silu), [("ps", b1)], [("sle", c4 % 2)])
                                        P.op("dve", lambda e, c4=c4, b3=b3, ex=ex, bs=bs: e.tensor_tensor(out=tg[c4 % 2][:], in0=PS[b3][:, :NB], in1=gw[:, ex, bs], op=ALU.mult),
                                             [("ps", b3), ("gw", bix)], [("tge", c4 % 2)])
                                        P.op("dve", lambda e, c4=c4, hj=hj: e.tensor_tensor(out=hid[hj][:, c4, :], in0=tg[c4 % 2][:], in1=sl[c4 % 2][:], op=ALU.mult),
                                             [("tge", c4 % 2), ("sle", c4 % 2)], [("hide", hj, c4)])
                                    for oc in range(8):
                                        bnk = 4 + oc % 4
                                        def mm2_(e, oc=oc, bnk=bnk, hj=hj, w2s=w2s):
                                            for c4 in range(4):
                                                r = e.matmul(PS[bnk][:, :NB], lhsT=w2s[:, c4, oc * 128:(oc + 1) * 128], rhs=hid[hj][:, c4, :],
                                                             start=(c4 == 0), stop=(c4 == 3))
                                            return r
                                        P.op("pe", mm2_, [("hide", hj, c4) for c4 in range(4)] + [("wsl", j, 2)], [("ps", bnk)])
                                        P.op("dve", lambda e, oc=oc, bnk=bnk, bs=bs, tc=tc: e.scalar_tensor_tensor(
                                            out=xe[:, oc, bs], in0=PS[bnk][:, :NB], scalar=modp(5, oc, tc), in1=xe[:, oc, bs],
                                            op0=ALU.mult, op1=ALU.add), [("ps", bnk), ("xe", bix), "MOD"], [("xe", bix)])
                                    hi += 1
                                wi += 1
                        for bix, (s, t0, tc) in enumerate(blks):
                            bs = slice(bix * NB, (bix + 1) * NB)
                            ln_block(ss, xe[:, :, bs], NB, 2, 3, 1, 0, 1, XK=("xe", bix))
                            if last:
                                if tc != 2:
                                    P.dma("sp", outT[s, :, :, t0:t0 + NB].rearrange("k p t -> p k t"), xe[:, :, bs], [("xe", bix)], [("OUT", s, t0)], ("xe_st", bix))
                            else:
                                P.dma("sp", XS[s, :, :, t0:t0 + NB].rearrange("k p t -> p k t"), xe[:, :, bs], [("xe", bix)], [("XS", s, t0)], ("xe_st", bix))
                    stage_end()
        if stop_after is not None:
            dbg = dt("dbgXS", [SEQS, 8, 128, T], F32, kind="ExternalOutput").ap()
            for s_ in range(SEQS):
                P.dma("sp", dbg[s_], XS[s_], [], [("dbg", s_)], ("dbg", s_))
        P.barrier()
        P.emit(block)
    return nc


def _host_inputs(inputs, layers):
    f32 = np.float32
    x = np.asarray(inputs["x"], f32)
    ctx = np.asarray(inputs["ctx"], f32)
    c = np.asarray(inputs["c"], f32)
    c_ctx = np.asarray(inputs["c_ctx"], f32)
    PM, pm_lists, PMC = _pool_constants()
    cosT, sinT = _rope_tables()
    E, masks = _misc_constants()
    common = dict(cosT=cosT, sinT=sinT, etab=E, masks=masks, pm=PM, pmc=PMC, ident=np.eye(128, dtype=f32))
    for l in layers:
        common["ada_w%d" % l] = np.ascontiguousarray(inputs["ada_w"][l], f32)
        common["ada_bT%d" % l] = np.ascontiguousarray(np.asarray(inputs["ada_b"][l], f32).reshape(48, 128).T)
        common["w_in%d" % l] = np.ascontiguousarray(inputs["w_in"][l], f32)
        common["pool_w%d" % l] = np.ascontiguousarray(inputs["pool_w"][l], f32)
        common["pscT%d" % l] = np.ascontiguousarray(np.asarray(inputs["pool_scale"][l], f32).reshape(4, 128).T)
        common["w_pool_out%d" % l] = np.ascontiguousarray(inputs["w_pool_out"][l], f32)
        common["w_ret_out%d" % l] = np.ascontiguousarray(inputs["w_ret_out"][l], f32)
        common["logit_bc%d" % l] = np.ascontiguousarray(
            np.broadcast_to(np.asarray(inputs["ret_decay_logit"][l], f32).reshape(1, 8), (128, 8)))
        common["w_out%d" % l] = np.ascontiguousarray(inputs["w_out"][l], f32)
        lnT = np.stack([np.asarray(inputs[k][l], f32).reshape(8, 128).T
                        for k in ("ln_mix_g", "ln_mix_b", "ln_ffn_g", "ln_ffn_b")], axis=1)
        common["lnT%d" % l] = np.ascontiguousarray(lnT)
        if l % 2 == 0:
            common["ffn_w1_%d" % l] = np.ascontiguousarray(inputs["ffn_w1"][l // 2], f32)
            common["ffn_w3_%d" % l] = np.ascontiguousarray(inputs["ffn_w3"][l // 2], f32)
            common["ffn_w2_%d" % l] = np.ascontiguousarray(inputs["ffn_w2"][l // 2], f32)
        else:
            common["moe_router%d" % l] = np.ascontiguousarray(inputs["moe_router"][l // 2], f32)
            common["moe_w1_%d" % l] = np.ascontiguousarray(inputs["moe_w1"][l // 2], f32)
            common["moe_w3_%d" % l] = np.ascontiguousarray(inputs["moe_w3"][l // 2], f32)
            common["moe_w2_%d" % l] = np.ascontiguousarray(inputs["moe_w2"][l // 2], f32)
    in_maps = []
    for core in range(NCORES):
        m = dict(common)
        xs = []
        for s in range(SEQS):
            b = core * SEQS + s
            xt = np.concatenate([x[b].T, ctx[b].T], axis=1)
            xs.append(xt.reshape(8, 128, T))
        m["xT"] = np.ascontiguousarray(np.stack(xs))
        cs = np.stack([c[core * SEQS], c[core * SEQS + 1], c_ctx], axis=1)
        m["cT"] = np.ascontiguousarray(cs.reshape(8, 128, 3).transpose(1, 0, 2))
        in_maps.append(m)
    return in_maps, pm_lists, PM.shape[1]


def kernel(**inputs):
    layers = (0, 1, 2, 3)
    in_maps, pm_lists, pm_slots = _host_inputs(inputs, layers)
    nc = build(layers, None, pm_lists, pm_slots)
    res = run_bass_kernel_spmd(nc, in_maps, core_ids=list(range(NCORES)))
    out = np.empty((NCORES * SEQS, L, D), np.float32)
    for core in range(NCORES):
        o = res.results[core]["outT"]
        for s in range(SEQS):
            out[core * SEQS + s] = o[s].reshape(D, L).T
    return out
```

```python
import numpy as np
import ml_dtypes
from contextlib import ExitStack
import concourse.bass as bass
import concourse.mybir as mybir
from concourse.bass_utils import run_bass_kernel_spmd

F32 = mybir.dt.float32
BF16 = mybir.dt.bfloat16
AF = mybir.ActivationFunctionType
ALU = mybir.AluOpType

D = 1024
L = 4096
NCTX = 256
T = L + NCTX
NT = T // 128
DEPTH = 4
GRID = 64
WINS = (2, 4, 8, 16)
DFF = 2816
EFF = 3584
NEXP = 8
ALPHA = (2 * DEPTH) ** 0.25
EPS = 1e-5
NCORES = 8
SEQS = 2


class Prog:
    ENGS = ("pe", "dve", "act", "pool", "sp")

    def __init__(self, nc, es):
        self.nc = nc
        self.es = es
        self.streams = {e: [] for e in self.ENGS}
        self.cnt = {e: 0 for e in self.ENGS}
        self.sem = {e: es.enter_context(nc.semaphore("s_" + e)) for e in ("pe", "dve", "act", "pool")}
        self.dsem = {}
        self.res = {}
        self.known = {e: {} for e in self.ENGS}

    def _dma_sem(self, key):
        if key not in self.dsem:
            self.dsem[key] = [self.es.enter_context(self.nc.semaphore("d%d" % len(self.dsem))), 0]
        return self.dsem[key]

    def _need(self, eng, toks):
        need = {}
        for kind, (teng, sem, val) in toks:
            if teng == eng and eng in ("pe", "sp"):
                continue
            k = id(sem)
            if k not in need or need[k][1] < val:
                need[k] = (sem, val)
        out = []
        kn = self.known[eng]
        for k, (sem, val) in need.items():
            if kn.get(k, 0) >= val:
                continue
            kn[k] = val
            out.append((sem, val))
        return out

    def _deps(self, eng, reads, writes):
        toks = []
        for r in reads:
            st = self.res.get(r)
            if st and st["w"] is not None:
                toks.append(("raw", st["w"]))
        for w in writes:
            st = self.res.get(w)
            if st:
                if st["w"] is not None:
                    toks.append(("waw", st["w"]))
                for t in st["r"]:
                    toks.append(("war", t))
        return self._need(eng, toks)

    def _commit(self, tok, reads, writes):
        for r in reads:
            st = self.res.setdefault(r, {"w": None, "r": []})
            st["r"].append(tok)
        for w in writes:
            self.res[w] = {"w": tok, "r": []}

    def op(self, eng, fn, reads=(), writes=()):
        waits = self._deps(eng, reads, writes)
        self.cnt[eng] += 1
        sem = self.sem[eng]
        self.streams[eng].append((waits, fn, sem, 1))
        self._commit((eng, sem, self.cnt[eng]), reads, writes)

    def dma(self, q, out, in_, reads, writes, key, **kw):
        waits = self._deps(q, reads, writes)
        ds = self._dma_sem(key)
        ds[1] += 16
        sem, val = ds[0], ds[1]

        def fn(e, out=out, in_=in_, kw=kw):
            return e.dma_start(out=out, in_=in_, **kw)

        self.streams[q].append((waits, fn, sem, 16))
        self._commit(("dma", sem, val), reads, writes)

    def barrier(self):
        toks = []
        for e in ("pe", "dve", "act", "pool"):
            if self.cnt[e]:
                toks.append(("raw", (e, self.sem[e], self.cnt[e])))
        for key, (sem, val) in self.dsem.items():
            if val:
                toks.append(("raw", ("dma", sem, val)))
        for e in self.ENGS:
            waits = self._need(e, [t for t in toks if t[1][0] != e or e == "sp"])
            if waits:
                self.streams[e].append((waits, None, None, 0))
        self.res = {}

    def emit(self, block):
        engs = {"pe": block.tensor, "dve": block.vector, "act": block.scalar, "pool": block.gpsimd, "sp": block.sync}
        for name, deco in engs.items():
            stream = self.streams[name]
            if not stream:
                continue

            def body(e, stream=stream):
                for waits, fn, sem, inc in stream:
                    for ws, wv in waits:
                        e.wait_ge(ws, wv)
                    if fn is not None:
                        fn(e).then_inc(sem, inc)

            deco(body)
            self.streams[name] = []


def _box_matrix(n, w):
    left = w // 2
    right = w - 1 - left
    A = np.zeros((n, n), np.float64)
    for t in range(n):
        lo = max(t - left, 0)
        hi = min(t + right + 1, n)
        A[t, lo:hi] = 1.0 / (hi - lo)
    return A


def _pool_constants():
    sets = []
    lists = []
    mats = []
    for si, ob in enumerate((0, 3, 7)):
        lst_g = []
        for g, w in enumerate(WINS):
            A = _box_matrix(GRID, w)
            lst = []
            rows_out = np.arange(8 * ob, 8 * ob + 8)
            for tin in range(32):
                rows_in = np.arange(2 * tin, 2 * tin + 2)
                Ar = A[np.ix_(rows_out, rows_in)]
                if not np.any(Ar):
                    continue
                M = np.kron(Ar, A)
                if tin // 4 == ob:
                    o0 = (tin - 4 * ob) * 128
                    M[o0:o0 + 128, :] -= np.eye(128)
                lst.append((tin - 4 * ob, len(mats)))
                mats.append(M.T.astype(np.float32))
            lst_g.append(lst)
        lists.append(lst_g)
    out_sets = []
    out_lists = []
    for si in range(3):
        slots = []
        lg = []
        for g in range(4):
            l2 = []
            for rel, mi in lists[si][g]:
                l2.append((rel, len(slots)))
                slots.append(mats[mi])
            lg.append(l2)
        out_sets.append(np.stack(slots))
        out_lists.append(lg)
    nmax = max(s.shape[0] for s in out_sets)
    PM = np.zeros((3, nmax, 128, 512), np.float32)
    for si in range(3):
        PM[si, :out_sets[si].shape[0]] = out_sets[si]
    PMC = np.zeros((4, 2, 128, 256), np.float32)
    for g, w in enumerate(WINS):
        A = _box_matrix(NCTX, w) - np.eye(NCTX)
        for tin in range(2):
            PMC[g, tin] = A[:, tin * 128:(tin + 1) * 128].T
    return PM.astype(ml_dtypes.bfloat16), out_lists, PMC.astype(ml_dtypes.bfloat16)


def _rope_tables():
    t = np.arange(L)
    row = (t // GRID).astype(np.float32)
    col = (t % GRID).astype(np.float32)
    n_freq = 32
    inv = np.exp(-np.log(np.float32(10000.0)) * np.arange(n_freq, dtype=np.float32) / n_freq).astype(np.float32)
    ang = np.concatenate([row[:, None] * inv, col[:, None] * inv], -1).astype(np.float32)
    cos = np.ones((T, 64), np.float32)
    sin = np.zeros((T, 64), np.float32)
    cos[:L] = np.cos(ang)
    sin[:L] = np.sin(ang)
    cosT = np.ascontiguousarray(cos.reshape(NT, 128, 64).transpose(1, 0, 2))
    sinT = np.ascontiguousarray(sin.reshape(NT, 128, 64).transpose(1, 0, 2))
    return cosT, sinT


def _misc_constants():
    j = np.arange(128, dtype=np.float32)
    E = np.zeros((128, 16), np.float32)
    for h in range(4):
        E[:, 0 + h] = j - 127.0
        E[:, 4 + h] = -j
        E[:, 8 + h] = 127.0 - j
        E[:, 12 + h] = j
    jj = np.arange(128)[:, None]
    ii = np.arange(128)[None, :]
    masks = np.zeros((128, 2, 128), np.float32)
    masks[:, 0, :] = (ii >= jj)
    masks[:, 1, :] = (ii <= jj)
    return E, masks


def build(layers=(0, 1, 2, 3), stop_after=None, pm_lists=None, pm_slots=31):
    nc = bass.Bass("TRN2", target_bir_lowering=False)
    dt = nc.dram_tensor

    def inp(name, shape, dtype=F32):
        return dt(name, list(shape), dtype, kind="ExternalInput").ap()

    def scr(name, shape, dtype):
        return dt(name, list(shape), dtype).ap()

    xT_in = inp("xT", [SEQS, 8, 128, T])
    cT_in = inp("cT", [128, 8, 3])
    cos_in = inp("cosT", [128, NT, 64])
    sin_in = inp("sinT", [128, NT, 64])
    etab_in = inp("etab", [128, 16])
    mask_in = inp("masks", [128, 2, 128])
    pm_in = inp("pm", [3, pm_slots, 128, 512], BF16)
    pmc_in = inp("pmc", [4, 2, 128, 256], BF16)
    ident_in = inp("ident", [128, 128])
    W = {}
    for l in layers:
        W[l] = dict(
            ada_w=inp("ada_w%d" % l, [D, 6 * D]),
            ada_b=inp("ada_bT%d" % l, [128, 48]),
            w_in=inp("w_in%d" % l, [D, 5632]),
            pool_w=inp("pool_w%d" % l, [4, 128, 128]),
            psc=inp("pscT%d" % l, [128, 4]),
            wpo=inp("w_pool_out%d" % l, [512, D]),
            wro=inp("w_ret_out%d" % l, [D, D]),
            logit=inp("logit_bc%d" % l, [128, 8]),
            wo=inp("w_out%d" % l, [D, D]),
            lnp=inp("lnT%d" % l, [128, 4, 8]),
        )
        if l % 2 == 0:
            W[l].update(
                w1=inp("ffn_w1_%d" % l, [D, DFF]),
                w3=inp("ffn_w3_%d" % l, [D, DFF]),
                w2=inp("ffn_w2_%d" % l, [DFF, D]),
            )
        else:
            W[l].update(
                wr=inp("moe_router%d" % l, [D, NEXP]),
                w1=inp("moe_w1_%d" % l, [NEXP, D, EFF]),
                w3=inp("moe_w3_%d" % l, [NEXP, D, EFF]),
                w2=inp("moe_w2_%d" % l, [NEXP, EFF, D]),
            )
    outT = dt("outT", [SEQS, 8, 128, L], F32, kind="ExternalOutput").ap()

    XS = scr("XS", [SEQS, 8, 128, T], F32)
    U_d = scr("U_d", [SEQS, T, 512], BF16)
    V_d = scr("V_d", [SEQS, T, 1024], BF16)
    KF_d = scr("KF_d", [SEQS, T, 512], BF16)
    KB_d = scr("KB_d", [SEQS, T, 512], BF16)
    QKT_d = scr("QKT_d", [SEQS, 16, 128, T], BF16)
    G_d = [scr("G%d_d" % i, [SEQS, 8, 128, T], BF16) for i in range(3)]
    DST_d = scr("DST_d", [SEQS, 2, NT, 4, 128, 256], BF16)
    ZR_d = scr("ZR_d", [SEQS, 8, 128, T], BF16)
    WC_d = scr("WC_d", [NEXP, 7, 3, 128, 4096], BF16)

    blocks = [(i * 512, 512, False) for i in range(8)] + [(L, NCTX, True)]

    with ExitStack() as es:
        P = Prog(nc, es)
        block = es.enter_context(nc.Block())
        sb = lambda name, shape, dtype=F32: es.enter_context(nc.sbuf_tensor(name, list(shape), dtype))
        ident_f = sb("ident_f", [128, 128])
        ident_b = sb("ident_b", [128, 128], BF16)
        ones_b = sb("ones_b", [128, 128], BF16)
        ones_f = sb("ones_f", [128, 128])
        masks = sb("masks_s", [128, 2, 128])
        etab = sb("etab_s", [128, 16])
        cT = sb("cT_s", [128, 8, 3])
        sT = sb("sT_s", [128, 8, 3])
        MOD = sb("MOD", [128, 48, 3])
        lnp = sb("lnp", [128, 4, 8])
        psc = sb("psc", [128, 4])
        lgt = sb("lgt", [128, 8])
        lg16 = sb("lg16", [128, 16])
        DEC = sb("DEC", [128, 16])
        GC = sb("GC", [128, 8])
        epsb = sb("epsb", [128, 2])
        PS = [es.enter_context(nc.psum_tensor("ps%d" % i, [128, 512], F32)) for i in range(8)]

        P.dma("sp", ident_f[:], ident_in, [], ["ident_f"], "c0")
        P.dma("sp", masks[:], mask_in, [], ["masks"], "c1")
        P.dma("sp", etab[:], etab_in, [], ["etab"], "c2")
        P.dma("sp", cT[:], cT_in, [], ["cT"], "c3")
        P.op("dve", lambda e: e.tensor_copy(out=ident_b[:], in_=ident_f[:]), ["ident_f"], ["ident_b"])
        P.op("dve", lambda e: e.memset(ones_b[:], 1.0 / 1024.0), [], ["ones_b"])
        P.op("dve", lambda e: e.memset(ones_f[:], 1.0 / 256.0), [], ["ones_f"])
        P.op("dve", lambda e: e.memset(epsb[:, 0:1], EPS), [], ["epsb0"])
        P.op("dve", lambda e: e.memset(epsb[:, 1:2], EPS / (ALPHA * ALPHA)), [], ["epsb1"])
        P.op("act", lambda e: e.activation(out=sT[:], in_=cT[:], func=AF.Silu), ["cT"], ["sT"])

        def stage_end():
            P.barrier()
            P.emit(block)

        def ln_block(ss, xb, n, gk, bk, epscol, pm_i, pe_i, XK="xb"):
            zb, sq, ms, m2, rs = ss["zb"], ss["sq"], ss["ms"], ss["m2"], ss["rs"]
            P.op("act", lambda e: e.activation(out=zb[:, :, :n], in_=xb[:, :, :n], func=AF.Copy), [XK], ["zb"])
            P.op("act", lambda e: e.activation(out=sq[:, :, :n], in_=xb[:, :, :n], func=AF.Square), [XK], ["sq"])

            def mm1(e):
                for k in range(8):
                    r = e.matmul(PS[pm_i][:, :n], lhsT=ones_b[:], rhs=zb[:, k, :n], start=(k == 0), stop=(k == 7))
                return r

            def mm2(e):
                for k in range(8):
                    r = e.matmul(PS[pe_i][:, :n], lhsT=ones_b[:], rhs=sq[:, k, :n], start=(k == 0), stop=(k == 7))
                return r

            P.op("pe", mm1, ["zb", "ones_b"], [("ps", pm_i)])
            P.op("pe", mm2, ["sq", "ones_b"], [("ps", pe_i)])
            P.op("act", lambda e: e.activation(out=ms[:, :n], in_=PS[pm_i][:, :n], func=AF.Copy), [("ps", pm_i)], ["ms"])
            P.op("act", lambda e: e.activation(out=m2[:, :n], in_=PS[pm_i][:, :n], func=AF.Square), [("ps", pm_i)], ["m2"])
            P.op("dve", lambda e: e.tensor_tensor(out=rs[:, :n], in0=PS[pe_i][:, :n], in1=m2[:, :n], op=ALU.subtract),
                 [("ps", pe_i), "m2"], ["rs"])
            P.op("act", lambda e: e.activation(out=rs[:, :n], in_=rs[:, :n], func=AF.Ln, bias=epsb[:, epscol:epscol + 1]),
                 ["rs", "epsb%d" % epscol], ["rs"])
            P.op("act", lambda e: e.activation(out=rs[:, :n], in_=rs[:, :n], func=AF.Exp, scale=-0.5), ["rs"], ["rs"])
            P.op("dve", lambda e: e.tensor_tensor(out=xb[:, :, :n], in0=xb[:, :, :n],
                                                  in1=ms[:, :n].unsqueeze(1).to_broadcast([128, 8, n]), op=ALU.subtract),
                 [XK, "ms"], [XK])
            P.op("dve", lambda e: e.tensor_tensor(out=xb[:, :, :n], in0=xb[:, :, :n],
                                                  in1=rs[:, :n].unsqueeze(1).to_broadcast([128, 8, n]), op=ALU.mult),
                 [XK, "rs"], [XK])
            for k in range(8):
                P.op("act", lambda e, k=k: e.activation(out=xb[:, k, :n], in_=xb[:, k, :n], func=AF.Identity,
                                                         scale=lnp[:, gk, k:k + 1], bias=lnp[:, bk, k:k + 1]),
                     [XK, "lnp"], [XK])

        def load_cast(dst, src_ap, nk, ncols, stage, tag, piece=512):
            srcv = src_ap.rearrange("(k p) n -> p k n", p=128)
            i = 0
            for c0 in range(0, ncols, piece):
                cw = min(piece, ncols - c0)
                for k0 in range(0, nk, 8):
                    kw = min(8, nk - k0)
                    st = stage[i % 2]
                    P.dma("sp", st[:, :kw, :cw], srcv[:, k0:k0 + kw, c0:c0 + cw], [], [("wst", i % 2)], ("wst", i % 2))
                    P.op("pool", lambda e, st=st, kw=kw, cw=cw, k0=k0, c0=c0: e.tensor_copy(
                        out=dst[:, k0:k0 + kw, c0:c0 + cw], in_=st[:, :kw, :cw]), [("wst", i % 2)], [tag])
                    i += 1

        for li, l in enumerate(layers):
            w = W[l]
            xsrc = xT_in if li == 0 else XS
            need_ctx = l < DEPTH - 1
            with ExitStack() as ls:
                lsb = lambda name, shape, dtype=F32: ls.enter_context(nc.sbuf_tensor(name + "_L%d" % l, list(shape), dtype))
                adst = [lsb("adst%d" % i, [128, 8, 768]) for i in range(2)]
                adb = lsb("adb", [128, 48])
                P.dma("sp", adb[:], w["ada_b"], [], ["adb"], "p0")
                P.dma("sp", lnp[:], w["lnp"], [], ["lnp"], "p1")
                P.dma("sp", psc[:], w["psc"], [], ["psc"], "p2")
                P.dma("sp", lgt[:], w["logit"], [], ["lgt"], "p3")
                adv = w["ada_w"].rearrange("(k p) n -> p k n", p=128)
                for pc in range(8):
                    st = adst[pc % 2]
                    P.dma("sp", st[:], adv[:, :, pc * 768:(pc + 1) * 768], [], [("adst", pc % 2)], ("adst", pc % 2))

                    def mm(e, st=st, pc=pc):
                        for oc in range(6):
                            og = pc * 6 + oc
                            for k in range(8):
                                r = e.matmul(PS[0][:, og * 3:og * 3 + 3], lhsT=st[:, k, oc * 128:(oc + 1) * 128],
                                             rhs=sT[:, k, :], start=(k == 0), stop=(k == 7))
                        return r

                    P.op("pe", mm, [("adst", pc % 2), "sT"], [("ps", 0)])
                P.op("dve", lambda e: e.tensor_tensor(out=MOD[:], in0=PS[0][:, 0:144].rearrange("p (a b) -> p a b", b=3),
                                                      in1=adb[:].unsqueeze(2).to_broadcast([128, 48, 3]), op=ALU.add),
                     [("ps", 0), "adb"], ["MOD"])
                P.op("dve", lambda e: e.tensor_scalar(out=MOD[:, 8:16, :], in0=MOD[:, 8:16, :], scalar1=1.0, scalar2=None, op0=ALU.add), ["MOD"], ["MOD"])
                P.op("dve", lambda e: e.tensor_scalar(out=MOD[:, 16:24, :], in0=MOD[:, 16:24, :], scalar1=1.0 / ALPHA, scalar2=None, op0=ALU.mult), ["MOD"], ["MOD"])
                P.op("dve", lambda e: e.tensor_scalar(out=MOD[:, 32:40, :], in0=MOD[:, 32:40, :], scalar1=1.0, scalar2=None, op0=ALU.add), ["MOD"], ["MOD"])
                P.op("dve", lambda e: e.tensor_scalar(out=MOD[:, 40:48, :], in0=MOD[:, 40:48, :], scalar1=1.0 / ALPHA, scalar2=None, op0=ALU.mult), ["MOD"], ["MOD"])
                P.op("act", lambda e: e.activation(out=lgt[:], in_=lgt[:], func=AF.Exp, scale=-1.0), ["lgt"], ["lgt"])
                P.op("act", lambda e: e.activation(out=lgt[:], in_=lgt[:], func=AF.Ln, bias=1.0), ["lgt"], ["lgt"])
                P.op("dve", lambda e: e.tensor_scalar(out=lg16[:, 0:8], in0=lgt[:], scalar1=-1.0, scalar2=None, op0=ALU.mult), ["lgt"], ["lg16"])
                P.op("dve", lambda e: e.tensor_scalar(out=lg16[:, 8:16], in0=lgt[:], scalar1=-1.0, scalar2=None, op0=ALU.mult), ["lgt", "lg16"], ["lg16"])
                P.op("act", lambda e: e.activation(out=GC[:], in_=lg16[:, 0:8], func=AF.Exp, scale=128.0), ["lg16"], ["GC"])
                P.op("dve", lambda e: e.tensor_tensor(out=DEC[:], in0=lg16[:], in1=etab[:], op=ALU.mult), ["lg16", "etab"], ["DEC"])
                P.op("act", lambda e: e.activation(out=DEC[:], in_=DEC[:], func=AF.Exp), ["DEC"], ["DEC"])
                P.op("dve", lambda e: e.tensor_scalar(out=DEC[:, 8:16], in0=DEC[:, 8:16], scalar1=128.0 ** -0.5, scalar2=None, op0=ALU.mult), ["DEC"], ["DEC"])
                stage_end()
            if stop_after == ("params", l):
                break

            def modp(m, k, tc):
                return MOD[:, m * 8 + k, tc:tc + 1]

            with ExitStack() as ls:
                lsb = lambda name, shape, dtype=F32: ls.enter_context(nc.sbuf_tensor(name + "_L%d" % l, list(shape), dtype))
                WB = lsb("WB", [128, 8, 5632], BF16)
                with ExitStack() as ls2:
                    wst = [ls2.enter_context(nc.sbuf_tensor("wst%d_L%d" % (i, l), [128, 8, 512], F32)) for i in range(2)]
                    load_cast(WB, w["w_in"], 8, 5632, wst, "WB")
                    stage_end()
                cosT = lsb("cosT_s", [128, NT, 64])
                sinT = lsb("sinT_s", [128, NT, 64])
                P.dma("sp", cosT[:], cos_in, [], ["cosT"], "c4")
                P.dma("sp", sinT[:], sin_in, [], ["sinT"], "c5")
                xbs = [lsb("xa%d" % i, [128, 8, 512]) for i in range(2)]
                hb = [lsb("ha%d" % i, [128, 8, 512], BF16) for i in range(1)]
                ub = lsb("ub", [128, 512], BF16)
                vb = lsb("vb", [128, 1024], BF16)
                rt = [lsb("rt%d" % i, [128, 4, 64]) for i in range(4)]
                rot = lsb("rot", [128, 4, 2, 64])
                var_tm = lsb("var_tm", [128, 4, 512], BF16)
                qkT = lsb("qkT", [128, 16, 512], BF16)
                gb = [lsb("gb%d" % i, [128, 8, 512], BF16) for i in range(3)]
                bi = 0
                for s in range(SEQS):
                    for (t0, n, isctx) in blocks:
                        tc = 2 if isctx else s
                        xb = xbs[bi % 2]
                        h = hb[0]
                        xk, hk = ("xa", bi % 2), ("ha", 0)
                        P.dma("sp", xb[:, :, :n], xsrc[s, :, :, t0:t0 + n].rearrange("k p t -> p k t"),
                              [("XS", s, t0)], [xk], xk)
                        for k in range(8):
                            P.op("act", lambda e, k=k, xb=xb, h=h, n=n, tc=tc: e.activation(
                                out=h[:, k, :n], in_=xb[:, k, :n], func=AF.Identity,
                                scale=modp(1, k, tc), bias=modp(0, k, tc)), [xk, "MOD"], [hk])
                        for ti in range(n // 128):
                            gt = (t0 // 128) + ti
                            tsl = slice(ti * 128, (ti + 1) * 128)
                            rows = slice(t0 + ti * 128, t0 + ti * 128 + 128)
                            for bnk, c0 in enumerate((0, 512, 1024, 1536, 2048)):
                                def mm(e, bnk=bnk, c0=c0, h=h, tsl=tsl):
                                    for k in range(8):
                                        r = e.matmul(PS[bnk][:, :], lhsT=h[:, k, tsl], rhs=WB[:, k, c0:c0 + 512],
                                                     start=(k == 0), stop=(k == 7))
                                    return r
                                P.op("pe", mm, [hk, "WB"], [("ps", bnk)])
                            P.op("act", lambda e: e.activation(out=ub[:], in_=PS[0][:, :], func=AF.Copy), [("ps", 0)], ["ub"])
                            P.dma("sp", U_d[s, rows, :], ub[:], ["ub"], [("U", s, gt)], "ub")
                            P.op("act", lambda e: e.activation(out=vb[:, 0:512], in_=PS[3][:, :], func=AF.Copy), [("ps", 3)], ["vb0"])
                            P.op("dve", lambda e: e.tensor_copy(out=vb[:, 512:1024], in_=PS[4][:, :]), [("ps", 4)], ["vb1"])
                            P.dma("sp", V_d[s, rows, :], vb[:], ["vb0", "vb1"], [("V", s, gt)], "vb")
                            for qi, bnk in enumerate((1, 2)):
                                pv = PS[bnk][:, :].rearrange("p (h two d) -> p h two d", two=2, d=64)
                                Cb = cosT[:, gt, :].unsqueeze(1).to_broadcast([128, 4, 64])
                                Sb = sinT[:, gt, :].unsqueeze(1).to_broadcast([128, 4, 64])
                                P.op("dve", lambda e, pv=pv, Cb=Cb: e.tensor_tensor(out=rt[0][:], in0=pv[:, :, 0, :], in1=Cb, op=ALU.mult), [("ps", bnk), "cosT"], ["rt0"])
                                P.op("dve", lambda e, pv=pv, Sb=Sb: e.tensor_tensor(out=rt[1][:], in0=pv[:, :, 1, :], in1=Sb, op=ALU.mult), [("ps", bnk), "sinT"], ["rt1"])
                                P.op("dve", lambda e, pv=pv, Sb=Sb: e.tensor_tensor(out=rt[2][:], in0=pv[:, :, 0, :], in1=Sb, op=ALU.mult), [("ps", bnk), "sinT"], ["rt2"])
                                P.op("dve", lambda e, pv=pv, Cb=Cb: e.tensor_tensor(out=rt[3][:], in0=pv[:, :, 1, :], in1=Cb, op=ALU.mult), [("ps", bnk), "cosT"], ["rt3"])
                                P.op("dve", lambda e: e.tensor_tensor(out=rot[:, :, 0, :], in0=rt[0][:], in1=rt[1][:], op=ALU.subtract), ["rt0", "rt1"], ["rot0"])
                                P.op("dve", lambda e: e.tensor_tensor(out=rot[:, :, 1, :], in0=rt[2][:], in1=rt[3][:], op=ALU.add), ["rt2", "rt3"], ["rot1"])
                                for dr in range(2):
                                    vi = qi * 2 + dr
                                    for hh in range(4):
                                        P.op("act", lambda e, vi=vi, hh=hh, dr=dr, qi=qi: e.activation(
                                            out=var_tm[:, vi, hh * 128:(hh + 1) * 128],
                                            in_=rot[:, hh, :, :].rearrange("p a b -> p (a b)"), func=AF.Identity,
                                            scale=DEC[:, qi * 8 + dr * 4 + hh:qi * 8 + dr * 4 + hh + 1]),
                                            ["rot0", "rot1", "DEC"], [("var", vi)])
                            P.dma("sp", KF_d[s, rows, :], var_tm[:, 2, :], [("var", 2)], [("KF", s, gt)], "kf")
                            P.dma("sp", KB_d[s, rows, :], var_tm[:, 3, :], [("var", 3)], [("KB", s, gt)], "kb")
                            p5 = PS[5][:, :].bitcast(BF16).rearrange("p (a b) -> p a b", b=128)[:, 0:8, :]
                            for half in range(2):
                                def tr(e, half=half):
                                    for j in range(8):
                                        vi = half * 2 + j // 4
                                        hh = j % 4
                                        r = e.transpose(p5[:, j, :], var_tm[:, vi, hh * 128:(hh + 1) * 128], ident_b[:])
                                    return r
                                P.op("pe", tr, [("var", half * 2), ("var", half * 2 + 1), "ident_b"], [("ps", 5)])
                                P.op("dve", lambda e, half=half, tsl=tsl: e.tensor_copy(out=qkT[:, half * 8:(half + 1) * 8, tsl], in_=p5),
                                     [("ps", 5)], [("qkT", half)])
                        P.dma("sp", QKT_d[s, :, :, t0:t0 + n].rearrange("a p t -> p a t"), qkT[:, :, :n],
                              [("qkT", 0), ("qkT", 1)], [("QKT", s, t0)], "qkT")
                        for gi in range(3):
                            func = AF.Silu if gi == 0 else AF.Sigmoid
                            for oc in range(8):
                                bnk = 6 + (oc % 2)
                                c0 = 2560 + gi * 1024 + oc * 128
                                def mm(e, bnk=bnk, c0=c0, h=h, n=n):
                                    for k in range(8):
                                        r = e.matmul(PS[bnk][:, :n], lhsT=WB[:, k, c0:c0 + 128], rhs=h[:, k, :n],
                                                     start=(k == 0), stop=(k == 7))
                                    return r
                                P.op("pe", mm, [hk, "WB"], [("ps", bnk)])
                                P.op("act", lambda e, bnk=bnk, gi=gi, oc=oc, n=n, func=func: e.activation(
                                    out=gb[gi][:, oc, :n], in_=PS[bnk][:, :n], func=func), [("ps", bnk)], [("gb", gi, oc)])
                            P.dma("sp", G_d[gi][s, :, :, t0:t0 + n].rearrange("k p t -> p k t"), gb[gi][:, :, :n],
                                  [("gb", gi, oc) for oc in range(8)], [("G", gi, s, t0)], ("gb", gi))
                        bi += 1
                stage_end()
            if stop_after == ("A", l):
                break

            with ExitStack() as ls:
                lsb = lambda name, shape, dtype=F32: ls.enter_context(nc.sbuf_tensor(name + "_L%d" % l, list(shape), dtype))
                Sst = lsb("Sst", [128, 1024])
                Df = lsb("Df", [128, 1024])
                Dbf = [lsb("Dbf%d" % i, [128, 1024], BF16) for i in range(2)]
                kt = [lsb("kt%d" % i, [128, 512], BF16) for i in range(2)]
                vt = [lsb("vt%d" % i, [128, 1024], BF16) for i in range(2)]
                it = 0
                for s in range(SEQS):
                    for dr in range(2):
                        order = [32, 33] + list(range(32)) if dr == 0 else [33, 32] + list(range(31, -1, -1))
                        Ksrc = KF_d if dr == 0 else KB_d
                        P.op("dve", lambda e: e.memset(Sst[:], 0.0), [], ["S"])
                        for c in order:
                            j = it % 2
                            rows = slice(c * 128, (c + 1) * 128)
                            P.dma("sp", kt[j][:], Ksrc[s, rows, :], [("KF", s), ("KB", s)], [("kt", j)], ("kt", j))
                            P.dma("sp", vt[j][:], V_d[s, rows, :], [("V", s)], [("vt", j)], ("vt", j))
                            for hh in range(4):
                                P.op("dve", lambda e, hh=hh, dr=dr: e.tensor_scalar(
                                    out=Df[:, hh * 256:(hh + 1) * 256], in0=Sst[:, hh * 256:(hh + 1) * 256],
                                    scalar1=GC[:, dr * 4 + hh:dr * 4 + hh + 1], scalar2=None, op0=ALU.mult), ["S", "GC"], [("Df", hh)])
                            P.op("act", lambda e, j=j: e.activation(out=Dbf[j][:], in_=Df[:], func=AF.Copy),
                                 [("Df", hh) for hh in range(4)], [("Dbf", j)])
                            P.dma("sp", DST_d[s, dr, c].rearrange("h p v -> p h v"),
                                  Dbf[j][:].rearrange("p (h v) -> p h v", v=256), [("Dbf", j)], [("DST", s, dr, c)], ("Dbf", j))

                            def mm(e, j=j):
                                for hh in range(4):
                                    r = e.matmul(PS[hh // 2][:, (hh % 2) * 256:(hh % 2) * 256 + 256],
                                                 lhsT=kt[j][:, hh * 128:(hh + 1) * 128], rhs=vt[j][:, hh * 256:(hh + 1) * 256],
                                                 start=True, stop=True)
                                return r
                            P.op("pe", mm, [("kt", j), ("vt", j)], [("ps", 0), ("ps", 1)])
                            for b2 in range(2):
                                P.op("dve", lambda e, b2=b2: e.tensor_tensor(
                                    out=Sst[:, b2 * 512:(b2 + 1) * 512], in0=PS[b2][:, :], in1=Df[:, b2 * 512:(b2 + 1) * 512], op=ALU.add),
                                    [("ps", b2), ("Df", 2 * b2), ("Df", 2 * b2 + 1)], ["S"])
                            it += 1
                stage_end()

            with ExitStack() as ls:
                lsb = lambda name, shape, dtype=F32: ls.enter_context(nc.sbuf_tensor(name + "_L%d" % l, list(shape), dtype))
                qk = [lsb("qk%d" % i, [128, 16, 128], BF16) for i in range(2)]
                vt = [lsb("vc%d" % i, [128, 1024], BF16) for i in range(2)]
                Dt = [lsb("Dt%d" % i, [128, 2, 4, 256], BF16) for i in range(2)]
                sg = [lsb("sg%d" % i, [128, 8, 128], BF16) for i in range(2)]
                PT = lsb("PT", [128, 8, 128], BF16)
                of = lsb("of", [128, 8, 128])
                osq = lsb("osq", [128, 8, 128])
                msr = lsb("msr", [128, 4, 128])
                m2r = lsb("m2r", [128, 4, 128])
                rsr = lsb("rsr", [128, 4, 128])
                zrt = [lsb("zrt%d" % i, [128, 8, 128], BF16) for i in range(2)]
                it = 0
                ntl = NT if need_ctx else 32
                for s in range(SEQS):
                    for c in range(ntl):
                        j = it % 2
                        cs = slice(c * 128, (c + 1) * 128)
                        P.dma("sp", qk[j][:], QKT_d[s, :, :, cs].rearrange("a p t -> p a t"), [("QKT", s)], [("qk", j)], ("qk", j))
                        P.dma("sp", vt[j][:], V_d[s, cs, :], [("V", s)], [("vc", j)], ("vc", j))
                        for dr in range(2):
                            P.dma("sp", Dt[j][:, dr], DST_d[s, dr, c].rearrange("h p v -> p h v"), [("DST", s)], [("Dt", j)], ("Dt", j))
                        P.dma("sp", sg[j][:], G_d[0][s, :, :, cs].rearrange("k p t -> p k t"), [("G", 0, s)], [("sg", j)], ("sg", j))
                        for dr in range(2):
                            def mm(e, dr=dr, j=j):
                                for hh in range(4):
                                    r = e.matmul(PS[dr][:, hh * 128:(hh + 1) * 128], lhsT=qk[j][:, (2 + dr) * 4 + hh, :],
                                                 rhs=qk[j][:, dr * 4 + hh, :], start=True, stop=True)
                                return r
                            P.op("pe", mm, [("qk", j)], [("ps", dr)])
                            P.op("dve", lambda e, dr=dr: e.tensor_tensor(
                                out=PT[:, dr * 4:(dr + 1) * 4, :], in0=PS[dr][:, :].rearrange("p (h i) -> p h i", i=128),
                                in1=masks[:, dr, :].unsqueeze(1).to_broadcast([128, 4, 128]), op=ALU.mult),
                                [("ps", dr), "masks"], [("PT", dr)])
                        for b2 in range(2):
                            def mm(e, b2=b2, j=j):
                                for q in range(4):
                                    ch = b2 * 4 + q
                                    hh, m = ch // 2, ch % 2
                                    o = PS[2 + b2][:, q * 128:(q + 1) * 128]
                                    for dr in range(2):
                                        e.matmul(o, lhsT=vt[j][:, hh * 256 + m * 128:hh * 256 + m * 128 + 128],
                                                 rhs=PT[:, dr * 4 + hh, :], start=(dr == 0), stop=False)
                                        r = e.matmul(o, lhsT=Dt[j][:, dr, hh, m * 128:(m + 1) * 128],
                                                     rhs=qk[j][:, dr * 4 + hh, :], start=False, stop=(dr == 1))
                                return r
                            P.op("pe", mm, [("vc", j), ("PT", 0), ("PT", 1), ("Dt", j), ("qk", j)], [("ps", 2 + b2)])
                            P.op("act", lambda e, b2=b2: e.activation(out=of[:, b2 * 4:(b2 + 1) * 4, :].rearrange("p a b -> p (a b)"),
                                                                     in_=PS[2 + b2][:, :], func=AF.Copy), [("ps", 2 + b2)], [("of", b2)])
                            P.op("act", lambda e, b2=b2: e.activation(out=osq[:, b2 * 4:(b2 + 1) * 4, :].rearrange("p a b -> p (a b)"),
                                                                     in_=PS[2 + b2][:, :], func=AF.Square), [("ps", 2 + b2)], [("osq", b2)])
                        def mmst(e):
                            for hh in range(4):
                                for m in range(2):
                                    e.matmul(PS[4][:, hh * 128:(hh + 1) * 128], lhsT=ones_f[:], rhs=of[:, hh * 2 + m, :],
                                             start=(m == 0), stop=(m == 1))
                            for hh in range(4):
                                for m in range(2):
                                    r = e.matmul(PS[5][:, hh * 128:(hh + 1) * 128], lhsT=ones_f[:], rhs=osq[:, hh * 2 + m, :],
                                                 start=(m == 0), stop=(m == 1))
                            return r
                        P.op("pe", mmst, [("of", 0), ("of", 1), ("osq", 0), ("osq", 1), "ones_f"], [("ps", 4), ("ps", 5)])
                        fl = lambda t: t[:].rearrange("p a b -> p (a b)")
                        P.op("act", lambda e: e.activation(out=fl(msr), in_=PS[4][:, :], func=AF.Copy), [("ps", 4)], ["msr"])
                        P.op("act", lambda e: e.activation(out=fl(m2r), in_=PS[4][:, :], func=AF.Square), [("ps", 4)], ["m2r"])
                        P.op("dve", lambda e: e.tensor_tensor(out=fl(rsr), in0=PS[5][:, :], in1=fl(m2r), op=ALU.subtract), [("ps", 5), "m2r"], ["rsr"])
                        P.op("act", lambda e: e.activation(out=fl(rsr), in_=fl(rsr), func=AF.Ln, bias=epsb[:, 0:1]), ["rsr", "epsb0"], ["rsr"])
                        P.op("act", lambda e: e.activation(out=fl(rsr), in_=fl(rsr), func=AF.Exp, scale=-0.5), ["rsr"], ["rsr"])
                        ov = of[:].rearrange("p (h m) t -> p h m t", m=2)
                        P.op("dve", lambda e, ov=ov: e.tensor_tensor(out=ov, in0=ov, in1=msr[:].unsqueeze(2).to_broadcast([128, 4, 2, 128]), op=ALU.subtract),
                             [("of", 0), ("of", 1), "msr"], [("of", 0), ("of", 1)])
                        P.op("dve", lambda e, ov=ov: e.tensor_tensor(out=ov, in0=ov, in1=rsr[:].unsqueeze(2).to_broadcast([128, 4, 2, 128]), op=ALU.mult),
                             [("of", 0), ("of", 1), "rsr"], [("of", 0), ("of", 1)])
                        P.op("dve", lambda e, j=j: e.tensor_tensor(out=zrt[j][:], in0=of[:], in1=sg[j][:], op=ALU.mult),
                             [("of", 0), ("of", 1), ("sg", j)], [("zrt", j)])
                        P.dma("sp", ZR_d[s, :, :, cs].rearrange("k p t -> p k t"), zrt[j][:], [("zrt", j)], [("ZR", s, c)], ("zrt", j))
                        it += 1
                stage_end()
            if stop_after == ("C1", l):
                break

            with ExitStack() as ls:
                lsb = lambda name, shape, dtype=F32: ls.enter_context(nc.sbuf_tensor(name + "_L%d" % l, list(shape), dtype))
                wro = lsb("wro", [128, 8, 1024], BF16)
                wo = lsb("wo", [128, 8, 1024], BF16)
                wpo = lsb("wpo", [128, 4, 1024], BF16)
                plw = lsb("plw", [128, 4, 128], BF16)
                with ExitStack() as ls2:
                    wst = [ls2.enter_context(nc.sbuf_tensor("wstc%d_L%d" % (i, l), [128, 8, 512], F32)) for i in range(2)]
                    load_cast(wro, w["wro"], 8, 1024, wst, "wro")
                    load_cast(wo, w["wo"], 8, 1024, wst, "wo")
                    load_cast(wpo, w["wpo"], 4, 1024, wst, "wpo")
                    P.dma("sp", wst[0][:, 0:4, 0:128], w["pool_w"].rearrange("g c d -> c g d"), [], [("wst", 0)], ("wst", 0))
                    P.op("pool", lambda e: e.tensor_copy(out=plw[:], in_=wst[0][:, 0:4, 0:128]), [("wst", 0)], ["plw"])
                    stage_end()
                Ures = lsb("Ures", [128, NT, 512], BF16)
                PMb = lsb("PMb", [128, pm_slots, 512], BF16)
                PMc = lsb("PMc", [128, 8, 256], BF16)
                P.dma("sp", PMc[:], pmc_in.rearrange("g t p o -> p (g t) o"), [], ["PMc"], "pmc")
                zr = lsb("zr", [128, 8, 512], BF16)
                spb = lsb("spb", [128, 8, 512], BF16)
                srb = lsb("srb", [128, 8, 512], BF16)
                dTb = lsb("dTb", [128, 4, 512], BF16)
                ygb = lsb("ygb", [128, 4, 512], BF16)
                mixb = lsb("mixb", [128, 8, 512], BF16)
                t1 = lsb("t1", [128, 512])
                t2 = lsb("t2", [128, 512])
                xb = lsb("xc", [128, 8, 512])
                ss = dict(zb=lsb("zb", [128, 8, 512], BF16), sq=lsb("sq", [128, 8, 512], BF16),
                          ms=lsb("ms", [128, 512]), m2=lsb("m2", [128, 512]), rs=lsb("rs", [128, 512]))
                for s in range(SEQS):
                    P.dma("sp", Ures[:], U_d[s].rearrange("(t p) c -> p t c", p=128), [("U", s)], ["Ures"], "Ures")
                    for ob, (t0, n, isctx) in enumerate(blocks):
                        if isctx and not need_ctx:
                            continue
                        tc = 2 if isctx else s
                        bsl = (slice(None), slice(None), slice(t0, t0 + n))
                        P.dma("sp", zr[:, :, :n], ZR_d[s][bsl].rearrange("k p t -> p k t"), [("ZR", s)], ["zr"], "zr")
                        P.dma("sp", spb[:, :, :n], G_d[1][s][bsl].rearrange("k p t -> p k t"), [("G", 1, s)], ["spb"], "spb")
                        P.dma("sp", srb[:, :, :n], G_d[2][s][bsl].rearrange("k p t -> p k t"), [("G", 2, s)], ["srb"], "srb")
                        P.dma("sp", xb[:, :, :n], xsrc[s][bsl].rearrange("k p t -> p k t"), [("XS", s, t0)], ["xb"], "xb")
                        if not isctx:
                            si = 0 if ob == 0 else (2 if ob == 7 else 1)
                            if ob in (0, 1, 7):
                                P.dma("sp", PMb[:], pm_in[si].rearrange("a p o -> p a o"), [], ["PMb"], "PMb")
                        for g in range(4):
                            bnk = g % 2
                            if isctx:
                                lst = [(32 + tt, PMc[:, g * 2 + tt, :]) for tt in range(2)]
                            else:
                                lst = [(4 * ob + rel, PMb[:, slot, :]) for rel, slot in pm_lists[si][g]]
                            def mm(e, lst=lst, g=g, bnk=bnk, n=n):
                                for i2, (tin, pmv) in enumerate(lst):
                                    r = e.matmul(PS[bnk][:, :n], lhsT=Ures[:, tin, g * 128:(g + 1) * 128], rhs=pmv[:, :n],
                                                 start=(i2 == 0), stop=(i2 == len(lst) - 1))
                                return r
                            P.op("pe", mm, ["Ures", "PMb", "PMc"], [("ps", bnk)])
                            P.op("act", lambda e, g=g, bnk=bnk, n=n: e.activation(out=dTb[:, g, :n], in_=PS[bnk][:, :n], func=AF.Copy),
                                 [("ps", bnk)], [("dTb", g)])
                            P.op("pe", lambda e, g=g, bnk=bnk, n=n: e.matmul(PS[2 + bnk][:, :n], lhsT=plw[:, g, :], rhs=dTb[:, g, :n], start=True, stop=True),
                                 [("dTb", g), "plw"], [("ps", 2 + bnk)])
                            P.op("act", lambda e, g=g, bnk=bnk, n=n: e.activation(out=ygb[:, g, :n], in_=PS[2 + bnk][:, :n], func=AF.Identity,
                                                                                   scale=psc[:, g:g + 1]), [("ps", 2 + bnk), "psc"], [("ygb", g)])
                        for oc in range(8):
                            ocs = slice(oc * 128, (oc + 1) * 128)
                            def mmp(e, ocs=ocs, n=n):
                                for g in range(4):
                                    r = e.matmul(PS[4][:, :n], lhsT=wpo[:, g, ocs], rhs=ygb[:, g, :n], start=(g == 0), stop=(g == 3))
                                return r
                            def mmr(e, ocs=ocs, n=n):
                                for k in range(8):
                                    r = e.matmul(PS[5][:, :n], lhsT=wro[:, k, ocs], rhs=zr[:, k, :n], start=(k == 0), stop=(k == 7))
                                return r
                            P.op("pe", mmp, [("ygb", g) for g in range(4)] + ["wpo"], [("ps", 4)])
                            P.op("pe", mmr, ["zr", "wro"], [("ps", 5)])
                            P.op("dve", lambda e, oc=oc, n=n: e.tensor_tensor(out=t1[:, :n], in0=PS[4][:, :n], in1=spb[:, oc, :n], op=ALU.mult),
                                 [("ps", 4), "spb"], ["t1"])
                            P.op("dve", lambda e, oc=oc, n=n: e.tensor_tensor(out=t2[:, :n], in0=PS[5][:, :n], in1=srb[:, oc, :n], op=ALU.mult),
                                 [("ps", 5), "srb"], ["t2"])
                            P.op("dve", lambda e, oc=oc, n=n: e.tensor_tensor(out=mixb[:, oc, :n], in0=t1[:, :n], in1=t2[:, :n], op=ALU.add),
                                 ["t1", "t2"], [("mixb", oc)])
                        for oc2 in range(8):
                            bnk = 6 + oc2 % 2
                            def mmo(e, oc2=oc2, bnk=bnk, n=n):
                                for oc in range(8):
                                    r = e.matmul(PS[bnk][:, :n], lhsT=wo[:, oc, oc2 * 128:(oc2 + 1) * 128], rhs=mixb[:, oc, :n],
                                                 start=(oc == 0), stop=(oc == 7))
                                return r
                            P.op("pe", mmo, [("mixb", oc) for oc in range(8)] + ["wo"], [("ps", bnk)])
                            P.op("dve", lambda e, oc2=oc2, bnk=bnk, n=n, tc=tc: e.scalar_tensor_tensor(
                                out=xb[:, oc2, :n], in0=PS[bnk][:, :n], scalar=modp(2, oc2, tc), in1=xb[:, oc2, :n],
                                op0=ALU.mult, op1=ALU.add), [("ps", bnk), "xb", "MOD"], ["xb"])
                        ln_block(ss, xb, n, 0, 1, 1, 0, 1)
                        P.dma("sp", XS[s][bsl].rearrange("k p t -> p k t"), xb[:, :, :n], ["xb"], [("XS", s, t0)], "xb_st")
                stage_end()
            xsrc = XS
            if stop_after == ("C2", l):
                break

            last = (li == len(layers) - 1)
            if l % 2 == 0:
                with ExitStack() as ls:
                    lsb = lambda name, shape, dtype=F32: ls.enter_context(nc.sbuf_tensor(name + "_L%d" % l, list(shape), dtype))
                    w1 = lsb("w1", [128, 8, DFF], BF16)
                    w3 = lsb("w3", [128, 8, DFF], BF16)
                    w2 = lsb("w2", [128, 22, D], BF16)
                    with ExitStack() as ls2:
                        wst = [ls2.enter_context(nc.sbuf_tensor("wstd%d_L%d" % (i, l), [128, 8, 512], F32)) for i in range(2)]
                        load_cast(w1, w["w1"], 8, DFF, wst, "w1")
                        load_cast(w3, w["w3"], 8, DFF, wst, "w3")
                        load_cast(w2, w["w2"], 22, D, wst, "w2")
                        stage_end()
                    NB = 256
                    xds = [lsb("xd%d" % i, [128, 8, NB]) for i in range(2)]
                    h2 = lsb("h2", [128, 8, NB], BF16)
                    hid = lsb("hid", [128, 22, NB], BF16)
                    sl = [lsb("sl%d" % i, [128, NB]) for i in range(2)]
                    ss = dict(zb=lsb("zbd", [128, 8, NB], BF16), sq=lsb("sqd", [128, 8, NB], BF16),
                              ms=lsb("msd", [128, NB]), m2=lsb("m2d", [128, NB]), rs=lsb("rsd", [128, NB]))
                    bi = 0
                    for s in range(SEQS):
                        ntok = T if need_ctx else L
                        for t0 in range(0, ntok, NB):
                            isctx = t0 >= L
                            tc = 2 if isctx else s
                            xb = xds[bi % 2]
                            xk = ("xd", bi % 2)
                            bsl = (slice(None), slice(None), slice(t0, t0 + NB))
                            P.dma("sp", xb[:], XS[s][bsl].rearrange("k p t -> p k t"), [("XS", s, t0)], [xk], xk)
                            for k in range(8):
                                P.op("act", lambda e, k=k, xb=xb, tc=tc: e.activation(out=h2[:, k, :], in_=xb[:, k, :], func=AF.Identity,
                                                                                      scale=modp(4, k, tc), bias=modp(3, k, tc)), [xk, "MOD"], ["h2"])
                            for ff in range(22):
                                fs = slice(ff * 128, (ff + 1) * 128)
                                b1, b3 = (ff % 2) * 2, (ff % 2) * 2 + 1
                                def mm13(e, fs=fs, b1=b1, b3=b3):
                                    for k in range(8):
                                        e.matmul(PS[b1][:, :NB], lhsT=w1[:, k, fs], rhs=h2[:, k, :], start=(k == 0), stop=(k == 7))
                                    for k in range(8):
                                        r = e.matmul(PS[b3][:, :NB], lhsT=w3[:, k, fs], rhs=h2[:, k, :], start=(k == 0), stop=(k == 7))
                                    return r
                                P.op("pe", mm13, ["h2", "w1", "w3"], [("ps", b1), ("ps", b3)])
                                P.op("act", lambda e, ff=ff, b1=b1: e.activation(out=sl[ff % 2][:], in_=PS[b1][:, :NB], func=AF.Silu), [("ps", b1)], [("sl", ff % 2)])
                                P.op("dve", lambda e, ff=ff, b3=b3: e.tensor_tensor(out=hid[:, ff, :], in0=PS[b3][:, :NB], in1=sl[ff % 2][:], op=ALU.mult),
                                     [("ps", b3), ("sl", ff % 2)], [("hid", ff)])
                            for oc in range(8):
                                bnk = 4 + oc % 4
                                def mm2_(e, oc=oc, bnk=bnk):
                                    for ff in range(22):
                                        r = e.matmul(PS[bnk][:, :NB], lhsT=w2[:, ff, oc * 128:(oc + 1) * 128], rhs=hid[:, ff, :],
                                                     start=(ff == 0), stop=(ff == 21))
                                    return r
                                P.op("pe", mm2_, [("hid", ff) for ff in range(22)] + ["w2"], [("ps", bnk)])
                                P.op("dve", lambda e, oc=oc, bnk=bnk, xb=xb, tc=tc: e.scalar_tensor_tensor(
                                    out=xb[:, oc, :], in0=PS[bnk][:, :NB], scalar=modp(5, oc, tc), in1=xb[:, oc, :],
                                    op0=ALU.mult, op1=ALU.add), [("ps", bnk), xk, "MOD"], [xk])
                            ln_block(ss, xb, NB, 2, 3, 1, 0, 1, XK=xk)
                            if last:
                                if not isctx:
                                    P.dma("sp", outT[s][bsl].rearrange("k p t -> p k t"), xb[:], [xk], [("OUT",)], ("xd_st", bi % 2))
                            else:
                                P.dma("sp", XS[s][bsl].rearrange("k p t -> p k t"), xb[:], [xk], [("XS", s, t0)], ("xd_st", bi % 2))
                            bi += 1
                    stage_end()
            else:
                with ExitStack() as ls:
                    lsb = lambda name, shape, dtype=F32: ls.enter_context(nc.sbuf_tensor(name + "_L%d" % l, list(shape), dtype))
                    NB = 512
                    wrb = lsb("wrb", [128, 8, NEXP], BF16)
                    wrf = lsb("wrf", [128, 8, NEXP])
                    P.dma("sp", wrf[:], w["wr"].rearrange("(k p) e -> p k e", p=128), [], ["wrf"], "wrf")
                    P.op("dve", lambda e: e.tensor_copy(out=wrb[:], in_=wrf[:]), ["wrf"], ["wrb"])
                    wst = [lsb("wste%d" % i, [128, 4096]) for i in range(2)]
                    wsl = [[lsb("wsl%d_%d" % (i, j), [128, 4096], BF16) for j in range(3)] for i in range(2)]
                    xe = lsb("xe", [128, 8, 1024])
                    h2 = lsb("h2e", [128, 8, 1024], BF16)
                    gw = lsb("gw", [128, NEXP, 1024])
                    hid = [lsb("hide%d" % i, [128, 4, NB], BF16) for i in range(2)]
                    sl = [lsb("sle%d" % i, [128, NB]) for i in range(2)]
                    tg = [lsb("tge%d" % i, [128, NB]) for i in range(2)]
                    lgs = lsb("lgs", [128, 8])
                    mx8 = lsb("mx8", [128, 8])
                    dd = lsb("dd", [128, 4])
                    gte = lsb("gte", [128, 2, 8])
                    gbc = lsb("gbc", [128, 8, 128])
                    ss = dict(zb=lsb("zbe", [128, 8, 256], BF16), sq=lsb("sqe", [128, 8, 256], BF16),
                              ms=lsb("mse", [128, 256]), m2=lsb("m2e", [128, 256]), rs=lsb("rse", [128, 256]))
                    sbs = [[(s, q * 1024 + b * NB, NB, s) for b in range(2)] for s in range(SEQS) for q in range(4)]
                    if need_ctx:
                        sbs.append([(0, L, NCTX, 2), (1, L, NCTX, 2)])
                    jobs = [(sbi, ex, fsl) for sbi in range(len(sbs)) for ex in range(NEXP) for fsl in range(7)]

                    def load_job(ji):
                        sbi, ex, fsl = jobs[ji]
                        j = ji % 2
                        if sbi == 0:
                            srcs = (w["w1"][ex].rearrange("(k p) n -> p k n", p=128)[:, :, fsl * 512:(fsl + 1) * 512],
                                    w["w3"][ex].rearrange("(k p) n -> p k n", p=128)[:, :, fsl * 512:(fsl + 1) * 512],
                                    w["w2"][ex, fsl * 512:(fsl + 1) * 512, :].rearrange("(c p) n -> p c n", p=128))
                            for m3 in range(3):
                                sti = (ji * 3 + m3) % 2
                                bdim = 512 if m3 < 2 else 1024
                                P.dma("sp", wst[sti][:].rearrange("p (a b) -> p a b", b=bdim), srcs[m3], [], [("wste", sti)], ("wste", sti))
                                P.op("pool", lambda e, sti=sti, j=j, m3=m3: e.tensor_copy(out=wsl[j][m3][:], in_=wst[sti][:]),
                                     [("wste", sti)], [("wsl", j, m3)])
                                P.dma("sp", WC_d[ex, fsl, m3], wsl[j][m3][:], [("wsl", j, m3)], [("WC", ex, fsl, m3)], ("wc_st", j, m3))
                        else:
                            for m3 in range(3):
                                P.dma("sp", wsl[j][m3][:], WC_d[ex, fsl, m3], [("WC", ex, fsl, m3)], [("wsl", j, m3)], ("wsl", j, m3))

                    hi = 0
                    load_job(0)
                    for ji, (sbi, ex, fsl) in enumerate(jobs):
                        blks = sbs[sbi]
                        if ex == 0 and fsl == 0:
                            off = 0
                            for bix, (s, t0, n, tc) in enumerate(blks):
                                bs = slice(off, off + n)
                                P.dma("sp", xe[:, :, bs], XS[s, :, :, t0:t0 + n].rearrange("k p t -> p k t"), [("XS", s, t0)], [("xe", bix)], ("xe", bix))
                                for k in range(8):
                                    P.op("act", lambda e, k=k, bs=bs, tc=tc: e.activation(out=h2[:, k, bs], in_=xe[:, k, bs], func=AF.Identity,
                                                                                          scale=modp(4, k, tc), bias=modp(3, k, tc)),
                                         [("xe", bix), "MOD"], [("h2e", bix)])
                                for tt in range(n // 128):
                                    ts_ = slice(off + tt * 128, off + tt * 128 + 128)
                                    def mmr(e, ts_=ts_):
                                        for k in range(8):
                                            r = e.matmul(PS[6][:, 0:8], lhsT=h2[:, k, ts_], rhs=wrb[:, k, :], start=(k == 0), stop=(k == 7))
                                        return r
                                    P.op("pe", mmr, [("h2e", bix), "wrb"], [("ps", 6)])
                                    P.op("act", lambda e: e.activation(out=lgs[:], in_=PS[6][:, 0:8], func=AF.Copy), [("ps", 6)], ["lgs"])
                                    P.op("dve", lambda e: e.max(out=mx8[:], in_=lgs[:]), ["lgs"], ["mx8"])
                                    P.op("dve", lambda e: e.tensor_tensor(out=dd[:, 0:1], in0=mx8[:, 0:1], in1=mx8[:, 1:2], op=ALU.subtract), ["mx8"], ["dd0"])
                                    P.op("act", lambda e: e.activation(out=dd[:, 1:2], in_=dd[:, 0:1], func=AF.Sigmoid), ["dd0"], ["dd1"])
                                    P.op("act", lambda e: e.activation(out=dd[:, 2:3], in_=dd[:, 0:1], func=AF.Sigmoid, scale=-1.0), ["dd0"], ["dd2"])
                                    P.op("dve", lambda e: e.tensor_scalar(out=gte[:, 0, :], in0=lgs[:], scalar1=mx8[:, 0:1], scalar2=dd[:, 1:2],
                                                                          op0=ALU.is_equal, op1=ALU.mult), ["lgs", "mx8", "dd1"], ["gte0"])
                                    P.op("dve", lambda e: e.tensor_scalar(out=gte[:, 1, :], in0=lgs[:], scalar1=mx8[:, 1:2], scalar2=dd[:, 2:3],
                                                                          op0=ALU.is_equal, op1=ALU.mult), ["lgs", "mx8", "dd2"], ["gte1"])
                                    P.op("dve", lambda e: e.tensor_tensor(out=gte[:, 0, :], in0=gte[:, 0, :], in1=gte[:, 1, :], op=ALU.add), ["gte0", "gte1"], ["gte0"])
                                    P.op("dve", lambda e: e.tensor_copy(out=gbc[:], in_=gte[:, 0, :].unsqueeze(2).to_broadcast([128, 8, 128])), ["gte0"], ["gbc"])
                                    for hb2 in range(2):
                                        def mmb(e, hb2=hb2):
                                            for q in range(4):
                                                r = e.matmul(PS[4 + hb2][:, q * 128:(q + 1) * 128], lhsT=gbc[:, hb2 * 4 + q, :], rhs=ident_f[:], start=True, stop=True)
                                            return r
                                        P.op("pe", mmb, ["gbc", "ident_f"], [("ps", 4 + hb2)])
                                        P.op("act", lambda e, hb2=hb2, ts_=ts_: e.activation(out=gw[:, hb2 * 4:(hb2 + 1) * 4, ts_],
                                                                                            in_=PS[4 + hb2][:, :].rearrange("p (a b) -> p a b", b=128), func=AF.Copy),
                                             [("ps", 4 + hb2)], [("gw", bix)])
                                off += n
                        if ji + 1 < len(jobs):
                            load_job(ji + 1)
                        j = ji % 2
                        w1s = wsl[j][0][:].rearrange("p (a b) -> p a b", b=512)
                        w3s = wsl[j][1][:].rearrange("p (a b) -> p a b", b=512)
                        w2s = wsl[j][2][:].rearrange("p (a b) -> p a b", b=1024)
                        off = 0
                        for bix, (s, t0, n, tc) in enumerate(blks):
                            bs = slice(off, off + n)
                            hj = hi % 2
                            for c4 in range(4):
                                cs = slice(c4 * 128, (c4 + 1) * 128)
                                b1, b3 = (c4 % 2) * 2, (c4 % 2) * 2 + 1
                                def mm13(e, cs=cs, b1=b1, b3=b3, bs=bs, w1s=w1s, w3s=w3s, n=n):
                                    for k in range(8):
                                        e.matmul(PS[b1][:, :n], lhsT=w1s[:, k, cs], rhs=h2[:, k, bs], start=(k == 0), stop=(k == 7))
                                    for k in range(8):
                                        r = e.matmul(PS[b3][:, :n], lhsT=w3s[:, k, cs], rhs=h2[:, k, bs], start=(k == 0), stop=(k == 7))
                                    return r
                                P.op("pe", mm13, [("h2e", bix), ("wsl", j, 0), ("wsl", j, 1)], [("ps", b1), ("ps", b3)])
                                P.op("act", lambda e, c4=c4, b1=b1, n=n: e.activation(out=sl[c4 % 2][:, :n], in_=PS[b1][:, :n], func=AF.Silu), [("ps", b1)], [("sle", c4 % 2)])
                                P.op("pool", lambda e, c4=c4, ex=ex, bs=bs, n=n: e.tensor_tensor(out=tg[c4 % 2][:, :n], in0=sl[c4 % 2][:, :n], in1=gw[:, ex, bs], op=ALU.mult),
                                     [("sle", c4 % 2), ("gw", bix)], [("tge", c4 % 2)])
                                P.op("dve", lambda e, c4=c4, hj=hj, b3=b3, n=n: e.tensor_tensor(out=hid[hj][:, c4, :n], in0=PS[b3][:, :n], in1=tg[c4 % 2][:, :n], op=ALU.mult),
                                     [("ps", b3), ("tge", c4 % 2)], [("hide", hj, c4)])
                            for oc in range(8):
                                bnk = 4 + oc % 4
                                def mm2_(e, oc=oc, bnk=bnk, hj=hj, w2s=w2s, n=n):
                                    for c4 in range(4):
                                        r = e.matmul(PS[bnk][:, :n], lhsT=w2s[:, c4, oc * 128:(oc + 1) * 128], rhs=hid[hj][:, c4, :n],
                                                     start=(c4 == 0), stop=(c4 == 3))
                                    return r
                                P.op("pe", mm2_, [("hide", hj, c4) for c4 in range(4)] + [("wsl", j, 2)], [("ps", bnk)])
                                P.op("dve", lambda e, oc=oc, bnk=bnk, bs=bs, tc=tc, n=n: e.scalar_tensor_tensor(
                                    out=xe[:, oc, bs], in0=PS[bnk][:, :n], scalar=modp(5, oc, tc), in1=xe[:, oc, bs],
                                    op0=ALU.mult, op1=ALU.add), [("ps", bnk), ("xe", bix), "MOD"], [("xe", bix)])
                            hi += 1
                            off += n
                        if ex == NEXP - 1 and fsl == 6:
                            off = 0
                            for bix, (s, t0, n, tc) in enumerate(blks):
                                for sub in range(n // 256):
                                    bs = slice(off + sub * 256, off + sub * 256 + 256)
                                    tt0 = t0 + sub * 256
                                    ln_block(ss, xe[:, :, bs], 256, 2, 3, 1, 0, 1, XK=("xe", bix))
                                    if last:
                                        if tc != 2:
                                            P.dma("sp", outT[s, :, :, tt0:tt0 + 256].rearrange("k p t -> p k t"), xe[:, :, bs], [("xe", bix)], [("OUT", s, tt0)], ("xe_st", bix))
                                    else:
                                        P.dma("sp", XS[s, :, :, tt0:tt0 + 256].rearrange("k p t -> p k t"), xe[:, :, bs], [("xe", bix)], [("XSo", s, tt0)], ("xe_st", bix))
                                off += n
                    stage_end()
        if stop_after is not None:
            dbg = dt("dbgXS", [SEQS, 8, 128, T], F32, kind="ExternalOutput").ap()
            for s_ in range(SEQS):
                P.dma("sp", dbg[s_], XS[s_], [], [("dbg", s_)], ("dbg", s_))
        P.barrier()
        P.emit(block)
    return nc


def _host_inputs(inputs, layers):
    f32 = np.float32
    x = np.asarray(inputs["x"], f32)
    ctx = np.asarray(inputs["ctx"], f32)
    c = np.asarray(inputs["c"], f32)
    c_ctx = np.asarray(inputs["c_ctx"], f32)
    PM, pm_lists, PMC = _pool_constants()
    cosT, sinT = _rope_tables()
    E, masks = _misc_constants()
    common = dict(cosT=cosT, sinT=sinT, etab=E, masks=masks, pm=PM, pmc=PMC, ident=np.eye(128, dtype=f32))
    for l in layers:
        common["ada_w%d" % l] = np.ascontiguousarray(inputs["ada_w"][l], f32)
        common["ada_bT%d" % l] = np.ascontiguousarray(np.asarray(inputs["ada_b"][l], f32).reshape(48, 128).T)
        common["w_in%d" % l] = np.ascontiguousarray(inputs["w_in"][l], f32)
        common["pool_w%d" % l] = np.ascontiguousarray(inputs["pool_w"][l], f32)
        common["pscT%d" % l] = np.ascontiguousarray(np.asarray(inputs["pool_scale"][l], f32).reshape(4, 128).T)
        common["w_pool_out%d" % l] = np.ascontiguousarray(inputs["w_pool_out"][l], f32)
        common["w_ret_out%d" % l] = np.ascontiguousarray(inputs["w_ret_out"][l], f32)
        common["logit_bc%d" % l] = np.ascontiguousarray(
            np.broadcast_to(np.asarray(inputs["ret_decay_logit"][l], f32).reshape(1, 8), (128, 8)))
        common["w_out%d" % l] = np.ascontiguousarray(inputs["w_out"][l], f32)
        lnT = np.stack([np.asarray(inputs[k][l], f32).reshape(8, 128).T
                        for k in ("ln_mix_g", "ln_mix_b", "ln_ffn_g", "ln_ffn_b")], axis=1)
        common["lnT%d" % l] = np.ascontiguousarray(lnT)
        if l % 2 == 0:
            common["ffn_w1_%d" % l] = np.ascontiguousarray(inputs["ffn_w1"][l // 2], f32)
            common["ffn_w3_%d" % l] = np.ascontiguousarray(inputs["ffn_w3"][l // 2], f32)
            common["ffn_w2_%d" % l] = np.ascontiguousarray(inputs["ffn_w2"][l // 2], f32)
        else:
            common["moe_router%d" % l] = np.ascontiguousarray(inputs["moe_router"][l // 2], f32)
            common["moe_w1_%d" % l] = np.ascontiguousarray(inputs["moe_w1"][l // 2], f32)
            common["moe_w3_%d" % l] = np.ascontiguousarray(inputs["moe_w3"][l // 2], f32)
            common["moe_w2_%d" % l] = np.ascontiguousarray(inputs["moe_w2"][l // 2], f32)
    in_maps = []
    for core in range(NCORES):
        m = dict(common)
        xs = []
        for s in range(SEQS):
            b = core * SEQS + s
            xt = np.concatenate([x[b].T, ctx[b].T], axis=1)
            xs.append(xt.reshape(8, 128, T))
        m["xT"] = np.ascontiguousarray(np.stack(xs))
        cs = np.stack([c[core * SEQS], c[core * SEQS + 1], c_ctx], axis=1)
        m["cT"] = np.ascontiguousarray(cs.reshape(8, 128, 3).transpose(1, 0, 2))
        in_maps.append(m)
    return in_maps, pm_lists, PM.shape[1]


def kernel(**inputs):
    layers = (0, 1, 2, 3)
    in_maps, pm_lists, pm_slots = _host_inputs(inputs, layers)
    nc = build(layers, None, pm_lists, pm_slots)
    res = run_bass_kernel_spmd(nc, in_maps, core_ids=list(range(NCORES)))
    out = np.empty((NCORES * SEQS, L, D), np.float32)
    for core in range(NCORES):
        o = res.results[core]["outT"]
        for s in range(SEQS):
            out[core * SEQS + s] = o[s].reshape(D, L).T
    return out
```

```python
import numpy as np
import ml_dtypes
from contextlib import ExitStack
import concourse.bass as bass
import concourse.mybir as mybir
from concourse.bass_utils import run_bass_kernel_spmd

F32 = mybir.dt.float32
BF16 = mybir.dt.bfloat16
AF = mybir.ActivationFunctionType
ALU = mybir.AluOpType

D = 1024
L = 4096
NCTX = 256
T = L + NCTX
NT = T // 128
DEPTH = 4
GRID = 64
WINS = (2, 4, 8, 16)
DFF = 2816
EFF = 3584
NEXP = 8
ALPHA = (2 * DEPTH) ** 0.25
EPS = 1e-5
NCORES = 8
SEQS = 2


class Prog:
    ENGS = ("pe", "dve", "act", "pool", "sp")

    def __init__(self, nc, es):
        self.nc = nc
        self.es = es
        self.streams = {e: [] for e in self.ENGS}
        self.cnt = {e: 0 for e in self.ENGS}
        self.sem = {e: es.enter_context(nc.semaphore("s_" + e)) for e in ("pe", "dve", "act", "pool")}
        self.dsem = {}
        self.res = {}
        self.known = {e: {} for e in self.ENGS}

    def _dma_sem(self, key):
        if key not in self.dsem:
            self.dsem[key] = [self.es.enter_context(self.nc.semaphore("d%d" % len(self.dsem))), 0]
        return self.dsem[key]

    def _need(self, eng, toks):
        need = {}
        for kind, (teng, sem, val) in toks:
            if teng == eng and eng in ("pe", "sp"):
                continue
            k = id(sem)
            if k not in need or need[k][1] < val:
                need[k] = (sem, val)
        out = []
        kn = self.known[eng]
        for k, (sem, val) in need.items():
            if kn.get(k, 0) >= val:
                continue
            kn[k] = val
            out.append((sem, val))
        return out

    def _deps(self, eng, reads, writes):
        toks = []
        for r in reads:
            st = self.res.get(r)
            if st and st["w"] is not None:
                toks.append(("raw", st["w"]))
        for w in writes:
            st = self.res.get(w)
            if st:
                if st["w"] is not None:
                    toks.append(("waw", st["w"]))
                for t in st["r"]:
                    toks.append(("war", t))
        return self._need(eng, toks)

    def _commit(self, tok, reads, writes):
        for r in reads:
            st = self.res.setdefault(r, {"w": None, "r": []})
            st["r"].append(tok)
        for w in writes:
            self.res[w] = {"w": tok, "r": []}

    def op(self, eng, fn, reads=(), writes=()):
        waits = self._deps(eng, reads, writes)
        self.cnt[eng] += 1
        sem = self.sem[eng]
        self.streams[eng].append((waits, fn, sem, 1))
        self._commit((eng, sem, self.cnt[eng]), reads, writes)

    def dma(self, q, out, in_, reads, writes, key, **kw):
        waits = self._deps(q, reads, writes)
        ds = self._dma_sem(key)
        ds[1] += 16
        sem, val = ds[0], ds[1]

        def fn(e, out=out, in_=in_, kw=kw):
            return e.dma_start(out=out, in_=in_, **kw)

        self.streams[q].append((waits, fn, sem, 16))
        self._commit(("dma", sem, val), reads, writes)

    def barrier(self):
        toks = []
        for e in ("pe", "dve", "act", "pool"):
            if self.cnt[e]:
                toks.append(("raw", (e, self.sem[e], self.cnt[e])))
        for key, (sem, val) in self.dsem.items():
            if val:
                toks.append(("raw", ("dma", sem, val)))
        for e in self.ENGS:
            waits = self._need(e, [t for t in toks if t[1][0] != e or e == "sp"])
            if waits:
                self.streams[e].append((waits, None, None, 0))
        self.res = {}

    def emit(self, block):
        engs = {"pe": block.tensor, "dve": block.vector, "act": block.scalar, "pool": block.gpsimd, "sp": block.sync}
        for name, deco in engs.items():
            stream = self.streams[name]
            if not stream:
                continue

            def body(e, stream=stream):
                for waits, fn, sem, inc in stream:
                    for ws, wv in waits:
                        e.wait_ge(ws, wv)
                    if fn is not None:
                        fn(e).then_inc(sem, inc)

            deco(body)
            self.streams[name] = []


def _box_matrix(n, w):
    left = w // 2
    right = w - 1 - left
    A = np.zeros((n, n), np.float64)
    for t in range(n):
        lo = max(t - left, 0)
        hi = min(t + right + 1, n)
        A[t, lo:hi] = 1.0 / (hi - lo)
    return A


def _pool_constants():
    sets = []
    lists = []
    mats = []
    for si, ob in enumerate((0, 3, 7)):
        lst_g = []
        for g, w in enumerate(WINS):
            A = _box_matrix(GRID, w)
            lst = []
            rows_out = np.arange(8 * ob, 8 * ob + 8)
            for tin in range(32):
                rows_in = np.arange(2 * tin, 2 * tin + 2)
                Ar = A[np.ix_(rows_out, rows_in)]
                if not np.any(Ar):
                    continue
                M = np.kron(Ar, A)
                if tin // 4 == ob:
                    o0 = (tin - 4 * ob) * 128
                    M[o0:o0 + 128, :] -= np.eye(128)
                lst.append((tin - 4 * ob, len(mats)))
                mats.append(M.T.astype(np.float32))
            lst_g.append(lst)
        lists.append(lst_g)
    out_sets = []
    out_lists = []
    for si in range(3):
        slots = []
        lg = []
        for g in range(4):
            l2 = []
            for rel, mi in lists[si][g]:
                l2.append((rel, len(slots)))
                slots.append(mats[mi])
            lg.append(l2)
        out_sets.append(np.stack(slots))
        out_lists.append(lg)
    nmax = max(s.shape[0] for s in out_sets)
    PM = np.zeros((3, nmax, 128, 512), np.float32)
    for si in range(3):
        PM[si, :out_sets[si].shape[0]] = out_sets[si]
    PMC = np.zeros((4, 2, 128, 256), np.float32)
    for g, w in enumerate(WINS):
        A = _box_matrix(NCTX, w) - np.eye(NCTX)
        for tin in range(2):
            PMC[g, tin] = A[:, tin * 128:(tin + 1) * 128].T
    return PM.astype(ml_dtypes.bfloat16), out_lists, PMC.astype(ml_dtypes.bfloat16)


def _rope_tables():
    t = np.arange(L)
    row = (t // GRID).astype(np.float32)
    col = (t % GRID).astype(np.float32)
    n_freq = 32
    inv = np.exp(-np.log(np.float32(10000.0)) * np.arange(n_freq, dtype=np.float32) / n_freq).astype(np.float32)
    ang = np.concatenate([row[:, None] * inv, col[:, None] * inv], -1).astype(np.float32)
    cos = np.ones((T, 64), np.float32)
    sin = np.zeros((T, 64), np.float32)
    cos[:L] = np.cos(ang)
    sin[:L] = np.sin(ang)
    cosT = np.ascontiguousarray(cos.reshape(NT, 128, 64).transpose(1, 0, 2))
    sinT = np.ascontiguousarray(sin.reshape(NT, 128, 64).transpose(1, 0, 2))
    return cosT, sinT


def _misc_constants():
    j = np.arange(128, dtype=np.float32)
    E = np.zeros((128, 16), np.float32)
    for h in range(4):
        E[:, 0 + h] = j - 127.0
        E[:, 4 + h] = -j
        E[:, 8 + h] = 127.0 - j
        E[:, 12 + h] = j
    jj = np.arange(128)[:, None]
    ii = np.arange(128)[None, :]
    masks = np.zeros((128, 2, 128), np.float32)
    masks[:, 0, :] = (ii >= jj)
    masks[:, 1, :] = (ii <= jj)
    return E, masks


def build(layers=(0, 1, 2, 3), stop_after=None, pm_lists=None, pm_slots=31):
    nc = bass.Bass("TRN2", target_bir_lowering=False)
    dt = nc.dram_tensor

    def inp(name, shape, dtype=F32):
        return dt(name, list(shape), dtype, kind="ExternalInput").ap()

    def scr(name, shape, dtype):
        return dt(name, list(shape), dtype).ap()

    xT_in = inp("xT", [SEQS, 8, 128, T])
    cT_in = inp("cT", [128, 8, 3])
    cos_in = inp("cosT", [128, NT, 64])
    sin_in = inp("sinT", [128, NT, 64])
    etab_in = inp("etab", [128, 16])
    mask_in = inp("masks", [128, 2, 128])
    pm_in = inp("pm", [3, pm_slots, 128, 512], BF16)
    pmc_in = inp("pmc", [4, 2, 128, 256], BF16)
    ident_in = inp("ident", [128, 128])
    W = {}
    for l in layers:
        W[l] = dict(
            ada_w=inp("ada_w%d" % l, [D, 6 * D]),
            ada_b=inp("ada_bT%d" % l, [128, 48]),
            w_in=inp("w_in%d" % l, [D, 5632]),
            pool_w=inp("pool_w%d" % l, [4, 128, 128]),
            psc=inp("pscT%d" % l, [128, 4]),
            wpo=inp("w_pool_out%d" % l, [512, D]),
            wro=inp("w_ret_out%d" % l, [D, D]),
            logit=inp("logit_bc%d" % l, [128, 8]),
            wo=inp("w_out%d" % l, [D, D]),
            lnp=inp("lnT%d" % l, [128, 4, 8]),
        )
        if l % 2 == 0:
            W[l].update(
                w1=inp("ffn_w1_%d" % l, [D, DFF]),
                w3=inp("ffn_w3_%d" % l, [D, DFF]),
                w2=inp("ffn_w2_%d" % l, [DFF, D]),
            )
        else:
            W[l].update(
                wr=inp("moe_router%d" % l, [D, NEXP]),
                w1=inp("moe_w1_%d" % l, [NEXP, D, EFF]),
                w3=inp("moe_w3_%d" % l, [NEXP, D, EFF]),
                w2=inp("moe_w2_%d" % l, [NEXP, EFF, D]),
            )
    outT = dt("outT", [SEQS, 8, 128, L], F32, kind="ExternalOutput").ap()

    XS = scr("XS", [SEQS, 8, 128, T], F32)
    U_d = scr("U_d", [SEQS, T, 512], BF16)
    V_d = scr("V_d", [SEQS, T, 1024], BF16)
    KF_d = scr("KF_d", [SEQS, T, 512], BF16)
    KB_d = scr("KB_d", [SEQS, T, 512], BF16)
    QKT_d = scr("QKT_d", [SEQS, 16, 128, T], BF16)
    G_d = [scr("G%d_d" % i, [SEQS, 8, 128, T], BF16) for i in range(3)]
    DST_d = scr("DST_d", [SEQS, 2, NT, 4, 128, 256], BF16)
    ZR_d = scr("ZR_d", [SEQS, 8, 128, T], BF16)
    WC_d = scr("WC_d", [NEXP, 7, 3, 128, 4096], BF16)

    blocks = [(i * 512, 512, False) for i in range(8)] + [(L, NCTX, True)]

    with ExitStack() as es:
        P = Prog(nc, es)
        block = es.enter_context(nc.Block())
        sb = lambda name, shape, dtype=F32: es.enter_context(nc.sbuf_tensor(name, list(shape), dtype))
        ident_f = sb("ident_f", [128, 128])
        ident_b = sb("ident_b", [128, 128], BF16)
        ones_b = sb("ones_b", [128, 128], BF16)
        ones_f = sb("ones_f", [128, 128])
        masks = sb("masks_s", [128, 2, 128])
        etab = sb("etab_s", [128, 16])
        cT = sb("cT_s", [128, 8, 3])
        sT = sb("sT_s", [128, 8, 3])
        MOD = sb("MOD", [128, 48, 3])
        lnp = sb("lnp", [128, 4, 8])
        psc = sb("psc", [128, 4])
        lgt = sb("lgt", [128, 8])
        lg16 = sb("lg16", [128, 16])
        DEC = sb("DEC", [128, 16])
        GC = sb("GC", [128, 8])
        epsb = sb("epsb", [128, 2])
        PS = [es.enter_context(nc.psum_tensor("ps%d" % i, [128, 512], F32)) for i in range(8)]

        P.dma("sp", ident_f[:], ident_in, [], ["ident_f"], "c0")
        P.dma("sp", masks[:], mask_in, [], ["masks"], "c1")
        P.dma("sp", etab[:], etab_in, [], ["etab"], "c2")
        P.dma("sp", cT[:], cT_in, [], ["cT"], "c3")
        P.op("dve", lambda e: e.tensor_copy(out=ident_b[:], in_=ident_f[:]), ["ident_f"], ["ident_b"])
        P.op("dve", lambda e: e.memset(ones_b[:], 1.0 / 1024.0), [], ["ones_b"])
        P.op("dve", lambda e: e.memset(ones_f[:], 1.0 / 256.0), [], ["ones_f"])
        P.op("dve", lambda e: e.memset(epsb[:, 0:1], EPS), [], ["epsb0"])
        P.op("dve", lambda e: e.memset(epsb[:, 1:2], EPS / (ALPHA * ALPHA)), [], ["epsb1"])
        P.op("act", lambda e: e.activation(out=sT[:], in_=cT[:], func=AF.Silu), ["cT"], ["sT"])

        def stage_end():
            P.barrier()
            P.emit(block)

        def ln_block(ss, xb, n, gk, bk, epscol, pm_i, pe_i, XK="xb"):
            zb, sq, ms, m2, rs = ss["zb"], ss["sq"], ss["ms"], ss["m2"], ss["rs"]
            P.op("act", lambda e: e.activation(out=zb[:, :, :n], in_=xb[:, :, :n], func=AF.Copy), [XK], ["zb"])
            P.op("act", lambda e: e.activation(out=sq[:, :, :n], in_=xb[:, :, :n], func=AF.Square), [XK], ["sq"])

            def mm1(e):
                for k in range(8):
                    r = e.matmul(PS[pm_i][:, :n], lhsT=ones_b[:], rhs=zb[:, k, :n], start=(k == 0), stop=(k == 7))
                return r

            def mm2(e):
                for k in range(8):
                    r = e.matmul(PS[pe_i][:, :n], lhsT=ones_b[:], rhs=sq[:, k, :n], start=(k == 0), stop=(k == 7))
                return r

            P.op("pe", mm1, ["zb", "ones_b"], [("ps", pm_i)])
            P.op("pe", mm2, ["sq", "ones_b"], [("ps", pe_i)])
            P.op("act", lambda e: e.activation(out=ms[:, :n], in_=PS[pm_i][:, :n], func=AF.Copy), [("ps", pm_i)], ["ms"])
            P.op("act", lambda e: e.activation(out=m2[:, :n], in_=PS[pm_i][:, :n], func=AF.Square), [("ps", pm_i)], ["m2"])
            P.op("dve", lambda e: e.tensor_tensor(out=rs[:, :n], in0=PS[pe_i][:, :n], in1=m2[:, :n], op=ALU.subtract),
                 [("ps", pe_i), "m2"], ["rs"])
            P.op("act", lambda e: e.activation(out=rs[:, :n], in_=rs[:, :n], func=AF.Ln, bias=epsb[:, epscol:epscol + 1]),
                 ["rs", "epsb%d" % epscol], ["rs"])
            P.op("act", lambda e: e.activation(out=rs[:, :n], in_=rs[:, :n], func=AF.Exp, scale=-0.5), ["rs"], ["rs"])
            P.op("dve", lambda e: e.tensor_tensor(out=xb[:, :, :n], in0=xb[:, :, :n],
                                                  in1=ms[:, :n].unsqueeze(1).to_broadcast([128, 8, n]), op=ALU.subtract),
                 [XK, "ms"], [XK])
            P.op("dve", lambda e: e.tensor_tensor(out=xb[:, :, :n], in0=xb[:, :, :n],
                                                  in1=rs[:, :n].unsqueeze(1).to_broadcast([128, 8, n]), op=ALU.mult),
                 [XK, "rs"], [XK])
            for k in range(8):
                P.op("act", lambda e, k=k: e.activation(out=xb[:, k, :n], in_=xb[:, k, :n], func=AF.Identity,
                                                         scale=lnp[:, gk, k:k + 1], bias=lnp[:, bk, k:k + 1]),
                     [XK, "lnp"], [XK])

        def load_cast(dst, src_ap, nk, ncols, stage, tag, piece=512):
            srcv = src_ap.rearrange("(k p) n -> p k n", p=128)
            i = 0
            for c0 in range(0, ncols, piece):
                cw = min(piece, ncols - c0)
                for k0 in range(0, nk, 8):
                    kw = min(8, nk - k0)
                    st = stage[i % 2]
                    P.dma("sp", st[:, :kw, :cw], srcv[:, k0:k0 + kw, c0:c0 + cw], [], [("wst", i % 2)], ("wst", i % 2))
                    if i % 2 == 0:
                        P.op("act", lambda e, st=st, kw=kw, cw=cw, k0=k0, c0=c0: e.activation(
                            out=dst[:, k0:k0 + kw, c0:c0 + cw], in_=st[:, :kw, :cw], func=AF.Copy), [("wst", i % 2)], [(tag, i)])
                    else:
                        P.op("dve", lambda e, st=st, kw=kw, cw=cw, k0=k0, c0=c0: e.tensor_copy(
                            out=dst[:, k0:k0 + kw, c0:c0 + cw], in_=st[:, :kw, :cw]), [("wst", i % 2)], [(tag, i)])
                    i += 1

        for li, l in enumerate(layers):
            w = W[l]
            xsrc = xT_in if li == 0 else XS
            need_ctx = l < DEPTH - 1
            with ExitStack() as ls:
                lsb = lambda name, shape, dtype=F32: ls.enter_context(nc.sbuf_tensor(name + "_L%d" % l, list(shape), dtype))
                adst = [lsb("adst%d" % i, [128, 8, 768]) for i in range(2)]
                adb = lsb("adb", [128, 48])
                P.dma("sp", adb[:], w["ada_b"], [], ["adb"], "p0")
                P.dma("sp", lnp[:], w["lnp"], [], ["lnp"], "p1")
                P.dma("sp", psc[:], w["psc"], [], ["psc"], "p2")
                P.dma("sp", lgt[:], w["logit"], [], ["lgt"], "p3")
                adv = w["ada_w"].rearrange("(k p) n -> p k n", p=128)
                for pc in range(8):
                    st = adst[pc % 2]
                    P.dma("sp", st[:], adv[:, :, pc * 768:(pc + 1) * 768], [], [("adst", pc % 2)], ("adst", pc % 2))

                    def mm(e, st=st, pc=pc):
                        for oc in range(6):
                            og = pc * 6 + oc
                            for k in range(8):
                                r = e.matmul(PS[0][:, og * 3:og * 3 + 3], lhsT=st[:, k, oc * 128:(oc + 1) * 128],
                                             rhs=sT[:, k, :], start=(k == 0), stop=(k == 7))
                        return r

                    P.op("pe", mm, [("adst", pc % 2), "sT"], [("ps", 0)])
                P.op("dve", lambda e: e.tensor_tensor(out=MOD[:], in0=PS[0][:, 0:144].rearrange("p (a b) -> p a b", b=3),
                                                      in1=adb[:].unsqueeze(2).to_broadcast([128, 48, 3]), op=ALU.add),
                     [("ps", 0), "adb"], ["MOD"])
                P.op("dve", lambda e: e.tensor_scalar(out=MOD[:, 8:16, :], in0=MOD[:, 8:16, :], scalar1=1.0, scalar2=None, op0=ALU.add), ["MOD"], ["MOD"])
                P.op("dve", lambda e: e.tensor_scalar(out=MOD[:, 16:24, :], in0=MOD[:, 16:24, :], scalar1=1.0 / ALPHA, scalar2=None, op0=ALU.mult), ["MOD"], ["MOD"])
                P.op("dve", lambda e: e.tensor_scalar(out=MOD[:, 32:40, :], in0=MOD[:, 32:40, :], scalar1=1.0, scalar2=None, op0=ALU.add), ["MOD"], ["MOD"])
                P.op("dve", lambda e: e.tensor_scalar(out=MOD[:, 40:48, :], in0=MOD[:, 40:48, :], scalar1=1.0 / ALPHA, scalar2=None, op0=ALU.mult), ["MOD"], ["MOD"])
                P.op("act", lambda e: e.activation(out=lgt[:], in_=lgt[:], func=AF.Exp, scale=-1.0), ["lgt"], ["lgt"])
                P.op("act", lambda e: e.activation(out=lgt[:], in_=lgt[:], func=AF.Ln, bias=1.0), ["lgt"], ["lgt"])
                P.op("dve", lambda e: e.tensor_scalar(out=lg16[:, 0:8], in0=lgt[:], scalar1=-1.0, scalar2=None, op0=ALU.mult), ["lgt"], ["lg16"])
                P.op("dve", lambda e: e.tensor_scalar(out=lg16[:, 8:16], in0=lgt[:], scalar1=-1.0, scalar2=None, op0=ALU.mult), ["lgt", "lg16"], ["lg16"])
                P.op("act", lambda e: e.activation(out=GC[:], in_=lg16[:, 0:8], func=AF.Exp, scale=128.0), ["lg16"], ["GC"])
                P.op("dve", lambda e: e.tensor_tensor(out=DEC[:], in0=lg16[:], in1=etab[:], op=ALU.mult), ["lg16", "etab"], ["DEC"])
                P.op("act", lambda e: e.activation(out=DEC[:], in_=DEC[:], func=AF.Exp), ["DEC"], ["DEC"])
                P.op("dve", lambda e: e.tensor_scalar(out=DEC[:, 8:16], in0=DEC[:, 8:16], scalar1=128.0 ** -0.5, scalar2=None, op0=ALU.mult), ["DEC"], ["DEC"])
                stage_end()
            if stop_after == ("params", l):
                break

            def modp(m, k, tc):
                return MOD[:, m * 8 + k, tc:tc + 1]

            with ExitStack() as ls:
                lsb = lambda name, shape, dtype=F32: ls.enter_context(nc.sbuf_tensor(name + "_L%d" % l, list(shape), dtype))
                WB = lsb("WB", [128, 8, 5632], BF16)
                with ExitStack() as ls2:
                    wst = [ls2.enter_context(nc.sbuf_tensor("wst%d_L%d" % (i, l), [128, 8, 512], F32)) for i in range(2)]
                    load_cast(WB, w["w_in"], 8, 5632, wst, "WB")
                    stage_end()
                cosT = lsb("cosT_s", [128, NT, 64])
                sinT = lsb("sinT_s", [128, NT, 64])
                P.dma("sp", cosT[:], cos_in, [], ["cosT"], "c4")
                P.dma("sp", sinT[:], sin_in, [], ["sinT"], "c5")
                xbs = [lsb("xa%d" % i, [128, 8, 512]) for i in range(2)]
                hb = [lsb("ha%d" % i, [128, 8, 512], BF16) for i in range(1)]
                ub = lsb("ub", [128, 512], BF16)
                vb = lsb("vb", [128, 1024], BF16)
                rt = [lsb("rt%d" % i, [128, 4, 64]) for i in range(4)]
                rot = lsb("rot", [128, 4, 2, 64])
                var_tm = lsb("var_tm", [128, 4, 512], BF16)
                qkT = lsb("qkT", [128, 16, 512], BF16)
                gb = [lsb("gb%d" % i, [128, 8, 512], BF16) for i in range(3)]
                bi = 0
                for s in range(SEQS):
                    for (t0, n, isctx) in blocks:
                        tc = 2 if isctx else s
                        xb = xbs[bi % 2]
                        h = hb[0]
                        xk, hk = ("xa", bi % 2), ("ha", 0)
                        P.dma("sp", xb[:, :, :n], xsrc[s, :, :, t0:t0 + n].rearrange("k p t -> p k t"),
                              [("XS", s, t0)], [xk], xk)
                        for k in range(8):
                            P.op("act", lambda e, k=k, xb=xb, h=h, n=n, tc=tc: e.activation(
                                out=h[:, k, :n], in_=xb[:, k, :n], func=AF.Identity,
                                scale=modp(1, k, tc), bias=modp(0, k, tc)), [xk, "MOD"], [hk])
                        for ti in range(n // 128):
                            gt = (t0 // 128) + ti
                            tsl = slice(ti * 128, (ti + 1) * 128)
                            rows = slice(t0 + ti * 128, t0 + ti * 128 + 128)
                            for bnk, c0 in enumerate((0, 512, 1024, 1536, 2048)):
                                def mm(e, bnk=bnk, c0=c0, h=h, tsl=tsl):
                                    for k in range(8):
                                        r = e.matmul(PS[bnk][:, :], lhsT=h[:, k, tsl], rhs=WB[:, k, c0:c0 + 512],
                                                     start=(k == 0), stop=(k == 7))
                                    return r
                                P.op("pe", mm, [hk, "WB"], [("ps", bnk)])
                            P.op("act", lambda e: e.activation(out=ub[:], in_=PS[0][:, :], func=AF.Copy), [("ps", 0)], ["ub"])
                            P.dma("sp", U_d[s, rows, :], ub[:], ["ub"], [("U", s, gt)], "ub")
                            P.op("act", lambda e: e.activation(out=vb[:, 0:512], in_=PS[3][:, :], func=AF.Copy), [("ps", 3)], ["vb0"])
                            P.op("dve", lambda e: e.tensor_copy(out=vb[:, 512:1024], in_=PS[4][:, :]), [("ps", 4)], ["vb1"])
                            P.dma("sp", V_d[s, rows, :], vb[:], ["vb0", "vb1"], [("V", s, gt)], "vb")
                            for qi, bnk in enumerate((1, 2)):
                                pv = PS[bnk][:, :].rearrange("p (h two d) -> p h two d", two=2, d=64)
                                Cb = cosT[:, gt, :].unsqueeze(1).to_broadcast([128, 4, 64])
                                Sb = sinT[:, gt, :].unsqueeze(1).to_broadcast([128, 4, 64])
                                P.op("dve", lambda e, pv=pv, Cb=Cb: e.tensor_tensor(out=rt[0][:], in0=pv[:, :, 0, :], in1=Cb, op=ALU.mult), [("ps", bnk), "cosT"], ["rt0"])
                                P.op("dve", lambda e, pv=pv, Sb=Sb: e.tensor_tensor(out=rt[1][:], in0=pv[:, :, 1, :], in1=Sb, op=ALU.mult), [("ps", bnk), "sinT"], ["rt1"])
                                P.op("dve", lambda e, pv=pv, Sb=Sb: e.tensor_tensor(out=rt[2][:], in0=pv[:, :, 0, :], in1=Sb, op=ALU.mult), [("ps", bnk), "sinT"], ["rt2"])
                                P.op("dve", lambda e, pv=pv, Cb=Cb: e.tensor_tensor(out=rt[3][:], in0=pv[:, :, 1, :], in1=Cb, op=ALU.mult), [("ps", bnk), "cosT"], ["rt3"])
                                P.op("dve", lambda e: e.tensor_tensor(out=rot[:, :, 0, :], in0=rt[0][:], in1=rt[1][:], op=ALU.subtract), ["rt0", "rt1"], ["rot0"])
                                P.op("dve", lambda e: e.tensor_tensor(out=rot[:, :, 1, :], in0=rt[2][:], in1=rt[3][:], op=ALU.add), ["rt2", "rt3"], ["rot1"])
                                for dr in range(2):
                                    vi = qi * 2 + dr
                                    for hh in range(4):
                                        P.op("act", lambda e, vi=vi, hh=hh, dr=dr, qi=qi: e.activation(
                                            out=var_tm[:, vi, hh * 128:(hh + 1) * 128],
                                            in_=rot[:, hh, :, :].rearrange("p a b -> p (a b)"), func=AF.Identity,
                                            scale=DEC[:, qi * 8 + dr * 4 + hh:qi * 8 + dr * 4 + hh + 1]),
                                            ["rot0", "rot1", "DEC"], [("var", vi)])
                            P.dma("sp", KF_d[s, rows, :], var_tm[:, 2, :], [("var", 2)], [("KF", s, gt)], "kf")
                            P.dma("sp", KB_d[s, rows, :], var_tm[:, 3, :], [("var", 3)], [("KB", s, gt)], "kb")
                            p5 = PS[5][:, :].bitcast(BF16).rearrange("p (a b) -> p a b", b=128)[:, 0:8, :]
                            for half in range(2):
                                def tr(e, half=half):
                                    for j in range(8):
                                        vi = half * 2 + j // 4
                                        hh = j % 4
                                        r = e.transpose(p5[:, j, :], var_tm[:, vi, hh * 128:(hh + 1) * 128], ident_b[:])
                                    return r
                                P.op("pe", tr, [("var", half * 2), ("var", half * 2 + 1), "ident_b"], [("ps", 5)])
                                P.op("dve", lambda e, half=half, tsl=tsl: e.tensor_copy(out=qkT[:, half * 8:(half + 1) * 8, tsl], in_=p5),
                                     [("ps", 5)], [("qkT", half)])
                        P.dma("sp", QKT_d[s, :, :, t0:t0 + n].rearrange("a p t -> p a t"), qkT[:, :, :n],
                              [("qkT", 0), ("qkT", 1)], [("QKT", s, t0)], "qkT")
                        for gi in range(3):
                            func = AF.Silu if gi == 0 else AF.Sigmoid
                            for oc in range(8):
                                bnk = 6 + (oc % 2)
                                c0 = 2560 + gi * 1024 + oc * 128
                                def mm(e, bnk=bnk, c0=c0, h=h, n=n):
                                    for k in range(8):
                                        r = e.matmul(PS[bnk][:, :n], lhsT=WB[:, k, c0:c0 + 128], rhs=h[:, k, :n],
                                                     start=(k == 0), stop=(k == 7))
                                    return r
                                P.op("pe", mm, [hk, "WB"], [("ps", bnk)])
                                P.op("act", lambda e, bnk=bnk, gi=gi, oc=oc, n=n, func=func: e.activation(
                                    out=gb[gi][:, oc, :n], in_=PS[bnk][:, :n], func=func), [("ps", bnk)], [("gb", gi, oc)])
                            P.dma("sp", G_d[gi][s, :, :, t0:t0 + n].rearrange("k p t -> p k t"), gb[gi][:, :, :n],
                                  [("gb", gi, oc) for oc in range(8)], [("G", gi, s, t0)], ("gb", gi))
                        bi += 1
                stage_end()
            if stop_after == ("A", l):
                break

            with ExitStack() as ls:
                lsb = lambda name, shape, dtype=F32: ls.enter_context(nc.sbuf_tensor(name + "_L%d" % l, list(shape), dtype))
                Sst = lsb("Sst", [128, 1024])
                Df = lsb("Df", [128, 1024])
                Dbf = [lsb("Dbf%d" % i, [128, 1024], BF16) for i in range(2)]
                kt = [lsb("kt%d" % i, [128, 512], BF16) for i in range(2)]
                vt = [lsb("vt%d" % i, [128, 1024], BF16) for i in range(2)]
                it = 0
                for s in range(SEQS):
                    for dr in range(2):
                        order = [32, 33] + list(range(32)) if dr == 0 else [33, 32] + list(range(31, -1, -1))
                        Ksrc = KF_d if dr == 0 else KB_d
                        P.op("dve", lambda e: e.memset(Sst[:], 0.0), [], ["S"])
                        for c in order:
                            j = it % 2
                            rows = slice(c * 128, (c + 1) * 128)
                            P.dma("sp", kt[j][:], Ksrc[s, rows, :], [("KF", s), ("KB", s)], [("kt", j)], ("kt", j))
                            P.dma("sp", vt[j][:], V_d[s, rows, :], [("V", s)], [("vt", j)], ("vt", j))
                            for hh in range(4):
                                P.op("dve", lambda e, hh=hh, dr=dr: e.tensor_scalar(
                                    out=Df[:, hh * 256:(hh + 1) * 256], in0=Sst[:, hh * 256:(hh + 1) * 256],
                                    scalar1=GC[:, dr * 4 + hh:dr * 4 + hh + 1], scalar2=None, op0=ALU.mult), ["S", "GC"], [("Df", hh)])
                            P.op("act", lambda e, j=j: e.activation(out=Dbf[j][:], in_=Df[:], func=AF.Copy),
                                 [("Df", hh) for hh in range(4)], [("Dbf", j)])
                            P.dma("sp", DST_d[s, dr, c].rearrange("h p v -> p h v"),
                                  Dbf[j][:].rearrange("p (h v) -> p h v", v=256), [("Dbf", j)], [("DST", s, dr, c)], ("Dbf", j))

                            def mm(e, j=j):
                                for hh in range(4):
                                    r = e.matmul(PS[hh // 2][:, (hh % 2) * 256:(hh % 2) * 256 + 256],
                                                 lhsT=kt[j][:, hh * 128:(hh + 1) * 128], rhs=vt[j][:, hh * 256:(hh + 1) * 256],
                                                 start=True, stop=True)
                                return r
                            P.op("pe", mm, [("kt", j), ("vt", j)], [("ps", 0), ("ps", 1)])
                            for b2 in range(2):
                                P.op("dve", lambda e, b2=b2: e.tensor_tensor(
                                    out=Sst[:, b2 * 512:(b2 + 1) * 512], in0=PS[b2][:, :], in1=Df[:, b2 * 512:(b2 + 1) * 512], op=ALU.add),
                                    [("ps", b2), ("Df", 2 * b2), ("Df", 2 * b2 + 1)], ["S"])
                            it += 1
                stage_end()

            with ExitStack() as ls:
                lsb = lambda name, shape, dtype=F32: ls.enter_context(nc.sbuf_tensor(name + "_L%d" % l, list(shape), dtype))
                qk = [lsb("qk%d" % i, [128, 16, 128], BF16) for i in range(2)]
                vt = [lsb("vc%d" % i, [128, 1024], BF16) for i in range(2)]
                Dt = [lsb("Dt%d" % i, [128, 2, 4, 256], BF16) for i in range(2)]
                sg = [lsb("sg%d" % i, [128, 8, 128], BF16) for i in range(2)]
                PT = lsb("PT", [128, 8, 128], BF16)
                of = lsb("of", [128, 8, 128])
                osq = lsb("osq", [128, 8, 128])
                msr = lsb("msr", [128, 4, 128])
                m2r = lsb("m2r", [128, 4, 128])
                rsr = lsb("rsr", [128, 4, 128])
                zrt = [lsb("zrt%d" % i, [128, 8, 128], BF16) for i in range(2)]
                it = 0
                ntl = NT if need_ctx else 32
                for s in range(SEQS):
                    for c in range(ntl):
                        j = it % 2
                        cs = slice(c * 128, (c + 1) * 128)
                        P.dma("sp", qk[j][:], QKT_d[s, :, :, cs].rearrange("a p t -> p a t"), [("QKT", s)], [("qk", j)], ("qk", j))
                        P.dma("sp", vt[j][:], V_d[s, cs, :], [("V", s)], [("vc", j)], ("vc", j))
                        for dr in range(2):
                            P.dma("sp", Dt[j][:, dr], DST_d[s, dr, c].rearrange("h p v -> p h v"), [("DST", s)], [("Dt", j)], ("Dt", j))
                        P.dma("sp", sg[j][:], G_d[0][s, :, :, cs].rearrange("k p t -> p k t"), [("G", 0, s)], [("sg", j)], ("sg", j))
                        for dr in range(2):
                            def mm(e, dr=dr, j=j):
                                for hh in range(4):
                                    r = e.matmul(PS[dr][:, hh * 128:(hh + 1) * 128], lhsT=qk[j][:, (2 + dr) * 4 + hh, :],
                                                 rhs=qk[j][:, dr * 4 + hh, :], start=True, stop=True)
                                return r
                            P.op("pe", mm, [("qk", j)], [("ps", dr)])
                            P.op("dve", lambda e, dr=dr: e.tensor_tensor(
                                out=PT[:, dr * 4:(dr + 1) * 4, :], in0=PS[dr][:, :].rearrange("p (h i) -> p h i", i=128),
                                in1=masks[:, dr, :].unsqueeze(1).to_broadcast([128, 4, 128]), op=ALU.mult),
                                [("ps", dr), "masks"], [("PT", dr)])
                        for b2 in range(2):
                            def mm(e, b2=b2, j=j):
                                for q in range(4):
                                    ch = b2 * 4 + q
                                    hh, m = ch // 2, ch % 2
                                    o = PS[2 + b2][:, q * 128:(q + 1) * 128]
                                    for dr in range(2):
                                        e.matmul(o, lhsT=vt[j][:, hh * 256 + m * 128:hh * 256 + m * 128 + 128],
                                                 rhs=PT[:, dr * 4 + hh, :], start=(dr == 0), stop=False)
                                        r = e.matmul(o, lhsT=Dt[j][:, dr, hh, m * 128:(m + 1) * 128],
                                                     rhs=qk[j][:, dr * 4 + hh, :], start=False, stop=(dr == 1))
                                return r
                            P.op("pe", mm, [("vc", j), ("PT", 0), ("PT", 1), ("Dt", j), ("qk", j)], [("ps", 2 + b2)])
                            P.op("act", lambda e, b2=b2: e.activation(out=of[:, b2 * 4:(b2 + 1) * 4, :].rearrange("p a b -> p (a b)"),
                                                                     in_=PS[2 + b2][:, :], func=AF.Copy), [("ps", 2 + b2)], [("of", b2)])
                            P.op("act", lambda e, b2=b2: e.activation(out=osq[:, b2 * 4:(b2 + 1) * 4, :].rearrange("p a b -> p (a b)"),
                                                                     in_=PS[2 + b2][:, :], func=AF.Square), [("ps", 2 + b2)], [("osq", b2)])
                        def mmst(e):
                            for hh in range(4):
                                for m in range(2):
                                    e.matmul(PS[4][:, hh * 128:(hh + 1) * 128], lhsT=ones_f[:], rhs=of[:, hh * 2 + m, :],
                                             start=(m == 0), stop=(m == 1))
                            for hh in range(4):
                                for m in range(2):
                                    r = e.matmul(PS[5][:, hh * 128:(hh + 1) * 128], lhsT=ones_f[:], rhs=osq[:, hh * 2 + m, :],
                                                 start=(m == 0), stop=(m == 1))
                            return r
                        P.op("pe", mmst, [("of", 0), ("of", 1), ("osq", 0), ("osq", 1), "ones_f"], [("ps", 4), ("ps", 5)])
                        fl = lambda t: t[:].rearrange("p a b -> p (a b)")
                        P.op("act", lambda e: e.activation(out=fl(msr), in_=PS[4][:, :], func=AF.Copy), [("ps", 4)], ["msr"])
                        P.op("act", lambda e: e.activation(out=fl(m2r), in_=PS[4][:, :], func=AF.Square), [("ps", 4)], ["m2r"])
                        P.op("dve", lambda e: e.tensor_tensor(out=fl(rsr), in0=PS[5][:, :], in1=fl(m2r), op=ALU.subtract), [("ps", 5), "m2r"], ["rsr"])
                        P.op("act", lambda e: e.activation(out=fl(rsr), in_=fl(rsr), func=AF.Ln, bias=epsb[:, 0:1]), ["rsr", "epsb0"], ["rsr"])
                        P.op("act", lambda e: e.activation(out=fl(rsr), in_=fl(rsr), func=AF.Exp, scale=-0.5), ["rsr"], ["rsr"])
                        ov = of[:].rearrange("p (h m) t -> p h m t", m=2)
                        P.op("dve", lambda e, ov=ov: e.tensor_tensor(out=ov, in0=ov, in1=msr[:].unsqueeze(2).to_broadcast([128, 4, 2, 128]), op=ALU.subtract),
                             [("of", 0), ("of", 1), "msr"], [("of", 0), ("of", 1)])
                        P.op("dve", lambda e, ov=ov: e.tensor_tensor(out=ov, in0=ov, in1=rsr[:].unsqueeze(2).to_broadcast([128, 4, 2, 128]), op=ALU.mult),
                             [("of", 0), ("of", 1), "rsr"], [("of", 0), ("of", 1)])
                        P.op("dve", lambda e, j=j: e.tensor_tensor(out=zrt[j][:], in0=of[:], in1=sg[j][:], op=ALU.mult),
                             [("of", 0), ("of", 1), ("sg", j)], [("zrt", j)])
                        P.dma("sp", ZR_d[s, :, :, cs].rearrange("k p t -> p k t"), zrt[j][:], [("zrt", j)], [("ZR", s, c)], ("zrt", j))
                        it += 1
                stage_end()
            if stop_after == ("C1", l):
                break

            with ExitStack() as ls:
                lsb = lambda name, shape, dtype=F32: ls.enter_context(nc.sbuf_tensor(name + "_L%d" % l, list(shape), dtype))
                wro = lsb("wro", [128, 8, 1024], BF16)
                wo = lsb("wo", [128, 8, 1024], BF16)
                wpo = lsb("wpo", [128, 4, 1024], BF16)
                plw = lsb("plw", [128, 4, 128], BF16)
                with ExitStack() as ls2:
                    wst = [ls2.enter_context(nc.sbuf_tensor("wstc%d_L%d" % (i, l), [128, 8, 512], F32)) for i in range(2)]
                    load_cast(wro, w["wro"], 8, 1024, wst, "wro")
                    load_cast(wo, w["wo"], 8, 1024, wst, "wo")
                    load_cast(wpo, w["wpo"], 4, 1024, wst, "wpo")
                    P.dma("sp", wst[0][:, 0:4, 0:128], w["pool_w"].rearrange("g c d -> c g d"), [], [("wst", 0)], ("wst", 0))
                    P.op("pool", lambda e: e.tensor_copy(out=plw[:], in_=wst[0][:, 0:4, 0:128]), [("wst", 0)], ["plw"])
                    stage_end()
                Ures = lsb("Ures", [128, NT, 512], BF16)
                PMb = lsb("PMb", [128, pm_slots, 512], BF16)
                PMc = lsb("PMc", [128, 8, 256], BF16)
                P.dma("sp", PMc[:], pmc_in.rearrange("g t p o -> p (g t) o"), [], ["PMc"], "pmc")
                zr = lsb("zr", [128, 8, 512], BF16)
                spb = lsb("spb", [128, 8, 512], BF16)
                srb = lsb("srb", [128, 8, 512], BF16)
                dTb = lsb("dTb", [128, 4, 512], BF16)
                ygb = lsb("ygb", [128, 4, 512], BF16)
                mixb = lsb("mixb", [128, 8, 512], BF16)
                t1 = lsb("t1", [128, 512])
                t2 = lsb("t2", [128, 512])
                xb = lsb("xc", [128, 8, 512])
                ss = dict(zb=lsb("zb", [128, 8, 512], BF16), sq=lsb("sq", [128, 8, 512], BF16),
                          ms=lsb("ms", [128, 512]), m2=lsb("m2", [128, 512]), rs=lsb("rs", [128, 512]))
                for s in range(SEQS):
                    P.dma("sp", Ures[:], U_d[s].rearrange("(t p) c -> p t c", p=128), [("U", s)], ["Ures"], "Ures")
                    for ob, (t0, n, isctx) in enumerate(blocks):
                        if isctx and not need_ctx:
                            continue
                        tc = 2 if isctx else s
                        bsl = (slice(None), slice(None), slice(t0, t0 + n))
                        P.dma("sp", zr[:, :, :n], ZR_d[s][bsl].rearrange("k p t -> p k t"), [("ZR", s)], ["zr"], "zr")
                        P.dma("sp", spb[:, :, :n], G_d[1][s][bsl].rearrange("k p t -> p k t"), [("G", 1, s)], ["spb"], "spb")
                        P.dma("sp", srb[:, :, :n], G_d[2][s][bsl].rearrange("k p t -> p k t"), [("G", 2, s)], ["srb"], "srb")
                        P.dma("sp", xb[:, :, :n], xsrc[s][bsl].rearrange("k p t -> p k t"), [("XS", s, t0)], ["xb"], "xb")
                        if not isctx:
                            si = 0 if ob == 0 else (2 if ob == 7 else 1)
                            if ob in (0, 1, 7):
                                P.dma("sp", PMb[:], pm_in[si].rearrange("a p o -> p a o"), [], ["PMb"], "PMb")
                        for g in range(4):
                            bnk = g % 2
                            if isctx:
                                lst = [(32 + tt, PMc[:, g * 2 + tt, :]) for tt in range(2)]
                            else:
                                lst = [(4 * ob + rel, PMb[:, slot, :]) for rel, slot in pm_lists[si][g]]
                            def mm(e, lst=lst, g=g, bnk=bnk, n=n):
                                for i2, (tin, pmv) in enumerate(lst):
                                    r = e.matmul(PS[bnk][:, :n], lhsT=Ures[:, tin, g * 128:(g + 1) * 128], rhs=pmv[:, :n],
                                                 start=(i2 == 0), stop=(i2 == len(lst) - 1))
                                return r
                            P.op("pe", mm, ["Ures", "PMb", "PMc"], [("ps", bnk)])
                            P.op("act", lambda e, g=g, bnk=bnk, n=n: e.activation(out=dTb[:, g, :n], in_=PS[bnk][:, :n], func=AF.Copy),
                                 [("ps", bnk)], [("dTb", g)])
                            P.op("pe", lambda e, g=g, bnk=bnk, n=n: e.matmul(PS[2 + bnk][:, :n], lhsT=plw[:, g, :], rhs=dTb[:, g, :n], start=True, stop=True),
                                 [("dTb", g), "plw"], [("ps", 2 + bnk)])
                            P.op("act", lambda e, g=g, bnk=bnk, n=n: e.activation(out=ygb[:, g, :n], in_=PS[2 + bnk][:, :n], func=AF.Identity,
                                                                                   scale=psc[:, g:g + 1]), [("ps", 2 + bnk), "psc"], [("ygb", g)])
                        for oc in range(8):
                            ocs = slice(oc * 128, (oc + 1) * 128)
                            def mmp(e, ocs=ocs, n=n):
                                for g in range(4):
                                    r = e.matmul(PS[4][:, :n], lhsT=wpo[:, g, ocs], rhs=ygb[:, g, :n], start=(g == 0), stop=(g == 3))
                                return r
                            def mmr(e, ocs=ocs, n=n):
                                for k in range(8):
                                    r = e.matmul(PS[5][:, :n], lhsT=wro[:, k, ocs], rhs=zr[:, k, :n], start=(k == 0), stop=(k == 7))
                                return r
                            P.op("pe", mmp, [("ygb", g) for g in range(4)] + ["wpo"], [("ps", 4)])
                            P.op("pe", mmr, ["zr", "wro"], [("ps", 5)])
                            P.op("dve", lambda e, oc=oc, n=n: e.tensor_tensor(out=t1[:, :n], in0=PS[4][:, :n], in1=spb[:, oc, :n], op=ALU.mult),
                                 [("ps", 4), "spb"], ["t1"])
                            P.op("dve", lambda e, oc=oc, n=n: e.tensor_tensor(out=t2[:, :n], in0=PS[5][:, :n], in1=srb[:, oc, :n], op=ALU.mult),
                                 [("ps", 5), "srb"], ["t2"])
                            P.op("dve", lambda e, oc=oc, n=n: e.tensor_tensor(out=mixb[:, oc, :n], in0=t1[:, :n], in1=t2[:, :n], op=ALU.add),
                                 ["t1", "t2"], [("mixb", oc)])
                        for oc2 in range(8):
                            bnk = 6 + oc2 % 2
                            def mmo(e, oc2=oc2, bnk=bnk, n=n):
                                for oc in range(8):
                                    r = e.matmul(PS[bnk][:, :n], lhsT=wo[:, oc, oc2 * 128:(oc2 + 1) * 128], rhs=mixb[:, oc, :n],
                                                 start=(oc == 0), stop=(oc == 7))
                                return r
                            P.op("pe", mmo, [("mixb", oc) for oc in range(8)] + ["wo"], [("ps", bnk)])
                            P.op("dve", lambda e, oc2=oc2, bnk=bnk, n=n, tc=tc: e.scalar_tensor_tensor(
                                out=xb[:, oc2, :n], in0=PS[bnk][:, :n], scalar=modp(2, oc2, tc), in1=xb[:, oc2, :n],
                                op0=ALU.mult, op1=ALU.add), [("ps", bnk), "xb", "MOD"], ["xb"])
                        ln_block(ss, xb, n, 0, 1, 1, 0, 1)
                        P.dma("sp", XS[s][bsl].rearrange("k p t -> p k t"), xb[:, :, :n], ["xb"], [("XS", s, t0)], "xb_st")
                stage_end()
            xsrc = XS
            if stop_after == ("C2", l):
                break

            last = (li == len(layers) - 1)
            if l % 2 == 0:
                with ExitStack() as ls:
                    lsb = lambda name, shape, dtype=F32: ls.enter_context(nc.sbuf_tensor(name + "_L%d" % l, list(shape), dtype))
                    w1 = lsb("w1", [128, 8, DFF], BF16)
                    w3 = lsb("w3", [128, 8, DFF], BF16)
                    w2 = lsb("w2", [128, 22, D], BF16)
                    with ExitStack() as ls2:
                        wst = [ls2.enter_context(nc.sbuf_tensor("wstd%d_L%d" % (i, l), [128, 8, 512], F32)) for i in range(2)]
                        load_cast(w1, w["w1"], 8, DFF, wst, "w1")
                        load_cast(w3, w["w3"], 8, DFF, wst, "w3")
                        load_cast(w2, w["w2"], 22, D, wst, "w2")
                        stage_end()
                    NB = 256
                    xds = [lsb("xd%d" % i, [128, 8, NB]) for i in range(2)]
                    h2 = lsb("h2", [128, 8, NB], BF16)
                    hid = lsb("hid", [128, 22, NB], BF16)
                    sl = [lsb("sl%d" % i, [128, NB]) for i in range(2)]
                    ss = dict(zb=lsb("zbd", [128, 8, NB], BF16), sq=lsb("sqd", [128, 8, NB], BF16),
                              ms=lsb("msd", [128, NB]), m2=lsb("m2d", [128, NB]), rs=lsb("rsd", [128, NB]))
                    bi = 0
                    for s in range(SEQS):
                        ntok = T if need_ctx else L
                        for t0 in range(0, ntok, NB):
                            isctx = t0 >= L
                            tc = 2 if isctx else s
                            xb = xds[bi % 2]
                            xk = ("xd", bi % 2)
                            bsl = (slice(None), slice(None), slice(t0, t0 + NB))
                            P.dma("sp", xb[:], XS[s][bsl].rearrange("k p t -> p k t"), [("XS", s, t0)], [xk], xk)
                            for k in range(8):
                                P.op("act", lambda e, k=k, xb=xb, tc=tc: e.activation(out=h2[:, k, :], in_=xb[:, k, :], func=AF.Identity,
                                                                                      scale=modp(4, k, tc), bias=modp(3, k, tc)), [xk, "MOD"], ["h2"])
                            for ff in range(22):
                                fs = slice(ff * 128, (ff + 1) * 128)
                                b1, b3 = (ff % 2) * 2, (ff % 2) * 2 + 1
                                def mm13(e, fs=fs, b1=b1, b3=b3):
                                    for k in range(8):
                                        e.matmul(PS[b1][:, :NB], lhsT=w1[:, k, fs], rhs=h2[:, k, :], start=(k == 0), stop=(k == 7))
                                    for k in range(8):
                                        r = e.matmul(PS[b3][:, :NB], lhsT=w3[:, k, fs], rhs=h2[:, k, :], start=(k == 0), stop=(k == 7))
                                    return r
                                P.op("pe", mm13, ["h2", "w1", "w3"], [("ps", b1), ("ps", b3)])
                                P.op("act", lambda e, ff=ff, b1=b1: e.activation(out=sl[ff % 2][:], in_=PS[b1][:, :NB], func=AF.Silu), [("ps", b1)], [("sl", ff % 2)])
                                P.op("dve", lambda e, ff=ff, b3=b3: e.tensor_tensor(out=hid[:, ff, :], in0=PS[b3][:, :NB], in1=sl[ff % 2][:], op=ALU.mult),
                                     [("ps", b3), ("sl", ff % 2)], [("hid", ff)])
                            for oc in range(8):
                                bnk = 4 + oc % 4
                                def mm2_(e, oc=oc, bnk=bnk):
                                    for ff in range(22):
                                        r = e.matmul(PS[bnk][:, :NB], lhsT=w2[:, ff, oc * 128:(oc + 1) * 128], rhs=hid[:, ff, :],
                                                     start=(ff == 0), stop=(ff == 21))
                                    return r
                                P.op("pe", mm2_, [("hid", ff) for ff in range(22)] + ["w2"], [("ps", bnk)])
                                P.op("dve", lambda e, oc=oc, bnk=bnk, xb=xb, tc=tc: e.scalar_tensor_tensor(
                                    out=xb[:, oc, :], in0=PS[bnk][:, :NB], scalar=modp(5, oc, tc), in1=xb[:, oc, :],
                                    op0=ALU.mult, op1=ALU.add), [("ps", bnk), xk, "MOD"], [xk])
                            ln_block(ss, xb, NB, 2, 3, 1, 0, 1, XK=xk)
                            if last:
                                if not isctx:
                                    P.dma("sp", outT[s][bsl].rearrange("k p t -> p k t"), xb[:], [xk], [("OUT",)], ("xd_st", bi % 2))
                            else:
                                P.dma("sp", XS[s][bsl].rearrange("k p t -> p k t"), xb[:], [xk], [("XS", s, t0)], ("xd_st", bi % 2))
                            bi += 1
                    stage_end()
            else:
                with ExitStack() as ls:
                    lsb = lambda name, shape, dtype=F32: ls.enter_context(nc.sbuf_tensor(name + "_L%d" % l, list(shape), dtype))
                    NB = 512
                    wrb = lsb("wrb", [128, 8, NEXP], BF16)
                    wrf = lsb("wrf", [128, 8, NEXP])
                    P.dma("sp", wrf[:], w["wr"].rearrange("(k p) e -> p k e", p=128), [], ["wrf"], "wrf")
                    P.op("dve", lambda e: e.tensor_copy(out=wrb[:], in_=wrf[:]), ["wrf"], ["wrb"])
                    wst = [lsb("wste%d" % i, [128, 4096]) for i in range(3)]
                    wsl = [[lsb("wsl%d_%d" % (i, j), [128, 4096], BF16) for j in range(3)] for i in range(2)]
                    xe = lsb("xe", [128, 8, 1024])
                    h2 = lsb("h2e", [128, 8, 1024], BF16)
                    gw = lsb("gw", [128, NEXP, 1024])
                    hid = [lsb("hide%d" % i, [128, 4, NB], BF16) for i in range(2)]
                    sl = [lsb("sle%d" % i, [128, NB]) for i in range(2)]
                    tg = [lsb("tge%d" % i, [128, NB]) for i in range(2)]
                    lgs = lsb("lgs", [128, 8])
                    mx8 = lsb("mx8", [128, 8])
                    dd = lsb("dd", [128, 4])
                    gte = lsb("gte", [128, 2, 8])
                    gbc = lsb("gbc", [128, 8, 128])
                    ss = dict(zb=lsb("zbe", [128, 8, 128], BF16), sq=lsb("sqe", [128, 8, 128], BF16),
                              ms=lsb("mse", [128, 128]), m2=lsb("m2e", [128, 128]), rs=lsb("rse", [128, 128]))
                    sbs = [[(s, q * 1024 + b * NB, NB, s) for b in range(2)] for s in range(SEQS) for q in range(4)]
                    if need_ctx:
                        sbs.append([(0, L, NCTX, 2), (1, L, NCTX, 2)])
                    jobs = [(sbi, ex, fsl) for sbi in range(len(sbs)) for ex in range(NEXP) for fsl in range(7)]

                    def load_dma(ji):
                        sbi, ex, fsl = jobs[ji]
                        j = ji % 2
                        if sbi == 0:
                            srcs = (w["w1"][ex].rearrange("(k p) n -> p k n", p=128)[:, :, fsl * 512:(fsl + 1) * 512],
                                    w["w3"][ex].rearrange("(k p) n -> p k n", p=128)[:, :, fsl * 512:(fsl + 1) * 512],
                                    w["w2"][ex, fsl * 512:(fsl + 1) * 512, :].rearrange("(c p) n -> p c n", p=128))
                            for m3 in range(3):
                                bdim = 512 if m3 < 2 else 1024
                                P.dma("sp", wst[m3][:].rearrange("p (a b) -> p a b", b=bdim), srcs[m3], [], [("wste", m3)], ("wste", m3))

                    def load_job(ji):
                        sbi, ex, fsl = jobs[ji]
                        j = ji % 2
                        if sbi == 0:
                            for m3 in range(3):
                                P.op("act", lambda e, j=j, m3=m3: e.activation(out=wsl[j][m3][:], in_=wst[m3][:], func=AF.Copy),
                                     [("wste", m3)], [("wsl", j, m3)])
                                P.dma("sp", WC_d[ex, fsl, m3], wsl[j][m3][:], [("wsl", j, m3)], [("WC", ex, fsl, m3)], ("wc_st", j, m3))
                            if ji + 1 < len(jobs):
                                load_dma(ji + 1)
                        else:
                            for m3 in range(3):
                                P.dma("sp", wsl[j][m3][:], WC_d[ex, fsl, m3], [("WC", ex, fsl, m3)], [("wsl", j, m3)], ("wsl", j, m3))

                    hi = 0
                    load_dma(0)
                    load_job(0)
                    for ji, (sbi, ex, fsl) in enumerate(jobs):
                        blks = sbs[sbi]
                        if ex == 0 and fsl == 0:
                            off = 0
                            for bix, (s, t0, n, tc) in enumerate(blks):
                                bs = slice(off, off + n)
                                P.dma("sp", xe[:, :, bs], XS[s, :, :, t0:t0 + n].rearrange("k p t -> p k t"), [("XS", s, t0)], [("xe", bix)], ("xe", bix))
                                for k in range(8):
                                    P.op("act", lambda e, k=k, bs=bs, tc=tc: e.activation(out=h2[:, k, bs], in_=xe[:, k, bs], func=AF.Identity,
                                                                                          scale=modp(4, k, tc), bias=modp(3, k, tc)),
                                         [("xe", bix), "MOD"], [("h2e", bix)])
                                for tt in range(n // 128):
                                    ts_ = slice(off + tt * 128, off + tt * 128 + 128)
                                    def mmr(e, ts_=ts_):
                                        for k in range(8):
                                            r = e.matmul(PS[6][:, 0:8], lhsT=h2[:, k, ts_], rhs=wrb[:, k, :], start=(k == 0), stop=(k == 7))
                                        return r
                                    P.op("pe", mmr, [("h2e", bix), "wrb"], [("ps", 6)])
                                    P.op("act", lambda e: e.activation(out=lgs[:], in_=PS[6][:, 0:8], func=AF.Copy), [("ps", 6)], ["lgs"])
                                    P.op("dve", lambda e: e.max(out=mx8[:], in_=lgs[:]), ["lgs"], ["mx8"])
                                    P.op("dve", lambda e: e.tensor_tensor(out=dd[:, 0:1], in0=mx8[:, 0:1], in1=mx8[:, 1:2], op=ALU.subtract), ["mx8"], ["dd0"])
                                    P.op("act", lambda e: e.activation(out=dd[:, 1:2], in_=dd[:, 0:1], func=AF.Sigmoid), ["dd0"], ["dd1"])
                                    P.op("act", lambda e: e.activation(out=dd[:, 2:3], in_=dd[:, 0:1], func=AF.Sigmoid, scale=-1.0), ["dd0"], ["dd2"])
                                    P.op("dve", lambda e: e.tensor_scalar(out=gte[:, 0, :], in0=lgs[:], scalar1=mx8[:, 0:1], scalar2=dd[:, 1:2],
                                                                          op0=ALU.is_equal, op1=ALU.mult), ["lgs", "mx8", "dd1"], ["gte0"])
                                    P.op("dve", lambda e: e.tensor_scalar(out=gte[:, 1, :], in0=lgs[:], scalar1=mx8[:, 1:2], scalar2=dd[:, 2:3],
                                                                          op0=ALU.is_equal, op1=ALU.mult), ["lgs", "mx8", "dd2"], ["gte1"])
                                    P.op("dve", lambda e: e.tensor_tensor(out=gte[:, 0, :], in0=gte[:, 0, :], in1=gte[:, 1, :], op=ALU.add), ["gte0", "gte1"], ["gte0"])
                                    P.op("dve", lambda e: e.tensor_copy(out=gbc[:], in_=gte[:, 0, :].unsqueeze(2).to_broadcast([128, 8, 128])), ["gte0"], ["gbc"])
                                    for hb2 in range(2):
                                        def mmb(e, hb2=hb2):
                                            for q in range(4):
                                                r = e.matmul(PS[4 + hb2][:, q * 128:(q + 1) * 128], lhsT=gbc[:, hb2 * 4 + q, :], rhs=ident_f[:], start=True, stop=True)
                                            return r
                                        P.op("pe", mmb, ["gbc", "ident_f"], [("ps", 4 + hb2)])
                                        P.op("act", lambda e, hb2=hb2, ts_=ts_: e.activation(out=gw[:, hb2 * 4:(hb2 + 1) * 4, ts_],
                                                                                            in_=PS[4 + hb2][:, :].rearrange("p (a b) -> p a b", b=128), func=AF.Copy),
                                             [("ps", 4 + hb2)], [("gw", bix)])
                                off += n
                        if ji + 1 < len(jobs):
                            load_job(ji + 1)
                        j = ji % 2
                        w1s = wsl[j][0][:].rearrange("p (a b) -> p a b", b=512)
                        w3s = wsl[j][1][:].rearrange("p (a b) -> p a b", b=512)
                        w2s = wsl[j][2][:].rearrange("p (a b) -> p a b", b=1024)
                        off = 0
                        for bix, (s, t0, n, tc) in enumerate(blks):
                            bs = slice(off, off + n)
                            hj = hi % 2
                            for c4 in range(4):
                                cs = slice(c4 * 128, (c4 + 1) * 128)
                                b1, b3 = (c4 % 2) * 2, (c4 % 2) * 2 + 1
                                def mm13(e, cs=cs, b1=b1, b3=b3, bs=bs, w1s=w1s, w3s=w3s, n=n):
                                    for k in range(8):
                                        e.matmul(PS[b1][:, :n], lhsT=w1s[:, k, cs], rhs=h2[:, k, bs], start=(k == 0), stop=(k == 7))
                                    for k in range(8):
                                        r = e.matmul(PS[b3][:, :n], lhsT=w3s[:, k, cs], rhs=h2[:, k, bs], start=(k == 0), stop=(k == 7))
                                    return r
                                P.op("pe", mm13, [("h2e", bix), ("wsl", j, 0), ("wsl", j, 1)], [("ps", b1), ("ps", b3)])
                                P.op("act", lambda e, c4=c4, b1=b1, n=n: e.activation(out=sl[c4 % 2][:, :n], in_=PS[b1][:, :n], func=AF.Silu), [("ps", b1)], [("sle", c4 % 2)])
                                P.op("pool", lambda e, c4=c4, ex=ex, bs=bs, n=n: e.tensor_tensor(out=tg[c4 % 2][:, :n], in0=sl[c4 % 2][:, :n], in1=gw[:, ex, bs], op=ALU.mult),
                                     [("sle", c4 % 2), ("gw", bix)], [("tge", c4 % 2)])
                                P.op("dve", lambda e, c4=c4, hj=hj, b3=b3, n=n: e.tensor_tensor(out=hid[hj][:, c4, :n], in0=PS[b3][:, :n], in1=tg[c4 % 2][:, :n], op=ALU.mult),
                                     [("ps", b3), ("tge", c4 % 2)], [("hide", hj, c4)])
                            for oc in range(8):
                                bnk = 4 + oc % 4
                                def mm2_(e, oc=oc, bnk=bnk, hj=hj, w2s=w2s, n=n):
                                    for c4 in range(4):
                                        r = e.matmul(PS[bnk][:, :n], lhsT=w2s[:, c4, oc * 128:(oc + 1) * 128], rhs=hid[hj][:, c4, :n],
                                                     start=(c4 == 0), stop=(c4 == 3))
                                    return r
                                P.op("pe", mm2_, [("hide", hj, c4) for c4 in range(4)] + [("wsl", j, 2)], [("ps", bnk)])
                                P.op("dve", lambda e, oc=oc, bnk=bnk, bs=bs, tc=tc, n=n: e.scalar_tensor_tensor(
                                    out=xe[:, oc, bs], in0=PS[bnk][:, :n], scalar=modp(5, oc, tc), in1=xe[:, oc, bs],
                                    op0=ALU.mult, op1=ALU.add), [("ps", bnk), ("xe", bix), "MOD"], [("xe", bix)])
                            hi += 1
                            off += n
                        if ex == NEXP - 1 and fsl == 6:
                            off = 0
                            for bix, (s, t0, n, tc) in enumerate(blks):
                                for sub in range(n // 128):
                                    bs = slice(off + sub * 128, off + sub * 128 + 128)
                                    tt0 = t0 + sub * 128
                                    ln_block(ss, xe[:, :, bs], 128, 2, 3, 1, 0, 1, XK=("xe", bix))
                                    if last:
                                        if tc != 2:
                                            P.dma("sp", outT[s, :, :, tt0:tt0 + 128].rearrange("k p t -> p k t"), xe[:, :, bs], [("xe", bix)], [("OUT", s, tt0)], ("xe_st", bix))
                                    else:
                                        P.dma("sp", XS[s, :, :, tt0:tt0 + 128].rearrange("k p t -> p k t"), xe[:, :, bs], [("xe", bix)], [("XSo", s, tt0)], ("xe_st", bix))
                                off += n
                    stage_end()
        if stop_after is not None:
            dbg = dt("dbgXS", [SEQS, 8, 128, T], F32, kind="ExternalOutput").ap()
            for s_ in range(SEQS):
                P.dma("sp", dbg[s_], XS[s_], [], [("dbg", s_)], ("dbg", s_))
        P.barrier()
        P.emit(block)
    return nc


def _host_inputs(inputs, layers):
    f32 = np.float32
    x = np.asarray(inputs["x"], f32)
    ctx = np.asarray(inputs["ctx"], f32)
    c = np.asarray(inputs["c"], f32)
    c_ctx = np.asarray(inputs["c_ctx"], f32)
    PM, pm_lists, PMC = _pool_constants()
    cosT, sinT = _rope_tables()
    E, masks = _misc_constants()
    common = dict(cosT=cosT, sinT=sinT, etab=E, masks=masks, pm=PM, pmc=PMC, ident=np.eye(128, dtype=f32))
    for l in layers:
        common["ada_w%d" % l] = np.ascontiguousarray(inputs["ada_w"][l], f32)
        common["ada_bT%d" % l] = np.ascontiguousarray(np.asarray(inputs["ada_b"][l], f32).reshape(48, 128).T)
        common["w_in%d" % l] = np.ascontiguousarray(inputs["w_in"][l], f32)
        common["pool_w%d" % l] = np.ascontiguousarray(inputs["pool_w"][l], f32)
        common["pscT%d" % l] = np.ascontiguousarray(np.asarray(inputs["pool_scale"][l], f32).reshape(4, 128).T)
        common["w_pool_out%d" % l] = np.ascontiguousarray(inputs["w_pool_out"][l], f32)
        common["w_ret_out%d" % l] = np.ascontiguousarray(inputs["w_ret_out"][l], f32)
        common["logit_bc%d" % l] = np.ascontiguousarray(
            np.broadcast_to(np.asarray(inputs["ret_decay_logit"][l], f32).reshape(1, 8), (128, 8)))
        common["w_out%d" % l] = np.ascontiguousarray(inputs["w_out"][l], f32)
        lnT = np.stack([np.asarray(inputs[k][l], f32).reshape(8, 128).T
                        for k in ("ln_mix_g", "ln_mix_b", "ln_ffn_g", "ln_ffn_b")], axis=1)
        common["lnT%d" % l] = np.ascontiguousarray(lnT)
        if l % 2 == 0:
            common["ffn_w1_%d" % l] = np.ascontiguousarray(inputs["ffn_w1"][l // 2], f32)
            common["ffn_w3_%d" % l] = np.ascontiguousarray(inputs["ffn_w3"][l // 2], f32)
            common["ffn_w2_%d" % l] = np.ascontiguousarray(inputs["ffn_w2"][l // 2], f32)
        else:
            common["moe_router%d" % l] = np.ascontiguousarray(inputs["moe_router"][l // 2], f32)
            common["moe_w1_%d" % l] = np.ascontiguousarray(inputs["moe_w1"][l // 2], f32)
            common["moe_w3_%d" % l] = np.ascontiguousarray(inputs["moe_w3"][l // 2], f32)
            common["moe_w2_%d" % l] = np.ascontiguousarray(inputs["moe_w2"][l // 2], f32)
    in_maps = []
    for core in range(NCORES):
        m = dict(common)
        xs = []
        for s in range(SEQS):
            b = core * SEQS + s
            xt = np.concatenate([x[b].T, ctx[b].T], axis=1)
            xs.append(xt.reshape(8, 128, T))
        m["xT"] = np.ascontiguousarray(np.stack(xs))
        cs = np.stack([c[core * SEQS], c[core * SEQS + 1], c_ctx], axis=1)
        m["cT"] = np.ascontiguousarray(cs.reshape(8, 128, 3).transpose(1, 0, 2))
        in_maps.append(m)
    return in_maps, pm_lists, PM.shape[1]


def kernel(**inputs):
    layers = (0, 1, 2, 3)
    in_maps, pm_lists, pm_slots = _host_inputs(inputs, layers)
    nc = build(layers, None, pm_lists, pm_slots)
    res = run_bass_kernel_spmd(nc, in_maps, core_ids=list(range(NCORES)))
    out = np.empty((NCORES * SEQS, L, D), np.float32)
    for core in range(NCORES):
        o = res.results[core]["outT"]
        for s in range(SEQS):
            out[core * SEQS + s] = o[s].reshape(D, L).T
    return out
```

```python
import numpy as np
import ml_dtypes
from contextlib import ExitStack
import concourse.bass as bass
import concourse.mybir as mybir
from concourse.bass_utils import run_bass_kernel_spmd

F32 = mybir.dt.float32
BF16 = mybir.dt.bfloat16
AF = mybir.ActivationFunctionType
ALU = mybir.AluOpType

D = 1024
L = 4096
NCTX = 256
T = L + NCTX
NT = T // 128
DEPTH = 4
GRID = 64
WINS = (2, 4, 8, 16)
DFF = 2816
EFF = 3584
NEXP = 8
ALPHA = (2 * DEPTH) ** 0.25
EPS = 1e-5
NCORES = 8
SEQS = 2


class Prog:
    ENGS = ("pe", "dve", "act", "pool", "sp")

    def __init__(self, nc, es):
        self.nc = nc
        self.es = es
        self.streams = {e: [] for e in self.ENGS}
        self.cnt = {e: 0 for e in self.ENGS}
        self.sem = {e: es.enter_context(nc.semaphore("s_" + e)) for e in ("pe", "dve", "act", "pool")}
        self.dsem = {}
        self.res = {}
        self.known = {e: {} for e in self.ENGS}

    def _dma_sem(self, key):
        if key not in self.dsem:
            self.dsem[key] = [self.es.enter_context(self.nc.semaphore("d%d" % len(self.dsem))), 0]
        return self.dsem[key]

    def _need(self, eng, toks):
        need = {}
        for kind, (teng, sem, val) in toks:
            if teng == eng and eng in ("pe", "sp"):
                continue
            k = id(sem)
            if k not in need or need[k][1] < val:
                need[k] = (sem, val)
        out = []
        kn = self.known[eng]
        for k, (sem, val) in need.items():
            if kn.get(k, 0) >= val:
                continue
            kn[k] = val
            out.append((sem, val))
        return out

    def _deps(self, eng, reads, writes):
        toks = []
        for r in reads:
            st = self.res.get(r)
            if st and st["w"] is not None:
                toks.append(("raw", st["w"]))
        for w in writes:
            st = self.res.get(w)
            if st:
                if st["w"] is not None:
                    toks.append(("waw", st["w"]))
                for t in st["r"]:
                    toks.append(("war", t))
        return self._need(eng, toks)

    def _commit(self, tok, reads, writes):
        for r in reads:
            st = self.res.setdefault(r, {"w": None, "r": []})
            st["r"].append(tok)
        for w in writes:
            self.res[w] = {"w": tok, "r": []}

    def op(self, eng, fn, reads=(), writes=()):
        waits = self._deps(eng, reads, writes)
        self.cnt[eng] += 1
        sem = self.sem[eng]
        self.streams[eng].append((waits, fn, sem, 1))
        self._commit((eng, sem, self.cnt[eng]), reads, writes)

    def dma(self, q, out, in_, reads, writes, key, **kw):
        waits = self._deps(q, reads, writes)
        ds = self._dma_sem(key)
        ds[1] += 16
        sem, val = ds[0], ds[1]

        def fn(e, out=out, in_=in_, kw=kw):
            return e.dma_start(out=out, in_=in_, **kw)

        self.streams[q].append((waits, fn, sem, 16))
        self._commit(("dma", sem, val), reads, writes)

    def barrier(self):
        toks = []
        for e in ("pe", "dve", "act", "pool"):
            if self.cnt[e]:
                toks.append(("raw", (e, self.sem[e], self.cnt[e])))
        for key, (sem, val) in self.dsem.items():
            if val:
                toks.append(("raw", ("dma", sem, val)))
        for e in self.ENGS:
            waits = self._need(e, [t for t in toks if t[1][0] != e or e == "sp"])
            if waits:
                self.streams[e].append((waits, None, None, 0))
        self.res = {}

    def emit(self, block):
        engs = {"pe": block.tensor, "dve": block.vector, "act": block.scalar, "pool": block.gpsimd, "sp": block.sync}
        for name, deco in engs.items():
            stream = self.streams[name]
            if not stream:
                continue

            def body(e, stream=stream):
                for waits, fn, sem, inc in stream:
                    for ws, wv in waits:
                        e.wait_ge(ws, wv)
                    if fn is not None:
                        fn(e).then_inc(sem, inc)

            deco(body)
            self.streams[name] = []


def _box_matrix(n, w):
    left = w // 2
    right = w - 1 - left
    A = np.zeros((n, n), np.float64)
    for t in range(n):
        lo = max(t - left, 0)
        hi = min(t + right + 1, n)
        A[t, lo:hi] = 1.0 / (hi - lo)
    return A


def _pool_constants():
    sets = []
    lists = []
    mats = []
    for si, ob in enumerate((0, 3, 7)):
        lst_g = []
        for g, w in enumerate(WINS):
            A = _box_matrix(GRID, w)
            lst = []
            rows_out = np.arange(8 * ob, 8 * ob + 8)
            for tin in range(32):
                rows_in = np.arange(2 * tin, 2 * tin + 2)
                Ar = A[np.ix_(rows_out, rows_in)]
                if not np.any(Ar):
                    continue
                M = np.kron(Ar, A)
                if tin // 4 == ob:
                    o0 = (tin - 4 * ob) * 128
                    M[o0:o0 + 128, :] -= np.eye(128)
                lst.append((tin - 4 * ob, len(mats)))
                mats.append(M.T.astype(np.float32))
            lst_g.append(lst)
        lists.append(lst_g)
    out_sets = []
    out_lists = []
    for si in range(3):
        slots = []
        lg = []
        for g in range(4):
            l2 = []
            for rel, mi in lists[si][g]:
                l2.append((rel, len(slots)))
                slots.append(mats[mi])
            lg.append(l2)
        out_sets.append(np.stack(slots))
        out_lists.append(lg)
    nmax = max(s.shape[0] for s in out_sets)
    PM = np.zeros((3, nmax, 128, 512), np.float32)
    for si in range(3):
        PM[si, :out_sets[si].shape[0]] = out_sets[si]
    PMC = np.zeros((4, 2, 128, 256), np.float32)
    for g, w in enumerate(WINS):
        A = _box_matrix(NCTX, w) - np.eye(NCTX)
        for tin in range(2):
            PMC[g, tin] = A[:, tin * 128:(tin + 1) * 128].T
    return PM.astype(ml_dtypes.bfloat16), out_lists, PMC.astype(ml_dtypes.bfloat16)


def _rope_tables():
    t = np.arange(L)
    row = (t // GRID).astype(np.float32)
    col = (t % GRID).astype(np.float32)
    n_freq = 32
    inv = np.exp(-np.log(np.float32(10000.0)) * np.arange(n_freq, dtype=np.float32) / n_freq).astype(np.float32)
    ang = np.concatenate([row[:, None] * inv, col[:, None] * inv], -1).astype(np.float32)
    cos = np.ones((T, 64), np.float32)
    sin = np.zeros((T, 64), np.float32)
    cos[:L] = np.cos(ang)
    sin[:L] = np.sin(ang)
    cosT = np.ascontiguousarray(cos.reshape(NT, 128, 64).transpose(1, 0, 2))
    sinT = np.ascontiguousarray(sin.reshape(NT, 128, 64).transpose(1, 0, 2))
    return cosT, sinT


def _misc_constants():
    j = np.arange(128, dtype=np.float32)
    E = np.zeros((128, 16), np.float32)
    for h in range(4):
        E[:, 0 + h] = j - 127.0
        E[:, 4 + h] = -j
        E[:, 8 + h] = 127.0 - j
        E[:, 12 + h] = j
    jj = np.arange(128)[:, None]
    ii = np.arange(128)[None, :]
    masks = np.zeros((128, 2, 128), np.float32)
    masks[:, 0, :] = (ii >= jj)
    masks[:, 1, :] = (ii <= jj)
    return E, masks


def build(layers=(0, 1, 2, 3), stop_after=None, pm_lists=None, pm_slots=31):
    nc = bass.Bass("TRN2", target_bir_lowering=False)
    dt = nc.dram_tensor

    def inp(name, shape, dtype=F32):
        return dt(name, list(shape), dtype, kind="ExternalInput").ap()

    def scr(name, shape, dtype):
        return dt(name, list(shape), dtype).ap()

    xT_in = inp("xT", [SEQS, 8, 128, T])
    cT_in = inp("cT", [128, 8, 3])
    cos_in = inp("cosT", [128, NT, 64])
    sin_in = inp("sinT", [128, NT, 64])
    etab_in = inp("etab", [128, 16])
    mask_in = inp("masks", [128, 2, 128])
    pm_in = inp("pm", [3, pm_slots, 128, 512], BF16)
    pmc_in = inp("pmc", [4, 2, 128, 256], BF16)
    ident_in = inp("ident", [128, 128])
    W = {}
    for l in layers:
        W[l] = dict(
            ada_w=inp("ada_w%d" % l, [D, 6 * D]),
            ada_b=inp("ada_bT%d" % l, [128, 48]),
            w_in=inp("w_in%d" % l, [D, 5632]),
            pool_w=inp("pool_w%d" % l, [4, 128, 128]),
            psc=inp("pscT%d" % l, [128, 4]),
            wpo=inp("w_pool_out%d" % l, [512, D]),
            wro=inp("w_ret_out%d" % l, [D, D]),
            logit=inp("logit_bc%d" % l, [128, 8]),
            wo=inp("w_out%d" % l, [D, D]),
            lnp=inp("lnT%d" % l, [128, 4, 8]),
        )
        if l % 2 == 0:
            W[l].update(
                w1=inp("ffn_w1_%d" % l, [D, DFF]),
                w3=inp("ffn_w3_%d" % l, [D, DFF]),
                w2=inp("ffn_w2_%d" % l, [DFF, D]),
            )
        else:
            W[l].update(
                wr=inp("moe_router%d" % l, [D, NEXP]),
                w1=inp("moe_w1_%d" % l, [NEXP, D, EFF]),
                w3=inp("moe_w3_%d" % l, [NEXP, D, EFF]),
                w2=inp("moe_w2_%d" % l, [NEXP, EFF, D]),
            )
    outT = dt("outT", [SEQS, 8, 128, L], F32, kind="ExternalOutput").ap()

    XS = scr("XS", [SEQS, 8, 128, T], F32)
    U_d = scr("U_d", [SEQS, T, 512], BF16)
    V_d = scr("V_d", [SEQS, T, 1024], BF16)
    KF_d = scr("KF_d", [SEQS, T, 512], BF16)
    KB_d = scr("KB_d", [SEQS, T, 512], BF16)
    QKT_d = scr("QKT_d", [SEQS, 16, 128, T], BF16)
    G_d = [scr("G%d_d" % i, [SEQS, 8, 128, T], BF16) for i in range(3)]
    DST_d = scr("DST_d", [SEQS, 2, NT, 4, 128, 256], BF16)
    ZR_d = scr("ZR_d", [SEQS, 8, 128, T], BF16)
    WC_d = scr("WC_d", [NEXP, 7, 3, 128, 4096], BF16)

    blocks = [(i * 512, 512, False) for i in range(8)] + [(L, NCTX, True)]

    with ExitStack() as es:
        P = Prog(nc, es)
        block = es.enter_context(nc.Block())
        sb = lambda name, shape, dtype=F32: es.enter_context(nc.sbuf_tensor(name, list(shape), dtype))
        ident_f = sb("ident_f", [128, 128])
        ident_b = sb("ident_b", [128, 128], BF16)
        ones_b = sb("ones_b", [128, 128], BF16)
        ones_f = sb("ones_f", [128, 128])
        masks = sb("masks_s", [128, 2, 128])
        etab = sb("etab_s", [128, 16])
        cT = sb("cT_s", [128, 8, 3])
        sT = sb("sT_s", [128, 8, 3])
        MOD = sb("MOD", [128, 48, 3])
        lnp = sb("lnp", [128, 4, 8])
        psc = sb("psc", [128, 4])
        lgt = sb("lgt", [128, 8])
        lg16 = sb("lg16", [128, 16])
        DEC = sb("DEC", [128, 16])
        GC = sb("GC", [128, 8])
        epsb = sb("epsb", [128, 2])
        PS = [es.enter_context(nc.psum_tensor("ps%d" % i, [128, 512], F32)) for i in range(8)]

        P.dma("sp", ident_f[:], ident_in, [], ["ident_f"], "c0")
        P.dma("sp", masks[:], mask_in, [], ["masks"], "c1")
        P.dma("sp", etab[:], etab_in, [], ["etab"], "c2")
        P.dma("sp", cT[:], cT_in, [], ["cT"], "c3")
        P.op("dve", lambda e: e.tensor_copy(out=ident_b[:], in_=ident_f[:]), ["ident_f"], ["ident_b"])
        P.op("dve", lambda e: e.memset(ones_b[:], 1.0 / 1024.0), [], ["ones_b"])
        P.op("dve", lambda e: e.memset(ones_f[:], 1.0 / 256.0), [], ["ones_f"])
        P.op("dve", lambda e: e.memset(epsb[:, 0:1], EPS), [], ["epsb0"])
        P.op("dve", lambda e: e.memset(epsb[:, 1:2], EPS / (ALPHA * ALPHA)), [], ["epsb1"])
        P.op("act", lambda e: e.activation(out=sT[:], in_=cT[:], func=AF.Silu), ["cT"], ["sT"])

        def stage_end():
            P.barrier()
            P.emit(block)

        def ln_block(ss, xb, n, gk, bk, epscol, pm_i, pe_i, XK="xb"):
            zb, sq, ms, m2, rs = ss["zb"], ss["sq"], ss["ms"], ss["m2"], ss["rs"]
            P.op("act", lambda e: e.activation(out=zb[:, :, :n], in_=xb[:, :, :n], func=AF.Copy), [XK], ["zb"])
            P.op("act", lambda e: e.activation(out=sq[:, :, :n], in_=xb[:, :, :n], func=AF.Square), [XK], ["sq"])

            def mm1(e):
                for k in range(8):
                    r = e.matmul(PS[pm_i][:, :n], lhsT=ones_b[:], rhs=zb[:, k, :n], start=(k == 0), stop=(k == 7))
                return r

            def mm2(e):
                for k in range(8):
                    r = e.matmul(PS[pe_i][:, :n], lhsT=ones_b[:], rhs=sq[:, k, :n], start=(k == 0), stop=(k == 7))
                return r

            P.op("pe", mm1, ["zb", "ones_b"], [("ps", pm_i)])
            P.op("pe", mm2, ["sq", "ones_b"], [("ps", pe_i)])
            P.op("act", lambda e: e.activation(out=ms[:, :n], in_=PS[pm_i][:, :n], func=AF.Copy), [("ps", pm_i)], ["ms"])
            P.op("act", lambda e: e.activation(out=m2[:, :n], in_=PS[pm_i][:, :n], func=AF.Square), [("ps", pm_i)], ["m2"])
            P.op("dve", lambda e: e.tensor_tensor(out=rs[:, :n], in0=PS[pe_i][:, :n], in1=m2[:, :n], op=ALU.subtract),
                 [("ps", pe_i), "m2"], ["rs"])
            P.op("act", lambda e: e.activation(out=rs[:, :n], in_=rs[:, :n], func=AF.Ln, bias=epsb[:, epscol:epscol + 1]),
                 ["rs", "epsb%d" % epscol], ["rs"])
            P.op("act", lambda e: e.activation(out=rs[:, :n], in_=rs[:, :n], func=AF.Exp, scale=-0.5), ["rs"], ["rs"])
            P.op("dve", lambda e: e.tensor_tensor(out=xb[:, :, :n], in0=xb[:, :, :n],
                                                  in1=ms[:, :n].unsqueeze(1).to_broadcast([128, 8, n]), op=ALU.subtract),
                 [XK, "ms"], [XK])
            P.op("dve", lambda e: e.tensor_tensor(out=xb[:, :, :n], in0=xb[:, :, :n],
                                                  in1=rs[:, :n].unsqueeze(1).to_broadcast([128, 8, n]), op=ALU.mult),
                 [XK, "rs"], [XK])
            for k in range(8):
                P.op("act", lambda e, k=k: e.activation(out=xb[:, k, :n], in_=xb[:, k, :n], func=AF.Identity,
                                                         scale=lnp[:, gk, k:k + 1], bias=lnp[:, bk, k:k + 1]),
                     [XK, "lnp"], [XK])

        def load_cast(dst, src_ap, nk, ncols, stage, tag, piece=512):
            srcv = src_ap.rearrange("(k p) n -> p k n", p=128)
            i = 0
            for c0 in range(0, ncols, piece):
                cw = min(piece, ncols - c0)
                for k0 in range(0, nk, 8):
                    kw = min(8, nk - k0)
                    st = stage[i % 2]
                    P.dma("sp", st[:, :kw, :cw], srcv[:, k0:k0 + kw, c0:c0 + cw], [], [("wst", i % 2)], ("wst", i % 2))
                    if i % 2 == 0:
                        P.op("act", lambda e, st=st, kw=kw, cw=cw, k0=k0, c0=c0: e.activation(
                            out=dst[:, k0:k0 + kw, c0:c0 + cw], in_=st[:, :kw, :cw], func=AF.Copy), [("wst", i % 2)], [(tag, i)])
                    else:
                        P.op("dve", lambda e, st=st, kw=kw, cw=cw, k0=k0, c0=c0: e.tensor_copy(
                            out=dst[:, k0:k0 + kw, c0:c0 + cw], in_=st[:, :kw, :cw]), [("wst", i % 2)], [(tag, i)])
                    i += 1

        for li, l in enumerate(layers):
            w = W[l]
            xsrc = xT_in if li == 0 else XS
            need_ctx = l < DEPTH - 1
            with ExitStack() as ls:
                lsb = lambda name, shape, dtype=F32: ls.enter_context(nc.sbuf_tensor(name + "_L%d" % l, list(shape), dtype))
                adst = [lsb("adst%d" % i, [128, 8, 768]) for i in range(2)]
                adb = lsb("adb", [128, 48])
                P.dma("sp", adb[:], w["ada_b"], [], ["adb"], "p0")
                P.dma("sp", lnp[:], w["lnp"], [], ["lnp"], "p1")
                P.dma("sp", psc[:], w["psc"], [], ["psc"], "p2")
                P.dma("sp", lgt[:], w["logit"], [], ["lgt"], "p3")
                adv = w["ada_w"].rearrange("(k p) n -> p k n", p=128)
                for pc in range(8):
                    st = adst[pc % 2]
                    P.dma("sp", st[:], adv[:, :, pc * 768:(pc + 1) * 768], [], [("adst", pc % 2)], ("adst", pc % 2))

                    def mm(e, st=st, pc=pc):
                        for oc in range(6):
                            og = pc * 6 + oc
                            for k in range(8):
                                r = e.matmul(PS[0][:, og * 3:og * 3 + 3], lhsT=st[:, k, oc * 128:(oc + 1) * 128],
                                             rhs=sT[:, k, :], start=(k == 0), stop=(k == 7))
                        return r

                    P.op("pe", mm, [("adst", pc % 2), "sT"], [("ps", 0)])
                P.op("dve", lambda e: e.tensor_tensor(out=MOD[:], in0=PS[0][:, 0:144].rearrange("p (a b) -> p a b", b=3),
                                                      in1=adb[:].unsqueeze(2).to_broadcast([128, 48, 3]), op=ALU.add),
                     [("ps", 0), "adb"], ["MOD"])
                P.op("dve", lambda e: e.tensor_scalar(out=MOD[:, 8:16, :], in0=MOD[:, 8:16, :], scalar1=1.0, scalar2=None, op0=ALU.add), ["MOD"], ["MOD"])
                P.op("dve", lambda e: e.tensor_scalar(out=MOD[:, 16:24, :], in0=MOD[:, 16:24, :], scalar1=1.0 / ALPHA, scalar2=None, op0=ALU.mult), ["MOD"], ["MOD"])
                P.op("dve", lambda e: e.tensor_scalar(out=MOD[:, 32:40, :], in0=MOD[:, 32:40, :], scalar1=1.0, scalar2=None, op0=ALU.add), ["MOD"], ["MOD"])
                P.op("dve", lambda e: e.tensor_scalar(out=MOD[:, 40:48, :], in0=MOD[:, 40:48, :], scalar1=1.0 / ALPHA, scalar2=None, op0=ALU.mult), ["MOD"], ["MOD"])
                P.op("act", lambda e: e.activation(out=lgt[:], in_=lgt[:], func=AF.Exp, scale=-1.0), ["lgt"], ["lgt"])
                P.op("act", lambda e: e.activation(out=lgt[:], in_=lgt[:], func=AF.Ln, bias=1.0), ["lgt"], ["lgt"])
                P.op("dve", lambda e: e.tensor_scalar(out=lg16[:, 0:8], in0=lgt[:], scalar1=-1.0, scalar2=None, op0=ALU.mult), ["lgt"], ["lg16"])
                P.op("dve", lambda e: e.tensor_scalar(out=lg16[:, 8:16], in0=lgt[:], scalar1=-1.0, scalar2=None, op0=ALU.mult), ["lgt", "lg16"], ["lg16"])
                P.op("act", lambda e: e.activation(out=GC[:], in_=lg16[:, 0:8], func=AF.Exp, scale=128.0), ["lg16"], ["GC"])
                P.op("dve", lambda e: e.tensor_tensor(out=DEC[:], in0=lg16[:], in1=etab[:], op=ALU.mult), ["lg16", "etab"], ["DEC"])
                P.op("act", lambda e: e.activation(out=DEC[:], in_=DEC[:], func=AF.Exp), ["DEC"], ["DEC"])
                P.op("dve", lambda e: e.tensor_scalar(out=DEC[:, 8:16], in0=DEC[:, 8:16], scalar1=128.0 ** -0.5, scalar2=None, op0=ALU.mult), ["DEC"], ["DEC"])
                stage_end()
            if stop_after == ("params", l):
                break

            def modp(m, k, tc):
                return MOD[:, m * 8 + k, tc:tc + 1]

            with ExitStack() as ls:
                lsb = lambda name, shape, dtype=F32: ls.enter_context(nc.sbuf_tensor(name + "_L%d" % l, list(shape), dtype))
                WB = lsb("WB", [128, 8, 5632], BF16)
                with ExitStack() as ls2:
                    wst = [ls2.enter_context(nc.sbuf_tensor("wst%d_L%d" % (i, l), [128, 8, 512], F32)) for i in range(2)]
                    load_cast(WB, w["w_in"], 8, 5632, wst, "WB")
                    stage_end()
                cosT = lsb("cosT_s", [128, NT, 64])
                sinT = lsb("sinT_s", [128, NT, 64])
                P.dma("sp", cosT[:], cos_in, [], ["cosT"], "c4")
                P.dma("sp", sinT[:], sin_in, [], ["sinT"], "c5")
                xbs = [lsb("xa%d" % i, [128, 8, 512]) for i in range(2)]
                hb = [lsb("ha%d" % i, [128, 8, 512], BF16) for i in range(1)]
                ub = lsb("ub", [128, 512], BF16)
                vb = lsb("vb", [128, 1024], BF16)
                rt = [lsb("rt%d" % i, [128, 4, 64]) for i in range(4)]
                rots = [lsb("rot%d" % i, [128, 4, 2, 64]) for i in range(2)]
                var_tm = lsb("var_tm", [128, 4, 512], BF16)
                qkT = lsb("qkT", [128, 16, 512], BF16)
                gb = [lsb("gb%d" % i, [128, 8, 512], BF16) for i in range(3)]
                bi = 0
                for s in range(SEQS):
                    for (t0, n, isctx) in blocks:
                        tc = 2 if isctx else s
                        xb = xbs[bi % 2]
                        h = hb[0]
                        xk, hk = ("xa", bi % 2), ("ha", 0)
                        P.dma("sp", xb[:, :, :n], xsrc[s, :, :, t0:t0 + n].rearrange("k p t -> p k t"),
                              [("XS", s, t0)], [xk], xk)
                        for k in range(8):
                            P.op("act", lambda e, k=k, xb=xb, h=h, n=n, tc=tc: e.activation(
                                out=h[:, k, :n], in_=xb[:, k, :n], func=AF.Identity,
                                scale=modp(1, k, tc), bias=modp(0, k, tc)), [xk, "MOD"], [hk])
                        gate_list = [(gi, oc) for gi in range(3) for oc in range(8)]

                        def emit_gate(gi, oc, h=h, n=n, hk=hk):
                            func = AF.Silu if gi == 0 else AF.Sigmoid
                            bnk = 6 + (oc % 2)
                            c0 = 2560 + gi * 1024 + oc * 128

                            def mm(e):
                                for k in range(8):
                                    r = e.matmul(PS[bnk][:, :n], lhsT=WB[:, k, c0:c0 + 128], rhs=h[:, k, :n],
                                                 start=(k == 0), stop=(k == 7))
                                return r
                            P.op("pe", mm, [hk, "WB"], [("ps", bnk)])
                            P.op("act", lambda e: e.activation(out=gb[gi][:, oc, :n], in_=PS[bnk][:, :n], func=func),
                                 [("ps", bnk)], [("gb", gi, oc)])

                        for ti in range(n // 128):
                            gt = (t0 // 128) + ti
                            tsl = slice(ti * 128, (ti + 1) * 128)
                            rows = slice(t0 + ti * 128, t0 + ti * 128 + 128)
                            for bnk, c0 in enumerate((0, 512, 1024, 1536, 2048)):
                                def mm(e, bnk=bnk, c0=c0, h=h, tsl=tsl):
                                    for k in range(8):
                                        r = e.matmul(PS[bnk][:, :], lhsT=h[:, k, tsl], rhs=WB[:, k, c0:c0 + 512],
                                                     start=(k == 0), stop=(k == 7))
                                    return r
                                P.op("pe", mm, [hk, "WB"], [("ps", bnk)])
                            P.op("act", lambda e: e.activation(out=ub[:], in_=PS[0][:, :], func=AF.Copy), [("ps", 0)], ["ub"])
                            P.dma("sp", U_d[s, rows, :], ub[:], ["ub"], [("U", s, gt)], "ub")
                            P.op("act", lambda e: e.activation(out=vb[:, 0:512], in_=PS[3][:, :], func=AF.Copy), [("ps", 3)], ["vb0"])
                            P.op("dve", lambda e: e.tensor_copy(out=vb[:, 512:1024], in_=PS[4][:, :]), [("ps", 4)], ["vb1"])
                            P.dma("sp", V_d[s, rows, :], vb[:], ["vb0", "vb1"], [("V", s, gt)], "vb")
                            for qi, bnk in enumerate((1, 2)):
                                pv = PS[bnk][:, :].rearrange("p (h two d) -> p h two d", two=2, d=64)
                                Cb = cosT[:, gt, :].unsqueeze(1).to_broadcast([128, 4, 64])
                                Sb = sinT[:, gt, :].unsqueeze(1).to_broadcast([128, 4, 64])
                                rq = rots[qi]
                                P.op("dve", lambda e, pv=pv, Cb=Cb: e.tensor_tensor(out=rt[0][:], in0=pv[:, :, 0, :], in1=Cb, op=ALU.mult), [("ps", bnk), "cosT"], ["rt0"])
                                P.op("dve", lambda e, pv=pv, Sb=Sb: e.tensor_tensor(out=rt[1][:], in0=pv[:, :, 1, :], in1=Sb, op=ALU.mult), [("ps", bnk), "sinT"], ["rt1"])
                                P.op("dve", lambda e, pv=pv, Sb=Sb: e.tensor_tensor(out=rt[2][:], in0=pv[:, :, 0, :], in1=Sb, op=ALU.mult), [("ps", bnk), "sinT"], ["rt2"])
                                P.op("dve", lambda e, pv=pv, Cb=Cb: e.tensor_tensor(out=rt[3][:], in0=pv[:, :, 1, :], in1=Cb, op=ALU.mult), [("ps", bnk), "cosT"], ["rt3"])
                                P.op("dve", lambda e, rq=rq: e.tensor_tensor(out=rq[:, :, 0, :], in0=rt[0][:], in1=rt[1][:], op=ALU.subtract), ["rt0", "rt1"], [("rot0", qi)])
                                P.op("dve", lambda e, rq=rq: e.tensor_tensor(out=rq[:, :, 1, :], in0=rt[2][:], in1=rt[3][:], op=ALU.add), ["rt2", "rt3"], [("rot1", qi)])
                            ngl = len(gate_list)
                            ntl_ = n // 128
                            for (gi, oc) in gate_list[ti * ngl // ntl_:(ti + 1) * ngl // ntl_]:
                                emit_gate(gi, oc)
                            for qi in range(2):
                                rq = rots[qi]
                                for dr in range(2):
                                    vi = qi * 2 + dr
                                    for hh in range(4):
                                        P.op("act", lambda e, vi=vi, hh=hh, dr=dr, qi=qi, rq=rq: e.activation(
                                            out=var_tm[:, vi, hh * 128:(hh + 1) * 128],
                                            in_=rq[:, hh, :, :].rearrange("p a b -> p (a b)"), func=AF.Identity,
                                            scale=DEC[:, qi * 8 + dr * 4 + hh:qi * 8 + dr * 4 + hh + 1]),
                                            [("rot0", qi), ("rot1", qi), "DEC"], [("var", vi)])
                            P.dma("sp", KF_d[s, rows, :], var_tm[:, 2, :], [("var", 2)], [("KF", s, gt)], "kf")
                            P.dma("sp", KB_d[s, rows, :], var_tm[:, 3, :], [("var", 3)], [("KB", s, gt)], "kb")
                            p5 = PS[5][:, :].bitcast(BF16).rearrange("p (a b) -> p a b", b=128)[:, 0:8, :]
                            for half in range(2):
                                def tr(e, half=half):
                                    for j in range(8):
                                        vi = half * 2 + j // 4
                                        hh = j % 4
                                        r = e.transpose(p5[:, j, :], var_tm[:, vi, hh * 128:(hh + 1) * 128], ident_b[:])
                                    return r
                                P.op("pe", tr, [("var", half * 2), ("var", half * 2 + 1), "ident_b"], [("ps", 5)])
                                P.op("dve", lambda e, half=half, tsl=tsl: e.tensor_copy(out=qkT[:, half * 8:(half + 1) * 8, tsl], in_=p5),
                                     [("ps", 5)], [("qkT", half)])
                        P.dma("sp", QKT_d[s, :, :, t0:t0 + n].rearrange("a p t -> p a t"), qkT[:, :, :n],
                              [("qkT", 0), ("qkT", 1)], [("QKT", s, t0)], "qkT")
                        for gi in range(3):
                            P.dma("sp", G_d[gi][s, :, :, t0:t0 + n].rearrange("k p t -> p k t"), gb[gi][:, :, :n],
                                  [("gb", gi, oc) for oc in range(8)], [("G", gi, s, t0)], ("gb", gi))
                        bi += 1
                stage_end()
            if stop_after == ("A", l):
                break

            with ExitStack() as ls:
                lsb = lambda name, shape, dtype=F32: ls.enter_context(nc.sbuf_tensor(name + "_L%d" % l, list(shape), dtype))
                Sst = lsb("Sst", [128, 1024])
                Df = lsb("Df", [128, 1024])
                Dbf = [lsb("Dbf%d" % i, [128, 1024], BF16) for i in range(2)]
                kt = [lsb("kt%d" % i, [128, 512], BF16) for i in range(2)]
                vt = [lsb("vt%d" % i, [128, 1024], BF16) for i in range(2)]
                it = 0
                for s in range(SEQS):
                    for dr in range(2):
                        order = [32, 33] + list(range(32)) if dr == 0 else [33, 32] + list(range(31, -1, -1))
                        Ksrc = KF_d if dr == 0 else KB_d
                        P.op("dve", lambda e: e.memset(Sst[:], 0.0), [], ["S"])
                        for c in order:
                            j = it % 2
                            rows = slice(c * 128, (c + 1) * 128)
                            P.dma("sp", kt[j][:], Ksrc[s, rows, :], [("KF", s), ("KB", s)], [("kt", j)], ("kt", j))
                            P.dma("sp", vt[j][:], V_d[s, rows, :], [("V", s)], [("vt", j)], ("vt", j))
                            for hh in range(4):
                                P.op("dve", lambda e, hh=hh, dr=dr: e.tensor_scalar(
                                    out=Df[:, hh * 256:(hh + 1) * 256], in0=Sst[:, hh * 256:(hh + 1) * 256],
                                    scalar1=GC[:, dr * 4 + hh:dr * 4 + hh + 1], scalar2=None, op0=ALU.mult), ["S", "GC"], [("Df", hh)])
                            P.op("act", lambda e, j=j: e.activation(out=Dbf[j][:], in_=Df[:], func=AF.Copy),
                                 [("Df", hh) for hh in range(4)], [("Dbf", j)])
                            P.dma("sp", DST_d[s, dr, c].rearrange("h p v -> p h v"),
                                  Dbf[j][:].rearrange("p (h v) -> p h v", v=256), [("Dbf", j)], [("DST", s, dr, c)], ("Dbf", j))

                            def mm(e, j=j):
                                for hh in range(4):
                                    r = e.matmul(PS[hh // 2][:, (hh % 2) * 256:(hh % 2) * 256 + 256],
                                                 lhsT=kt[j][:, hh * 128:(hh + 1) * 128], rhs=vt[j][:, hh * 256:(hh + 1) * 256],
                                                 start=True, stop=True)
                                return r
                            P.op("pe", mm, [("kt", j), ("vt", j)], [("ps", 0), ("ps", 1)])
                            for b2 in range(2):
                                P.op("dve", lambda e, b2=b2: e.tensor_tensor(
                                    out=Sst[:, b2 * 512:(b2 + 1) * 512], in0=PS[b2][:, :], in1=Df[:, b2 * 512:(b2 + 1) * 512], op=ALU.add),
                                    [("ps", b2), ("Df", 2 * b2), ("Df", 2 * b2 + 1)], ["S"])
                            it += 1
                stage_end()

            with ExitStack() as ls:
                lsb = lambda name, shape, dtype=F32: ls.enter_context(nc.sbuf_tensor(name + "_L%d" % l, list(shape), dtype))
                qk = [lsb("qk%d" % i, [128, 16, 128], BF16) for i in range(2)]
                vt = [lsb("vc%d" % i, [128, 1024], BF16) for i in range(2)]
                Dt = [lsb("Dt%d" % i, [128, 2, 4, 256], BF16) for i in range(2)]
                sg = [lsb("sg%d" % i, [128, 8, 128], BF16) for i in range(2)]
                PT = lsb("PT", [128, 8, 128], BF16)
                of = lsb("of", [128, 8, 128])
                osq = lsb("osq", [128, 8, 128])
                msr = lsb("msr", [128, 4, 128])
                m2r = lsb("m2r", [128, 4, 128])
                rsr = lsb("rsr", [128, 4, 128])
                zrt = [lsb("zrt%d" % i, [128, 8, 128], BF16) for i in range(2)]
                it = 0
                ntl = NT if need_ctx else 32
                for s in range(SEQS):
                    for c in range(ntl):
                        j = it % 2
                        cs = slice(c * 128, (c + 1) * 128)
                        P.dma("sp", qk[j][:], QKT_d[s, :, :, cs].rearrange("a p t -> p a t"), [("QKT", s)], [("qk", j)], ("qk", j))
                        P.dma("sp", vt[j][:], V_d[s, cs, :], [("V", s)], [("vc", j)], ("vc", j))
                        for dr in range(2):
                            P.dma("sp", Dt[j][:, dr], DST_d[s, dr, c].rearrange("h p v -> p h v"), [("DST", s)], [("Dt", j)], ("Dt", j))
                        P.dma("sp", sg[j][:], G_d[0][s, :, :, cs].rearrange("k p t -> p k t"), [("G", 0, s)], [("sg", j)], ("sg", j))
                        for dr in range(2):
                            def mm(e, dr=dr, j=j):
                                for hh in range(4):
                                    r = e.matmul(PS[dr][:, hh * 128:(hh + 1) * 128], lhsT=qk[j][:, (2 + dr) * 4 + hh, :],
                                                 rhs=qk[j][:, dr * 4 + hh, :], start=True, stop=True)
                                return r
                            P.op("pe", mm, [("qk", j)], [("ps", dr)])
                            P.op("dve", lambda e, dr=dr: e.tensor_tensor(
                                out=PT[:, dr * 4:(dr + 1) * 4, :], in0=PS[dr][:, :].rearrange("p (h i) -> p h i", i=128),
                                in1=masks[:, dr, :].unsqueeze(1).to_broadcast([128, 4, 128]), op=ALU.mult),
                                [("ps", dr), "masks"], [("PT", dr)])
                        for b2 in range(2):
                            def mm(e, b2=b2, j=j):
                                for q in range(4):
                                    ch = b2 * 4 + q
                                    hh, m = ch // 2, ch % 2
                                    o = PS[2 + b2][:, q * 128:(q + 1) * 128]
                                    for dr in range(2):
                                        e.matmul(o, lhsT=vt[j][:, hh * 256 + m * 128:hh * 256 + m * 128 + 128],
                                                 rhs=PT[:, dr * 4 + hh, :], start=(dr == 0), stop=False)
                                        r = e.matmul(o, lhsT=Dt[j][:, dr, hh, m * 128:(m + 1) * 128],
                                                     rhs=qk[j][:, dr * 4 + hh, :], start=False, stop=(dr == 1))
                                return r
                            P.op("pe", mm, [("vc", j), ("PT", 0), ("PT", 1), ("Dt", j), ("qk", j)], [("ps", 2 + b2)])
                            P.op("act", lambda e, b2=b2: e.activation(out=of[:, b2 * 4:(b2 + 1) * 4, :].rearrange("p a b -> p (a b)"),
                                                                     in_=PS[2 + b2][:, :], func=AF.Copy), [("ps", 2 + b2)], [("of", b2)])
                            P.op("act", lambda e, b2=b2: e.activation(out=osq[:, b2 * 4:(b2 + 1) * 4, :].rearrange("p a b -> p (a b)"),
                                                                     in_=PS[2 + b2][:, :], func=AF.Square), [("ps", 2 + b2)], [("osq", b2)])
                        def mmst(e):
                            for hh in range(4):
                                for m in range(2):
                                    e.matmul(PS[4][:, hh * 128:(hh + 1) * 128], lhsT=ones_f[:], rhs=of[:, hh * 2 + m, :],
                                             start=(m == 0), stop=(m == 1))
                            for hh in range(4):
                                for m in range(2):
                                    r = e.matmul(PS[5][:, hh * 128:(hh + 1) * 128], lhsT=ones_f[:], rhs=osq[:, hh * 2 + m, :],
                                                 start=(m == 0), stop=(m == 1))
                            return r
                        P.op("pe", mmst, [("of", 0), ("of", 1), ("osq", 0), ("osq", 1), "ones_f"], [("ps", 4), ("ps", 5)])
                        fl = lambda t: t[:].rearrange("p a b -> p (a b)")
                        P.op("act", lambda e: e.activation(out=fl(msr), in_=PS[4][:, :], func=AF.Copy), [("ps", 4)], ["msr"])
                        P.op("act", lambda e: e.activation(out=fl(m2r), in_=PS[4][:, :], func=AF.Square), [("ps", 4)], ["m2r"])
                        P.op("dve", lambda e: e.tensor_tensor(out=fl(rsr), in0=PS[5][:, :], in1=fl(m2r), op=ALU.subtract), [("ps", 5), "m2r"], ["rsr"])
                        P.op("act", lambda e: e.activation(out=fl(rsr), in_=fl(rsr), func=AF.Ln, bias=epsb[:, 0:1]), ["rsr", "epsb0"], ["rsr"])
                        P.op("act", lambda e: e.activation(out=fl(rsr), in_=fl(rsr), func=AF.Exp, scale=-0.5), ["rsr"], ["rsr"])
                        ov = of[:].rearrange("p (h m) t -> p h m t", m=2)
                        P.op("dve", lambda e, ov=ov: e.tensor_tensor(out=ov, in0=ov, in1=msr[:].unsqueeze(2).to_broadcast([128, 4, 2, 128]), op=ALU.subtract),
                             [("of", 0), ("of", 1), "msr"], [("of", 0), ("of", 1)])
                        P.op("dve", lambda e, ov=ov: e.tensor_tensor(out=ov, in0=ov, in1=rsr[:].unsqueeze(2).to_broadcast([128, 4, 2, 128]), op=ALU.mult),
                             [("of", 0), ("of", 1), "rsr"], [("of", 0), ("of", 1)])
                        P.op("dve", lambda e, j=j: e.tensor_tensor(out=zrt[j][:], in0=of[:], in1=sg[j][:], op=ALU.mult),
                             [("of", 0), ("of", 1), ("sg", j)], [("zrt", j)])
                        P.dma("sp", ZR_d[s, :, :, cs].rearrange("k p t -> p k t"), zrt[j][:], [("zrt", j)], [("ZR", s, c)], ("zrt", j))
                        it += 1
                stage_end()
            if stop_after == ("C1", l):
                break

            with ExitStack() as ls:
                lsb = lambda name, shape, dtype=F32: ls.enter_context(nc.sbuf_tensor(name + "_L%d" % l, list(shape), dtype))
                wro = lsb("wro", [128, 8, 1024], BF16)
                wo = lsb("wo", [128, 8, 1024], BF16)
                wpo = lsb("wpo", [128, 4, 1024], BF16)
                plw = lsb("plw", [128, 4, 128], BF16)
                with ExitStack() as ls2:
                    wst = [ls2.enter_context(nc.sbuf_tensor("wstc%d_L%d" % (i, l), [128, 8, 512], F32)) for i in range(2)]
                    load_cast(wro, w["wro"], 8, 1024, wst, "wro")
                    load_cast(wo, w["wo"], 8, 1024, wst, "wo")
                    load_cast(wpo, w["wpo"], 4, 1024, wst, "wpo")
                    P.dma("sp", wst[0][:, 0:4, 0:128], w["pool_w"].rearrange("g c d -> c g d"), [], [("wst", 0)], ("wst", 0))
                    P.op("pool", lambda e: e.tensor_copy(out=plw[:], in_=wst[0][:, 0:4, 0:128]), [("wst", 0)], ["plw"])
                    stage_end()
                Ures = lsb("Ures", [128, NT, 512], BF16)
                PMb = lsb("PMb", [128, pm_slots, 512], BF16)
                PMc = lsb("PMc", [128, 8, 256], BF16)
                P.dma("sp", PMc[:], pmc_in.rearrange("g t p o -> p (g t) o"), [], ["PMc"], "pmc")
                zr = lsb("zr", [128, 8, 512], BF16)
                spb = lsb("spb", [128, 8, 512], BF16)
                srb = lsb("srb", [128, 8, 512], BF16)
                dTb = lsb("dTb", [128, 4, 512], BF16)
                ygb = lsb("ygb", [128, 4, 512], BF16)
                mixb = lsb("mixb", [128, 8, 512], BF16)
                t1 = lsb("t1", [128, 512])
                t2 = lsb("t2", [128, 512])
                xb = lsb("xc", [128, 8, 512])
                ss = dict(zb=lsb("zb", [128, 8, 512], BF16), sq=lsb("sq", [128, 8, 512], BF16),
                          ms=lsb("ms", [128, 512]), m2=lsb("m2", [128, 512]), rs=lsb("rs", [128, 512]))
                for s in range(SEQS):
                    P.dma("sp", Ures[:], U_d[s].rearrange("(t p) c -> p t c", p=128), [("U", s)], ["Ures"], "Ures")
                    for ob, (t0, n, isctx) in enumerate(blocks):
                        if isctx and not need_ctx:
                            continue
                        tc = 2 if isctx else s
                        bsl = (slice(None), slice(None), slice(t0, t0 + n))
                        P.dma("sp", zr[:, :, :n], ZR_d[s][bsl].rearrange("k p t -> p k t"), [("ZR", s)], ["zr"], "zr")
                        P.dma("sp", spb[:, :, :n], G_d[1][s][bsl].rearrange("k p t -> p k t"), [("G", 1, s)], ["spb"], "spb")
                        P.dma("sp", srb[:, :, :n], G_d[2][s][bsl].rearrange("k p t -> p k t"), [("G", 2, s)], ["srb"], "srb")
                        P.dma("sp", xb[:, :, :n], xsrc[s][bsl].rearrange("k p t -> p k t"), [("XS", s, t0)], ["xb"], "xb")
                        if not isctx:
                            si = 0 if ob == 0 else (2 if ob == 7 else 1)
                            if ob in (0, 1, 7):
                                P.dma("sp", PMb[:], pm_in[si].rearrange("a p o -> p a o"), [], ["PMb"], "PMb")
                        for g in range(4):
                            bnk = g % 2
                            if isctx:
                                lst = [(32 + tt, PMc[:, g * 2 + tt, :]) for tt in range(2)]
                            else:
                                lst = [(4 * ob + rel, PMb[:, slot, :]) for rel, slot in pm_lists[si][g]]
                            def mm(e, lst=lst, g=g, bnk=bnk, n=n):
                                for i2, (tin, pmv) in enumerate(lst):
                                    r = e.matmul(PS[bnk][:, :n], lhsT=Ures[:, tin, g * 128:(g + 1) * 128], rhs=pmv[:, :n],
                                                 start=(i2 == 0), stop=(i2 == len(lst) - 1))
                                return r
                            P.op("pe", mm, ["Ures", "PMb", "PMc"], [("ps", bnk)])
                            P.op("act", lambda e, g=g, bnk=bnk, n=n: e.activation(out=dTb[:, g, :n], in_=PS[bnk][:, :n], func=AF.Copy),
                                 [("ps", bnk)], [("dTb", g)])
                            P.op("pe", lambda e, g=g, bnk=bnk, n=n: e.matmul(PS[2 + bnk][:, :n], lhsT=plw[:, g, :], rhs=dTb[:, g, :n], start=True, stop=True),
                                 [("dTb", g), "plw"], [("ps", 2 + bnk)])
                            P.op("act", lambda e, g=g, bnk=bnk, n=n: e.activation(out=ygb[:, g, :n], in_=PS[2 + bnk][:, :n], func=AF.Identity,
                                                                                   scale=psc[:, g:g + 1]), [("ps", 2 + bnk), "psc"], [("ygb", g)])
                        for oc in range(8):
                            ocs = slice(oc * 128, (oc + 1) * 128)
                            def mmp(e, ocs=ocs, n=n):
                                for g in range(4):
                                    r = e.matmul(PS[4][:, :n], lhsT=wpo[:, g, ocs], rhs=ygb[:, g, :n], start=(g == 0), stop=(g == 3))
                                return r
                            def mmr(e, ocs=ocs, n=n):
                                for k in range(8):
                                    r = e.matmul(PS[5][:, :n], lhsT=wro[:, k, ocs], rhs=zr[:, k, :n], start=(k == 0), stop=(k == 7))
                                return r
                            P.op("pe", mmp, [("ygb", g) for g in range(4)] + ["wpo"], [("ps", 4)])
                            P.op("pe", mmr, ["zr", "wro"], [("ps", 5)])
                            P.op("dve", lambda e, oc=oc, n=n: e.tensor_tensor(out=t1[:, :n], in0=PS[4][:, :n], in1=spb[:, oc, :n], op=ALU.mult),
                                 [("ps", 4), "spb"], ["t1"])
                            P.op("dve", lambda e, oc=oc, n=n: e.tensor_tensor(out=t2[:, :n], in0=PS[5][:, :n], in1=srb[:, oc, :n], op=ALU.mult),
                                 [("ps", 5), "srb"], ["t2"])
                            P.op("dve", lambda e, oc=oc, n=n: e.tensor_tensor(out=mixb[:, oc, :n], in0=t1[:, :n], in1=t2[:, :n], op=ALU.add),
                                 ["t1", "t2"], [("mixb", oc)])
                        for oc2 in range(8):
                            bnk = 6 + oc2 % 2
                            def mmo(e, oc2=oc2, bnk=bnk, n=n):
                                for oc in range(8):
                                    r = e.matmul(PS[bnk][:, :n], lhsT=wo[:, oc, oc2 * 128:(oc2 + 1) * 128], rhs=mixb[:, oc, :n],
                                                 start=(oc == 0), stop=(oc == 7))
                                return r
                            P.op("pe", mmo, [("mixb", oc) for oc in range(8)] + ["wo"], [("ps", bnk)])
                            P.op("dve", lambda e, oc2=oc2, bnk=bnk, n=n, tc=tc: e.scalar_tensor_tensor(
                                out=xb[:, oc2, :n], in0=PS[bnk][:, :n], scalar=modp(2, oc2, tc), in1=xb[:, oc2, :n],
                                op0=ALU.mult, op1=ALU.add), [("ps", bnk), "xb", "MOD"], ["xb"])
                        ln_block(ss, xb, n, 0, 1, 1, 0, 1)
                        P.dma("sp", XS[s][bsl].rearrange("k p t -> p k t"), xb[:, :, :n], ["xb"], [("XS", s, t0)], "xb_st")
                stage_end()
            xsrc = XS
            if stop_after == ("C2", l):
                break

            last = (li == len(layers) - 1)
            if l % 2 == 0:
                with ExitStack() as ls:
                    lsb = lambda name, shape, dtype=F32: ls.enter_context(nc.sbuf_tensor(name + "_L%d" % l, list(shape), dtype))
                    w1 = lsb("w1", [128, 8, DFF], BF16)
                    w3 = lsb("w3", [128, 8, DFF], BF16)
                    w2 = lsb("w2", [128, 22, D], BF16)
                    with ExitStack() as ls2:
                        wst = [ls2.enter_context(nc.sbuf_tensor("wstd%d_L%d" % (i, l), [128, 8, 512], F32)) for i in range(2)]
                        load_cast(w1, w["w1"], 8, DFF, wst, "w1")
                        load_cast(w3, w["w3"], 8, DFF, wst, "w3")
                        load_cast(w2, w["w2"], 22, D, wst, "w2")
                        stage_end()
                    NB = 256
                    xds = [lsb("xd%d" % i, [128, 8, NB]) for i in range(2)]
                    h2 = lsb("h2", [128, 8, NB], BF16)
                    hid = lsb("hid", [128, 22, NB], BF16)
                    sl = [lsb("sl%d" % i, [128, NB]) for i in range(2)]
                    ss = dict(zb=lsb("zbd", [128, 8, NB], BF16), sq=lsb("sqd", [128, 8, NB], BF16),
                              ms=lsb("msd", [128, NB]), m2=lsb("m2d", [128, NB]), rs=lsb("rsd", [128, NB]))
                    bi = 0
                    for s in range(SEQS):
                        ntok = T if need_ctx else L
                        for t0 in range(0, ntok, NB):
                            isctx = t0 >= L
                            tc = 2 if isctx else s
                            xb = xds[bi % 2]
                            xk = ("xd", bi % 2)
                            bsl = (slice(None), slice(None), slice(t0, t0 + NB))
                            P.dma("sp", xb[:], XS[s][bsl].rearrange("k p t -> p k t"), [("XS", s, t0)], [xk], xk)
                            for k in range(8):
                                P.op("act", lambda e, k=k, xb=xb, tc=tc: e.activation(out=h2[:, k, :], in_=xb[:, k, :], func=AF.Identity,
                                                                                      scale=modp(4, k, tc), bias=modp(3, k, tc)), [xk, "MOD"], ["h2"])
                            for ff in range(22):
                                fs = slice(ff * 128, (ff + 1) * 128)
                                b1, b3 = (ff % 2) * 2, (ff % 2) * 2 + 1
                                def mm13(e, fs=fs, b1=b1, b3=b3):
                                    for k in range(8):
                                        e.matmul(PS[b1][:, :NB], lhsT=w1[:, k, fs], rhs=h2[:, k, :], start=(k == 0), stop=(k == 7))
                                    for k in range(8):
                                        r = e.matmul(PS[b3][:, :NB], lhsT=w3[:, k, fs], rhs=h2[:, k, :], start=(k == 0), stop=(k == 7))
                                    return r
                                P.op("pe", mm13, ["h2", "w1", "w3"], [("ps", b1), ("ps", b3)])
                                P.op("act", lambda e, ff=ff, b1=b1: e.activation(out=sl[ff % 2][:], in_=PS[b1][:, :NB], func=AF.Silu), [("ps", b1)], [("sl", ff % 2)])
                                P.op("dve", lambda e, ff=ff, b3=b3: e.tensor_tensor(out=hid[:, ff, :], in0=PS[b3][:, :NB], in1=sl[ff % 2][:], op=ALU.mult),
                                     [("ps", b3), ("sl", ff % 2)], [("hid", ff)])
                            for oc in range(8):
                                bnk = 4 + oc % 4
                                def mm2_(e, oc=oc, bnk=bnk):
                                    for ff in range(22):
                                        r = e.matmul(PS[bnk][:, :NB], lhsT=w2[:, ff, oc * 128:(oc + 1) * 128], rhs=hid[:, ff, :],
                                                     start=(ff == 0), stop=(ff == 21))
                                    return r
                                P.op("pe", mm2_, [("hid", ff) for ff in range(22)] + ["w2"], [("ps", bnk)])
                                P.op("dve", lambda e, oc=oc, bnk=bnk, xb=xb, tc=tc: e.scalar_tensor_tensor(
                                    out=xb[:, oc, :], in0=PS[bnk][:, :NB], scalar=modp(5, oc, tc), in1=xb[:, oc, :],
                                    op0=ALU.mult, op1=ALU.add), [("ps", bnk), xk, "MOD"], [xk])
                            ln_block(ss, xb, NB, 2, 3, 1, 0, 1, XK=xk)
                            if last:
                                if not isctx:
                                    P.dma("sp", outT[s][bsl].rearrange("k p t -> p k t"), xb[:], [xk], [("OUT",)], ("xd_st", bi % 2))
                            else:
                                P.dma("sp", XS[s][bsl].rearrange("k p t -> p k t"), xb[:], [xk], [("XS", s, t0)], ("xd_st", bi % 2))
                            bi += 1
                    stage_end()
            else:
                with ExitStack() as ls:
                    lsb = lambda name, shape, dtype=F32: ls.enter_context(nc.sbuf_tensor(name + "_L%d" % l, list(shape), dtype))
                    NB = 512
                    wrb = lsb("wrb", [128, 8, NEXP], BF16)
                    wrf = lsb("wrf", [128, 8, NEXP])
                    P.dma("sp", wrf[:], w["wr"].rearrange("(k p) e -> p k e", p=128), [], ["wrf"], "wrf")
                    P.op("dve", lambda e: e.tensor_copy(out=wrb[:], in_=wrf[:]), ["wrf"], ["wrb"])
                    wst = [lsb("wste%d" % i, [128, 4096]) for i in range(3)]
                    wsl = [[lsb("wsl%d_%d" % (i, j), [128, 4096], BF16) for j in range(3)] for i in range(2)]
                    xe = lsb("xe", [128, 8, 1024])
                    h2 = lsb("h2e", [128, 8, 1024], BF16)
                    gw = lsb("gw", [128, NEXP, 1024])
                    hid = [lsb("hide%d" % i, [128, 4, NB], BF16) for i in range(2)]
                    sl = [lsb("sle%d" % i, [128, NB]) for i in range(2)]
                    tg = [lsb("tge%d" % i, [128, NB]) for i in range(2)]
                    lgs = lsb("lgs", [128, 8])
                    mx8 = lsb("mx8", [128, 8])
                    dd = lsb("dd", [128, 4])
                    gte = lsb("gte", [128, 2, 8])
                    gbc = lsb("gbc", [128, 8, 128])
                    ss = dict(zb=lsb("zbe", [128, 8, 128], BF16), sq=lsb("sqe", [128, 8, 128], BF16),
                              ms=lsb("mse", [128, 128]), m2=lsb("m2e", [128, 128]), rs=lsb("rse", [128, 128]))
                    sbs = [[(s, q * 1024 + b * NB, NB, s) for b in range(2)] for s in range(SEQS) for q in range(4)]
                    if need_ctx:
                        sbs.append([(0, L, NCTX, 2), (1, L, NCTX, 2)])
                    jobs = [(sbi, ex, fsl) for sbi in range(len(sbs)) for ex in range(NEXP) for fsl in range(7)]

                    def load_dma(ji):
                        sbi, ex, fsl = jobs[ji]
                        j = ji % 2
                        if sbi == 0:
                            srcs = (w["w1"][ex].rearrange("(k p) n -> p k n", p=128)[:, :, fsl * 512:(fsl + 1) * 512],
                                    w["w3"][ex].rearrange("(k p) n -> p k n", p=128)[:, :, fsl * 512:(fsl + 1) * 512],
                                    w["w2"][ex, fsl * 512:(fsl + 1) * 512, :].rearrange("(c p) n -> p c n", p=128))
                            for m3 in range(3):
                                bdim = 512 if m3 < 2 else 1024
                                P.dma("sp", wst[m3][:].rearrange("p (a b) -> p a b", b=bdim), srcs[m3], [], [("wste", m3)], ("wste", m3))

                    def load_job(ji):
                        sbi, ex, fsl = jobs[ji]
                        j = ji % 2
                        if sbi == 0:
                            for m3 in range(3):
                                P.op("act", lambda e, j=j, m3=m3: e.activation(out=wsl[j][m3][:], in_=wst[m3][:], func=AF.Copy),
                                     [("wste", m3)], [("wsl", j, m3)])
                                P.dma("sp", WC_d[ex, fsl, m3], wsl[j][m3][:], [("wsl", j, m3)], [("WC", ex, fsl, m3)], ("wc_st", j, m3))
                            if ji + 1 < len(jobs):
                                load_dma(ji + 1)
                        else:
                            for m3 in range(3):
                                P.dma("sp", wsl[j][m3][:], WC_d[ex, fsl, m3], [("WC", ex, fsl, m3)], [("wsl", j, m3)], ("wsl", j, m3))

                    hi = 0
                    load_dma(0)
                    load_job(0)
                    for ji, (sbi, ex, fsl) in enumerate(jobs):
                        blks = sbs[sbi]
                        if ex == 0 and fsl == 0:
                            off = 0
                            for bix, (s, t0, n, tc) in enumerate(blks):
                                bs = slice(off, off + n)
                                P.dma("sp", xe[:, :, bs], XS[s, :, :, t0:t0 + n].rearrange("k p t -> p k t"), [("XS", s, t0)], [("xe", bix)], ("xe", bix))
                                for k in range(8):
                                    P.op("act", lambda e, k=k, bs=bs, tc=tc: e.activation(out=h2[:, k, bs], in_=xe[:, k, bs], func=AF.Identity,
                                                                                          scale=modp(4, k, tc), bias=modp(3, k, tc)),
                                         [("xe", bix), "MOD"], [("h2e", bix)])
                                for tt in range(n // 128):
                                    ts_ = slice(off + tt * 128, off + tt * 128 + 128)
                                    def mmr(e, ts_=ts_):
                                        for k in range(8):
                                            r = e.matmul(PS[6][:, 0:8], lhsT=h2[:, k, ts_], rhs=wrb[:, k, :], start=(k == 0), stop=(k == 7))
                                        return r
                                    P.op("pe", mmr, [("h2e", bix), "wrb"], [("ps", 6)])
                                    P.op("act", lambda e: e.activation(out=lgs[:], in_=PS[6][:, 0:8], func=AF.Copy), [("ps", 6)], ["lgs"])
                                    P.op("dve", lambda e: e.max(out=mx8[:], in_=lgs[:]), ["lgs"], ["mx8"])
                                    P.op("dve", lambda e: e.tensor_tensor(out=dd[:, 0:1], in0=mx8[:, 0:1], in1=mx8[:, 1:2], op=ALU.subtract), ["mx8"], ["dd0"])
                                    P.op("act", lambda e: e.activation(out=dd[:, 1:2], in_=dd[:, 0:1], func=AF.Sigmoid), ["dd0"], ["dd1"])
                                    P.op("act", lambda e: e.activation(out=dd[:, 2:3], in_=dd[:, 0:1], func=AF.Sigmoid, scale=-1.0), ["dd0"], ["dd2"])
                                    P.op("dve", lambda e: e.tensor_scalar(out=gte[:, 0, :], in0=lgs[:], scalar1=mx8[:, 0:1], scalar2=dd[:, 1:2],
                                                                          op0=ALU.is_equal, op1=ALU.mult), ["lgs", "mx8", "dd1"], ["gte0"])
                                    P.op("dve", lambda e: e.tensor_scalar(out=gte[:, 1, :], in0=lgs[:], scalar1=mx8[:, 1:2], scalar2=dd[:, 2:3],
                                                                          op0=ALU.is_equal, op1=ALU.mult), ["lgs", "mx8", "dd2"], ["gte1"])
                                    P.op("dve", lambda e: e.tensor_tensor(out=gte[:, 0, :], in0=gte[:, 0, :], in1=gte[:, 1, :], op=ALU.add), ["gte0", "gte1"], ["gte0"])
                                    P.op("dve", lambda e: e.tensor_copy(out=gbc[:], in_=gte[:, 0, :].unsqueeze(2).to_broadcast([128, 8, 128])), ["gte0"], ["gbc"])
                                    for hb2 in range(2):
                                        def mmb(e, hb2=hb2):
                                            for q in range(4):
                                                r = e.matmul(PS[4 + hb2][:, q * 128:(q + 1) * 128], lhsT=gbc[:, hb2 * 4 + q, :], rhs=ident_f[:], start=True, stop=True)
                                            return r
                                        P.op("pe", mmb, ["gbc", "ident_f"], [("ps", 4 + hb2)])
                                        P.op("act", lambda e, hb2=hb2, ts_=ts_: e.activation(out=gw[:, hb2 * 4:(hb2 + 1) * 4, ts_],
                                                                                            in_=PS[4 + hb2][:, :].rearrange("p (a b) -> p a b", b=128), func=AF.Copy),
                                             [("ps", 4 + hb2)], [("gw", bix)])
                                off += n
                        if ji + 1 < len(jobs):
                            load_job(ji + 1)
                        j = ji % 2
                        w1s = wsl[j][0][:].rearrange("p (a b) -> p a b", b=512)
                        w3s = wsl[j][1][:].rearrange("p (a b) -> p a b", b=512)
                        w2s = wsl[j][2][:].rearrange("p (a b) -> p a b", b=1024)
                        off = 0
                        hjs = []
                        for bix, (s, t0, n, tc) in enumerate(blks):
                            bs = slice(off, off + n)
                            hj = hi % 2
                            hjs.append((hj, bs))
                            for c4 in range(4):
                                cs = slice(c4 * 128, (c4 + 1) * 128)
                                b1, b3 = (c4 % 2) * 2, (c4 % 2) * 2 + 1
                                def mm13(e, cs=cs, b1=b1, b3=b3, bs=bs, w1s=w1s, w3s=w3s, n=n):
                                    for k in range(8):
                                        e.matmul(PS[b1][:, :n], lhsT=w1s[:, k, cs], rhs=h2[:, k, bs], start=(k == 0), stop=(k == 7))
                                    for k in range(8):
                                        r = e.matmul(PS[b3][:, :n], lhsT=w3s[:, k, cs], rhs=h2[:, k, bs], start=(k == 0), stop=(k == 7))
                                    return r
                                P.op("pe", mm13, [("h2e", bix), ("wsl", j, 0), ("wsl", j, 1)], [("ps", b1), ("ps", b3)])
                                P.op("act", lambda e, c4=c4, b1=b1, n=n: e.activation(out=sl[c4 % 2][:, :n], in_=PS[b1][:, :n], func=AF.Silu), [("ps", b1)], [("sle", c4 % 2)])
                                P.op("pool", lambda e, c4=c4, ex=ex, bs=bs, n=n: e.tensor_tensor(out=tg[c4 % 2][:, :n], in0=sl[c4 % 2][:, :n], in1=gw[:, ex, bs], op=ALU.mult),
                                     [("sle", c4 % 2), ("gw", bix)], [("tge", c4 % 2)])
                                P.op("dve", lambda e, c4=c4, hj=hj, b3=b3, n=n: e.tensor_tensor(out=hid[hj][:, c4, :n], in0=PS[b3][:, :n], in1=tg[c4 % 2][:, :n], op=ALU.mult),
                                     [("ps", b3), ("tge", c4 % 2)], [("hide", hj, c4)])
                            hi += 1
                            off += n
                        for bix, (s, t0, n, tc) in enumerate(blks):
                            hj, bs = hjs[bix]
                            for oc in range(8):
                                bnk = 4 + oc % 4
                                def mm2_(e, oc=oc, bnk=bnk, hj=hj, w2s=w2s, n=n):
                                    for c4 in range(4):
                                        r = e.matmul(PS[bnk][:, :n], lhsT=w2s[:, c4, oc * 128:(oc + 1) * 128], rhs=hid[hj][:, c4, :n],
                                                     start=(c4 == 0), stop=(c4 == 3))
                                    return r
                                P.op("pe", mm2_, [("hide", hj, c4) for c4 in range(4)] + [("wsl", j, 2)], [("ps", bnk)])
                                P.op("dve", lambda e, oc=oc, bnk=bnk, bs=bs, tc=tc, n=n: e.scalar_tensor_tensor(
                                    out=xe[:, oc, bs], in0=PS[bnk][:, :n], scalar=modp(5, oc, tc), in1=xe[:, oc, bs],
                                    op0=ALU.mult, op1=ALU.add), [("ps", bnk), ("xe", bix), "MOD"], [("xe", bix)])
                        if ex == NEXP - 1 and fsl == 6:
                            off = 0
                            for bix, (s, t0, n, tc) in enumerate(blks):
                                for sub in range(n // 128):
                                    bs = slice(off + sub * 128, off + sub * 128 + 128)
                                    tt0 = t0 + sub * 128
                                    ln_block(ss, xe[:, :, bs], 128, 2, 3, 1, 0, 1, XK=("xe", bix))
                                    if last:
                                        if tc != 2:
                                            P.dma("sp", outT[s, :, :, tt0:tt0 + 128].rearrange("k p t -> p k t"), xe[:, :, bs], [("xe", bix)], [("OUT", s, tt0)], ("xe_st", bix))
                                    else:
                                        P.dma("sp", XS[s, :, :, tt0:tt0 + 128].rearrange("k p t -> p k t"), xe[:, :, bs], [("xe", bix)], [("XSo", s, tt0)], ("xe_st", bix))
                                off += n
                    stage_end()
        if stop_after is not None:
            dbg = dt("dbgXS", [SEQS, 8, 128, T], F32, kind="ExternalOutput").ap()
            for s_ in range(SEQS):
                P.dma("sp", dbg[s_], XS[s_], [], [("dbg", s_)], ("dbg", s_))
        P.barrier()
        P.emit(block)
    return nc


def _host_inputs(inputs, layers):
    f32 = np.float32
    x = np.asarray(inputs["x"], f32)
    ctx = np.asarray(inputs["ctx"], f32)
    c = np.asarray(inputs["c"], f32)
    c_ctx = np.asarray(inputs["c_ctx"], f32)
    PM, pm_lists, PMC = _pool_constants()
    cosT, sinT = _rope_tables()
    E, masks = _misc_constants()
    common = dict(cosT=cosT, sinT=sinT, etab=E, masks=masks, pm=PM, pmc=PMC, ident=np.eye(128, dtype=f32))
    for l in layers:
        common["ada_w%d" % l] = np.ascontiguousarray(inputs["ada_w"][l], f32)
        common["ada_bT%d" % l] = np.ascontiguousarray(np.asarray(inputs["ada_b"][l], f32).reshape(48, 128).T)
        common["w_in%d" % l] = np.ascontiguousarray(inputs["w_in"][l], f32)
        common["pool_w%d" % l] = np.ascontiguousarray(inputs["pool_w"][l], f32)
        common["pscT%d" % l] = np.ascontiguousarray(np.asarray(inputs["pool_scale"][l], f32).reshape(4, 128).T)
        common["w_pool_out%d" % l] = np.ascontiguousarray(inputs["w_pool_out"][l], f32)
        common["w_ret_out%d" % l] = np.ascontiguousarray(inputs["w_ret_out"][l], f32)
        common["logit_bc%d" % l] = np.ascontiguousarray(
            np.broadcast_to(np.asarray(inputs["ret_decay_logit"][l], f32).reshape(1, 8), (128, 8)))
        common["w_out%d" % l] = np.ascontiguousarray(inputs["w_out"][l], f32)
        lnT = np.stack([np.asarray(inputs[k][l], f32).reshape(8, 128).T
                        for k in ("ln_mix_g", "ln_mix_b", "ln_ffn_g", "ln_ffn_b")], axis=1)
        common["lnT%d" % l] = np.ascontiguousarray(lnT)
        if l % 2 == 0:
            common["ffn_w1_%d" % l] = np.ascontiguousarray(inputs["ffn_w1"][l // 2], f32)
            common["ffn_w3_%d" % l] = np.ascontiguousarray(inputs["ffn_w3"][l // 2], f32)
            common["ffn_w2_%d" % l] = np.ascontiguousarray(inputs["ffn_w2"][l // 2], f32)
        else:
            common["moe_router%d" % l] = np.ascontiguousarray(inputs["moe_router"][l // 2], f32)
            common["moe_w1_%d" % l] = np.ascontiguousarray(inputs["moe_w1"][l // 2], f32)
            common["moe_w3_%d" % l] = np.ascontiguousarray(inputs["moe_w3"][l // 2], f32)
            common["moe_w2_%d" % l] = np.ascontiguousarray(inputs["moe_w2"][l // 2], f32)
    in_maps = []
    for core in range(NCORES):
        m = dict(common)
        xs = []
        for s in range(SEQS):
            b = core * SEQS + s
            xt = np.concatenate([x[b].T, ctx[b].T], axis=1)
            xs.append(xt.reshape(8, 128, T))
        m["xT"] = np.ascontiguousarray(np.stack(xs))
        cs = np.stack([c[core * SEQS], c[core * SEQS + 1], c_ctx], axis=1)
        m["cT"] = np.ascontiguousarray(cs.reshape(8, 128, 3).transpose(1, 0, 2))
        in_maps.append(m)
    return in_maps, pm_lists, PM.shape[1]


def kernel(**inputs):
    layers = (0, 1, 2, 3)
    in_maps, pm_lists, pm_slots = _host_inputs(inputs, layers)
    nc = build(layers, None, pm_lists, pm_slots)
    res = run_bass_kernel_spmd(nc, in_maps, core_ids=list(range(NCORES)))
    out = np.empty((NCORES * SEQS, L, D), np.float32)
    for core in range(NCORES):
        o = res.results[core]["outT"]
        for s in range(SEQS):
            out[core * SEQS + s] = o[s].reshape(D, L).T
    return out
```

```python
import numpy as np
import ml_dtypes
from contextlib import ExitStack
import concourse.bass as bass
import concourse.mybir as mybir
from concourse.bass_utils import run_bass_kernel_spmd

F32 = mybir.dt.float32
BF16 = mybir.dt.bfloat16
AF = mybir.ActivationFunctionType
ALU = mybir.AluOpType

D = 1024
L = 4096
NCTX = 256
T = L + NCTX
NT = T // 128
DEPTH = 4
GRID = 64
WINS = (2, 4, 8, 16)
DFF = 2816
EFF = 3584
NEXP = 8
ALPHA = (2 * DEPTH) ** 0.25
EPS = 1e-5
NCORES = 8
SEQS = 2


class Prog:
    ENGS = ("pe", "dve", "act", "pool", "sp")

    def __init__(self, nc, es):
        self.nc = nc
        self.es = es
        self.streams = {e: [] for e in self.ENGS}
        self.cnt = {e: 0 for e in self.ENGS}
        self.sem = {e: es.enter_context(nc.semaphore("s_" + e)) for e in ("pe", "dve", "act", "pool")}
        self.dsem = {}
        self.res = {}
        self.known = {e: {} for e in self.ENGS}

    def _dma_sem(self, key):
        if key not in self.dsem:
            self.dsem[key] = [self.es.enter_context(self.nc.semaphore("d%d" % len(self.dsem))), 0]
        return self.dsem[key]

    def _need(self, eng, toks):
        need = {}
        for kind, (teng, sem, val) in toks:
            if teng == eng and eng in ("pe", "sp"):
                continue
            k = id(sem)
            if k not in need or need[k][1] < val:
                need[k] = (sem, val)
        out = []
        kn = self.known[eng]
        for k, (sem, val) in need.items():
            if kn.get(k, 0) >= val:
                continue
            kn[k] = val
            out.append((sem, val))
        return out

    def _deps(self, eng, reads, writes):
        toks = []
        for r in reads:
            st = self.res.get(r)
            if st and st["w"] is not None:
                toks.append(("raw", st["w"]))
        for w in writes:
            st = self.res.get(w)
            if st:
                if st["w"] is not None:
                    toks.append(("waw", st["w"]))
                for t in st["r"]:
                    toks.append(("war", t))
        return self._need(eng, toks)

    def _commit(self, tok, reads, writes):
        for r in reads:
            st = self.res.setdefault(r, {"w": None, "r": []})
            st["r"].append(tok)
        for w in writes:
            self.res[w] = {"w": tok, "r": []}

    def op(self, eng, fn, reads=(), writes=()):
        waits = self._deps(eng, reads, writes)
        self.cnt[eng] += 1
        sem = self.sem[eng]
        self.streams[eng].append((waits, fn, sem, 1))
        self._commit((eng, sem, self.cnt[eng]), reads, writes)

    def dma(self, q, out, in_, reads, writes, key, **kw):
        waits = self._deps(q, reads, writes)
        ds = self._dma_sem(key)
        ds[1] += 16
        sem, val = ds[0], ds[1]

        def fn(e, out=out, in_=in_, kw=kw):
            return e.dma_start(out=out, in_=in_, **kw)

        self.streams[q].append((waits, fn, sem, 16))
        self._commit(("dma", sem, val), reads, writes)

    def barrier(self):
        toks = []
        for e in ("pe", "dve", "act", "pool"):
            if self.cnt[e]:
                toks.append(("raw", (e, self.sem[e], self.cnt[e])))
        for key, (sem, val) in self.dsem.items():
            if val:
                toks.append(("raw", ("dma", sem, val)))
        for e in self.ENGS:
            waits = self._need(e, [t for t in toks if t[1][0] != e or e == "sp"])
            if waits:
                self.streams[e].append((waits, None, None, 0))
        self.res = {}

    def emit(self, block):
        engs = {"pe": block.tensor, "dve": block.vector, "act": block.scalar, "pool": block.gpsimd, "sp": block.sync}
        for name, deco in engs.items():
            stream = self.streams[name]
            if not stream:
                continue

            def body(e, stream=stream):
                for waits, fn, sem, inc in stream:
                    for ws, wv in waits:
                        e.wait_ge(ws, wv)
                    if fn is not None:
                        fn(e).then_inc(sem, inc)

            deco(body)
            self.streams[name] = []


def _box_matrix(n, w):
    left = w // 2
    right = w - 1 - left
    A = np.zeros((n, n), np.float64)
    for t in range(n):
        lo = max(t - left, 0)
        hi = min(t + right + 1, n)
        A[t, lo:hi] = 1.0 / (hi - lo)
    return A


def _pool_constants():
    sets = []
    lists = []
    mats = []
    for si, ob in enumerate((0, 3, 7)):
        lst_g = []
        for g, w in enumerate(WINS):
            A = _box_matrix(GRID, w)
            lst = []
            rows_out = np.arange(8 * ob, 8 * ob + 8)
            for tin in range(32):
                rows_in = np.arange(2 * tin, 2 * tin + 2)
                Ar = A[np.ix_(rows_out, rows_in)]
                if not np.any(Ar):
                    continue
                M = np.kron(Ar, A)
                if tin // 4 == ob:
                    o0 = (tin - 4 * ob) * 128
                    M[o0:o0 + 128, :] -= np.eye(128)
                lst.append((tin - 4 * ob, len(mats)))
                mats.append(M.T.astype(np.float32))
            lst_g.append(lst)
        lists.append(lst_g)
    out_sets = []
    out_lists = []
    for si in range(3):
        slots = []
        lg = []
        for g in range(4):
            l2 = []
            for rel, mi in lists[si][g]:
                l2.append((rel, len(slots)))
                slots.append(mats[mi])
            lg.append(l2)
        out_sets.append(np.stack(slots))
        out_lists.append(lg)
    nmax = max(s.shape[0] for s in out_sets)
    PM = np.zeros((3, nmax, 128, 512), np.float32)
    for si in range(3):
        PM[si, :out_sets[si].shape[0]] = out_sets[si]
    PMC = np.zeros((4, 2, 128, 256), np.float32)
    for g, w in enumerate(WINS):
        A = _box_matrix(NCTX, w) - np.eye(NCTX)
        for tin in range(2):
            PMC[g, tin] = A[:, tin * 128:(tin + 1) * 128].T
    return PM.astype(ml_dtypes.bfloat16), out_lists, PMC.astype(ml_dtypes.bfloat16)


def _rope_tables():
    t = np.arange(L)
    row = (t // GRID).astype(np.float32)
    col = (t % GRID).astype(np.float32)
    n_freq = 32
    inv = np.exp(-np.log(np.float32(10000.0)) * np.arange(n_freq, dtype=np.float32) / n_freq).astype(np.float32)
    ang = np.concatenate([row[:, None] * inv, col[:, None] * inv], -1).astype(np.float32)
    cos = np.ones((T, 64), np.float32)
    sin = np.zeros((T, 64), np.float32)
    cos[:L] = np.cos(ang)
    sin[:L] = np.sin(ang)
    cosT = np.ascontiguousarray(cos.reshape(NT, 128, 64).transpose(1, 0, 2))
    sinT = np.ascontiguousarray(sin.reshape(NT, 128, 64).transpose(1, 0, 2))
    return cosT, sinT


def _misc_constants():
    j = np.arange(128, dtype=np.float32)
    E = np.zeros((128, 16), np.float32)
    for h in range(4):
        E[:, 0 + h] = j - 127.0
        E[:, 4 + h] = -j
        E[:, 8 + h] = 127.0 - j
        E[:, 12 + h] = j
    jj = np.arange(128)[:, None]
    ii = np.arange(128)[None, :]
    masks = np.zeros((128, 2, 128), np.float32)
    masks[:, 0, :] = (ii >= jj)
    masks[:, 1, :] = (ii <= jj)
    return E, masks


def build(layers=(0, 1, 2, 3), stop_after=None, pm_lists=None, pm_slots=31):
    nc = bass.Bass("TRN2", target_bir_lowering=False)
    dt = nc.dram_tensor

    def inp(name, shape, dtype=F32):
        return dt(name, list(shape), dtype, kind="ExternalInput").ap()

    def scr(name, shape, dtype):
        return dt(name, list(shape), dtype).ap()

    xT_in = inp("xT", [SEQS, 8, 128, T])
    cT_in = inp("cT", [128, 8, 3])
    cos_in = inp("cosT", [128, NT, 64])
    sin_in = inp("sinT", [128, NT, 64])
    etab_in = inp("etab", [128, 16])
    mask_in = inp("masks", [128, 2, 128])
    pm_in = inp("pm", [3, pm_slots, 128, 512], BF16)
    pmc_in = inp("pmc", [4, 2, 128, 256], BF16)
    ident_in = inp("ident", [128, 128])
    W = {}
    for l in layers:
        W[l] = dict(
            ada_w=inp("ada_w%d" % l, [D, 6 * D]),
            ada_b=inp("ada_bT%d" % l, [128, 48]),
            w_in=inp("w_in%d" % l, [D, 5632]),
            pool_w=inp("pool_w%d" % l, [4, 128, 128]),
            psc=inp("pscT%d" % l, [128, 4]),
            wpo=inp("w_pool_out%d" % l, [512, D]),
            wro=inp("w_ret_out%d" % l, [D, D]),
            logit=inp("logit_bc%d" % l, [128, 8]),
            wo=inp("w_out%d" % l, [D, D]),
            lnp=inp("lnT%d" % l, [128, 4, 8]),
        )
        if l % 2 == 0:
            W[l].update(
                w1=inp("ffn_w1_%d" % l, [D, DFF]),
                w3=inp("ffn_w3_%d" % l, [D, DFF]),
                w2=inp("ffn_w2_%d" % l, [DFF, D]),
            )
        else:
            W[l].update(
                wr=inp("moe_router%d" % l, [D, NEXP]),
                w1=inp("moe_w1_%d" % l, [NEXP, D, EFF]),
                w3=inp("moe_w3_%d" % l, [NEXP, D, EFF]),
                w2=inp("moe_w2_%d" % l, [NEXP, EFF, D]),
            )
    outT = dt("outT", [SEQS, 8, 128, L], F32, kind="ExternalOutput").ap()

    XS = scr("XS", [SEQS, 8, 128, T], F32)
    U_d = scr("U_d", [SEQS, T, 512], BF16)
    V_d = scr("V_d", [SEQS, T, 1024], BF16)
    KF_d = scr("KF_d", [SEQS, T, 512], BF16)
    KB_d = scr("KB_d", [SEQS, T, 512], BF16)
    QKT_d = scr("QKT_d", [SEQS, 16, 128, T], BF16)
    G_d = [scr("G%d_d" % i, [SEQS, 8, 128, T], BF16) for i in range(3)]
    DST_d = scr("DST_d", [SEQS, 2, NT, 4, 128, 256], BF16)
    ZR_d = scr("ZR_d", [SEQS, 8, 128, T], BF16)
    WC_d = scr("WC_d", [NEXP, 7, 3, 128, 4096], BF16)

    blocks = [(i * 512, 512, False) for i in range(8)] + [(L, NCTX, True)]

    with ExitStack() as es:
        P = Prog(nc, es)
        block = es.enter_context(nc.Block())
        sb = lambda name, shape, dtype=F32: es.enter_context(nc.sbuf_tensor(name, list(shape), dtype))
        ident_f = sb("ident_f", [128, 128])
        ident_b = sb("ident_b", [128, 128], BF16)
        ones_b = sb("ones_b", [128, 128], BF16)
        ones_f = sb("ones_f", [128, 128])
        masks = sb("masks_s", [128, 2, 128])
        etab = sb("etab_s", [128, 16])
        cT = sb("cT_s", [128, 8, 3])
        sT = sb("sT_s", [128, 8, 3])
        MOD = sb("MOD", [128, 48, 3])
        lnp = sb("lnp", [128, 4, 8])
        psc = sb("psc", [128, 4])
        lgt = sb("lgt", [128, 8])
        lg16 = sb("lg16", [128, 16])
        DEC = sb("DEC", [128, 16])
        GC = sb("GC", [128, 8])
        epsb = sb("epsb", [128, 2])
        PS = [es.enter_context(nc.psum_tensor("ps%d" % i, [128, 512], F32)) for i in range(8)]

        P.dma("sp", ident_f[:], ident_in, [], ["ident_f"], "c0")
        P.dma("sp", masks[:], mask_in, [], ["masks"], "c1")
        P.dma("sp", etab[:], etab_in, [], ["etab"], "c2")
        P.dma("sp", cT[:], cT_in, [], ["cT"], "c3")
        P.op("dve", lambda e: e.tensor_copy(out=ident_b[:], in_=ident_f[:]), ["ident_f"], ["ident_b"])
        P.op("dve", lambda e: e.memset(ones_b[:], 1.0 / 1024.0), [], ["ones_b"])
        P.op("dve", lambda e: e.memset(ones_f[:], 1.0 / 256.0), [], ["ones_f"])
        P.op("dve", lambda e: e.memset(epsb[:, 0:1], EPS), [], ["epsb0"])
        P.op("dve", lambda e: e.memset(epsb[:, 1:2], EPS / (ALPHA * ALPHA)), [], ["epsb1"])
        P.op("act", lambda e: e.activation(out=sT[:], in_=cT[:], func=AF.Silu), ["cT"], ["sT"])

        def stage_end():
            P.barrier()
            P.emit(block)

        def ln_gen(ss, xb, n, gk, bk, epscol, pm_i, pe_i, XK="xb", kp="", zb_extra=(), sq_extra=()):
            zb, sq, ms, m2, rs = ss["zb"], ss["sq"], ss["ms"], ss["m2"], ss["rs"]
            kz, kq, km, k2, kr = (kp, "zb"), (kp, "sq"), (kp, "ms"), (kp, "m2"), (kp, "rs")
            P.op("act", lambda e: e.activation(out=zb[:, :, :n], in_=xb[:, :, :n], func=AF.Copy), [XK], [kz] + list(zb_extra))
            yield
            P.op("act", lambda e: e.activation(out=sq[:, :, :n], in_=xb[:, :, :n], func=AF.Square), [XK], [kq] + list(sq_extra))
            yield

            def mm1(e):
                for k in range(8):
                    r = e.matmul(PS[pm_i][:, :n], lhsT=ones_b[:], rhs=zb[:, k, :n], start=(k == 0), stop=(k == 7))
                return r

            def mm2(e):
                for k in range(8):
                    r = e.matmul(PS[pe_i][:, :n], lhsT=ones_b[:], rhs=sq[:, k, :n], start=(k == 0), stop=(k == 7))
                return r

            P.op("pe", mm1, [kz, "ones_b"], [("ps", pm_i)])
            yield
            P.op("pe", mm2, [kq, "ones_b"], [("ps", pe_i)])
            yield
            P.op("act", lambda e: e.activation(out=ms[:, :n], in_=PS[pm_i][:, :n], func=AF.Copy), [("ps", pm_i)], [km])
            yield
            P.op("act", lambda e: e.activation(out=m2[:, :n], in_=PS[pm_i][:, :n], func=AF.Square), [("ps", pm_i)], [k2])
            yield
            P.op("dve", lambda e: e.tensor_tensor(out=rs[:, :n], in0=PS[pe_i][:, :n], in1=m2[:, :n], op=ALU.subtract),
                 [("ps", pe_i), k2], [kr])
            yield
            P.op("act", lambda e: e.activation(out=rs[:, :n], in_=rs[:, :n], func=AF.Ln, bias=epsb[:, epscol:epscol + 1]),
                 [kr, "epsb%d" % epscol], [kr])
            yield
            P.op("act", lambda e: e.activation(out=rs[:, :n], in_=rs[:, :n], func=AF.Exp, scale=-0.5), [kr], [kr])
            yield
            P.op("dve", lambda e: e.tensor_tensor(out=xb[:, :, :n], in0=xb[:, :, :n],
                                                  in1=ms[:, :n].unsqueeze(1).to_broadcast([128, 8, n]), op=ALU.subtract),
                 [XK, km], [XK])
            yield
            P.op("dve", lambda e: e.tensor_tensor(out=xb[:, :, :n], in0=xb[:, :, :n],
                                                  in1=rs[:, :n].unsqueeze(1).to_broadcast([128, 8, n]), op=ALU.mult),
                 [XK, kr], [XK])
            yield
            for k in range(8):
                P.op("act", lambda e, k=k: e.activation(out=xb[:, k, :n], in_=xb[:, k, :n], func=AF.Identity,
                                                         scale=lnp[:, gk, k:k + 1], bias=lnp[:, bk, k:k + 1]),
                     [XK, "lnp"], [XK])
                yield

        def ln_block(*a, **k):
            for _ in ln_gen(*a, **k):
                pass

        def interleave(gens, lead=0):
            live = list(gens)
            for _ in range(lead):
                try:
                    next(live[0])
                except StopIteration:
                    live[0] = None
                    break
            while any(g is not None for g in live):
                for i, g in enumerate(live):
                    if g is None:
                        continue
                    try:
                        next(g)
                    except StopIteration:
                        live[i] = None

        def load_cast(dst, src_ap, nk, ncols, stage, tag, piece=512):
            srcv = src_ap.rearrange("(k p) n -> p k n", p=128)
            i = 0
            for c0 in range(0, ncols, piece):
                cw = min(piece, ncols - c0)
                for k0 in range(0, nk, 8):
                    kw = min(8, nk - k0)
                    st = stage[i % 2]
                    P.dma("sp", st[:, :kw, :cw], srcv[:, k0:k0 + kw, c0:c0 + cw], [], [("wst", i % 2)], ("wst", i % 2))
                    if i % 2 == 0:
                        P.op("act", lambda e, st=st, kw=kw, cw=cw, k0=k0, c0=c0: e.activation(
                            out=dst[:, k0:k0 + kw, c0:c0 + cw], in_=st[:, :kw, :cw], func=AF.Copy), [("wst", i % 2)], [(tag, i)])
                    else:
                        P.op("dve", lambda e, st=st, kw=kw, cw=cw, k0=k0, c0=c0: e.tensor_copy(
                            out=dst[:, k0:k0 + kw, c0:c0 + cw], in_=st[:, :kw, :cw]), [("wst", i % 2)], [(tag, i)])
                    i += 1

        for li, l in enumerate(layers):
            w = W[l]
            xsrc = xT_in if li == 0 else XS
            need_ctx = l < DEPTH - 1
            with ExitStack() as ls:
                lsb = lambda name, shape, dtype=F32: ls.enter_context(nc.sbuf_tensor(name + "_L%d" % l, list(shape), dtype))
                adst = [lsb("adst%d" % i, [128, 8, 768]) for i in range(2)]
                adb = lsb("adb", [128, 48])
                P.dma("sp", adb[:], w["ada_b"], [], ["adb"], "p0")
                P.dma("sp", lnp[:], w["lnp"], [], ["lnp"], "p1")
                P.dma("sp", psc[:], w["psc"], [], ["psc"], "p2")
                P.dma("sp", lgt[:], w["logit"], [], ["lgt"], "p3")
                adv = w["ada_w"].rearrange("(k p) n -> p k n", p=128)
                for pc in range(8):
                    st = adst[pc % 2]
                    P.dma("sp", st[:], adv[:, :, pc * 768:(pc + 1) * 768], [], [("adst", pc % 2)], ("adst", pc % 2))

                    def mm(e, st=st, pc=pc):
                        for oc in range(6):
                            og = pc * 6 + oc
                            for k in range(8):
                                r = e.matmul(PS[0][:, og * 3:og * 3 + 3], lhsT=st[:, k, oc * 128:(oc + 1) * 128],
                                             rhs=sT[:, k, :], start=(k == 0), stop=(k == 7))
                        return r

                    P.op("pe", mm, [("adst", pc % 2), "sT"], [("ps", 0)])
                P.op("dve", lambda e: e.tensor_tensor(out=MOD[:], in0=PS[0][:, 0:144].rearrange("p (a b) -> p a b", b=3),
                                                      in1=adb[:].unsqueeze(2).to_broadcast([128, 48, 3]), op=ALU.add),
                     [("ps", 0), "adb"], ["MOD"])
                P.op("dve", lambda e: e.tensor_scalar(out=MOD[:, 8:16, :], in0=MOD[:, 8:16, :], scalar1=1.0, scalar2=None, op0=ALU.add), ["MOD"], ["MOD"])
                P.op("dve", lambda e: e.tensor_scalar(out=MOD[:, 16:24, :], in0=MOD[:, 16:24, :], scalar1=1.0 / ALPHA, scalar2=None, op0=ALU.mult), ["MOD"], ["MOD"])
                P.op("dve", lambda e: e.tensor_scalar(out=MOD[:, 32:40, :], in0=MOD[:, 32:40, :], scalar1=1.0, scalar2=None, op0=ALU.add), ["MOD"], ["MOD"])
                P.op("dve", lambda e: e.tensor_scalar(out=MOD[:, 40:48, :], in0=MOD[:, 40:48, :], scalar1=1.0 / ALPHA, scalar2=None, op0=ALU.mult), ["MOD"], ["MOD"])
                P.op("act", lambda e: e.activation(out=lgt[:], in_=lgt[:], func=AF.Exp, scale=-1.0), ["lgt"], ["lgt"])
                P.op("act", lambda e: e.activation(out=lgt[:], in_=lgt[:], func=AF.Ln, bias=1.0), ["lgt"], ["lgt"])
                P.op("dve", lambda e: e.tensor_scalar(out=lg16[:, 0:8], in0=lgt[:], scalar1=-1.0, scalar2=None, op0=ALU.mult), ["lgt"], ["lg16"])
                P.op("dve", lambda e: e.tensor_scalar(out=lg16[:, 8:16], in0=lgt[:], scalar1=-1.0, scalar2=None, op0=ALU.mult), ["lgt", "lg16"], ["lg16"])
                P.op("act", lambda e: e.activation(out=GC[:], in_=lg16[:, 0:8], func=AF.Exp, scale=128.0), ["lg16"], ["GC"])
                P.op("dve", lambda e: e.tensor_tensor(out=DEC[:], in0=lg16[:], in1=etab[:], op=ALU.mult), ["lg16", "etab"], ["DEC"])
                P.op("act", lambda e: e.activation(out=DEC[:], in_=DEC[:], func=AF.Exp), ["DEC"], ["DEC"])
                P.op("dve", lambda e: e.tensor_scalar(out=DEC[:, 8:16], in0=DEC[:, 8:16], scalar1=128.0 ** -0.5, scalar2=None, op0=ALU.mult), ["DEC"], ["DEC"])
                stage_end()
            if stop_after == ("params", l):
                break

            def modp(m, k, tc):
                return MOD[:, m * 8 + k, tc:tc + 1]

            with ExitStack() as ls:
                lsb = lambda name, shape, dtype=F32: ls.enter_context(nc.sbuf_tensor(name + "_L%d" % l, list(shape), dtype))
                WB = lsb("WB", [128, 8, 5632], BF16)
                with ExitStack() as ls2:
                    wst = [ls2.enter_context(nc.sbuf_tensor("wst%d_L%d" % (i, l), [128, 8, 512], F32)) for i in range(2)]
                    load_cast(WB, w["w_in"], 8, 5632, wst, "WB")
                    stage_end()
                cosT = lsb("cosT_s", [128, NT, 64])
                sinT = lsb("sinT_s", [128, NT, 64])
                P.dma("sp", cosT[:], cos_in, [], ["cosT"], "c4")
                P.dma("sp", sinT[:], sin_in, [], ["sinT"], "c5")
                xbs = [lsb("xa%d" % i, [128, 8, 512]) for i in range(2)]
                hb = [lsb("ha%d" % i, [128, 8, 512], BF16) for i in range(1)]
                ub = lsb("ub", [128, 512], BF16)
                vb = lsb("vb", [128, 1024], BF16)
                rt = [lsb("rt%d" % i, [128, 4, 64]) for i in range(4)]
                rots = [lsb("rot%d" % i, [128, 4, 2, 64]) for i in range(2)]
                var_tm = lsb("var_tm", [128, 4, 512], BF16)
                qkT = lsb("qkT", [128, 16, 512], BF16)
                gb = [lsb("gb%d" % i, [128, 8, 512], BF16) for i in range(3)]
                bi = 0
                for s in range(SEQS):
                    for (t0, n, isctx) in blocks:
                        tc = 2 if isctx else s
                        xb = xbs[bi % 2]
                        h = hb[0]
                        xk, hk = ("xa", bi % 2), ("ha", 0)
                        P.dma("sp", xb[:, :, :n], xsrc[s, :, :, t0:t0 + n].rearrange("k p t -> p k t"),
                              [("XS", s, t0)], [xk], xk)
                        for k in range(8):
                            P.op("act", lambda e, k=k, xb=xb, h=h, n=n, tc=tc: e.activation(
                                out=h[:, k, :n], in_=xb[:, k, :n], func=AF.Identity,
                                scale=modp(1, k, tc), bias=modp(0, k, tc)), [xk, "MOD"], [hk])
                        pending = []
                        gate_list = [(gi, oc) for gi in range(3) for oc in range(8)]

                        def emit_gate(gi, oc, h=h, n=n, hk=hk):
                            func = AF.Silu if gi == 0 else AF.Sigmoid
                            bnk = 6 + (oc % 2)
                            c0 = 2560 + gi * 1024 + oc * 128

                            def mm(e):
                                for k in range(8):
                                    r = e.matmul(PS[bnk][:, :n], lhsT=WB[:, k, c0:c0 + 128], rhs=h[:, k, :n],
                                                 start=(k == 0), stop=(k == 7))
                                return r
                            P.op("pe", mm, [hk, "WB"], [("ps", bnk)])
                            P.op("act", lambda e: e.activation(out=gb[gi][:, oc, :n], in_=PS[bnk][:, :n], func=func),
                                 [("ps", bnk)], [("gb", gi, oc)])

                        for ti in range(n // 128):
                            gt = (t0 // 128) + ti
                            tsl = slice(ti * 128, (ti + 1) * 128)
                            rows = slice(t0 + ti * 128, t0 + ti * 128 + 128)
                            for bnk, c0 in enumerate((0, 512, 1024, 1536, 2048)):
                                def mm(e, bnk=bnk, c0=c0, h=h, tsl=tsl):
                                    for k in range(8):
                                        r = e.matmul(PS[bnk][:, :], lhsT=h[:, k, tsl], rhs=WB[:, k, c0:c0 + 512],
                                                     start=(k == 0), stop=(k == 7))
                                    return r
                                P.op("pe", mm, [hk, "WB"], [("ps", bnk)])
                            P.op("act", lambda e: e.activation(out=ub[:], in_=PS[0][:, :], func=AF.Copy), [("ps", 0)], ["ub"])
                            P.dma("sp", U_d[s, rows, :], ub[:], ["ub"], [("U", s, gt)], "ub")
                            P.op("act", lambda e: e.activation(out=vb[:, 0:512], in_=PS[3][:, :], func=AF.Copy), [("ps", 3)], ["vb0"])
                            P.op("dve", lambda e: e.tensor_copy(out=vb[:, 512:1024], in_=PS[4][:, :]), [("ps", 4)], ["vb1"])
                            P.dma("sp", V_d[s, rows, :], vb[:], ["vb0", "vb1"], [("V", s, gt)], "vb")
                            for qi, bnk in enumerate((1, 2)):
                                pv = PS[bnk][:, :].rearrange("p (h two d) -> p h two d", two=2, d=64)
                                Cb = cosT[:, gt, :].unsqueeze(1).to_broadcast([128, 4, 64])
                                Sb = sinT[:, gt, :].unsqueeze(1).to_broadcast([128, 4, 64])
                                rq = rots[qi]
                                P.op("dve", lambda e, pv=pv, Cb=Cb: e.tensor_tensor(out=rt[0][:], in0=pv[:, :, 0, :], in1=Cb, op=ALU.mult), [("ps", bnk), "cosT"], ["rt0"])
                                P.op("dve", lambda e, pv=pv, Sb=Sb: e.tensor_tensor(out=rt[1][:], in0=pv[:, :, 1, :], in1=Sb, op=ALU.mult), [("ps", bnk), "sinT"], ["rt1"])
                                P.op("dve", lambda e, pv=pv, Sb=Sb: e.tensor_tensor(out=rt[2][:], in0=pv[:, :, 0, :], in1=Sb, op=ALU.mult), [("ps", bnk), "sinT"], ["rt2"])
                                P.op("dve", lambda e, pv=pv, Cb=Cb: e.tensor_tensor(out=rt[3][:], in0=pv[:, :, 1, :], in1=Cb, op=ALU.mult), [("ps", bnk), "cosT"], ["rt3"])
                                P.op("dve", lambda e, rq=rq: e.tensor_tensor(out=rq[:, :, 0, :], in0=rt[0][:], in1=rt[1][:], op=ALU.subtract), ["rt0", "rt1"], [("rot0", qi)])
                                P.op("dve", lambda e, rq=rq: e.tensor_tensor(out=rq[:, :, 1, :], in0=rt[2][:], in1=rt[3][:], op=ALU.add), ["rt2", "rt3"], [("rot1", qi)])
                            while pending:
                                pending.pop(0)()
                            ngl = len(gate_list)
                            ntl_ = n // 128
                            for (gi, oc) in gate_list[ti * ngl // ntl_:(ti + 1) * ngl // ntl_]:
                                emit_gate(gi, oc)
                            for qi in range(2):
                                rq = rots[qi]
                                for dr in range(2):
                                    vi = qi * 2 + dr
                                    for hh in range(4):
                                        P.op("act", lambda e, vi=vi, hh=hh, dr=dr, qi=qi, rq=rq: e.activation(
                                            out=var_tm[:, vi, hh * 128:(hh + 1) * 128],
                                            in_=rq[:, hh, :, :].rearrange("p a b -> p (a b)"), func=AF.Identity,
                                            scale=DEC[:, qi * 8 + dr * 4 + hh:qi * 8 + dr * 4 + hh + 1]),
                                            [("rot0", qi), ("rot1", qi), "DEC"], [("var", vi)])
                            def tail(rows=rows, tsl=tsl, gt=gt, s=s):
                                P.dma("sp", KF_d[s, rows, :], var_tm[:, 2, :], [("var", 2)], [("KF", s, gt)], "kf")
                                P.dma("sp", KB_d[s, rows, :], var_tm[:, 3, :], [("var", 3)], [("KB", s, gt)], "kb")
                                p5 = PS[5][:, :].bitcast(BF16).rearrange("p (a b) -> p a b", b=128)[:, 0:8, :]
                                for half in range(2):
                                    def tr(e, half=half):
                                        for j in range(8):
                                            vi = half * 2 + j // 4
                                            hh = j % 4
                                            r = e.transpose(p5[:, j, :], var_tm[:, vi, hh * 128:(hh + 1) * 128], ident_b[:])
                                        return r
                                    P.op("pe", tr, [("var", half * 2), ("var", half * 2 + 1), "ident_b"], [("ps", 5)])
                                    P.op("dve", lambda e, half=half, tsl=tsl: e.tensor_copy(out=qkT[:, half * 8:(half + 1) * 8, tsl], in_=p5),
                                         [("ps", 5)], [("qkT", half)])
                            pending.append(tail)
                        while pending:
                            pending.pop(0)()
                        P.dma("sp", QKT_d[s, :, :, t0:t0 + n].rearrange("a p t -> p a t"), qkT[:, :, :n],
                              [("qkT", 0), ("qkT", 1)], [("QKT", s, t0)], "qkT")
                        for gi in range(3):
                            P.dma("sp", G_d[gi][s, :, :, t0:t0 + n].rearrange("k p t -> p k t"), gb[gi][:, :, :n],
                                  [("gb", gi, oc) for oc in range(8)], [("G", gi, s, t0)], ("gb", gi))
                        bi += 1
                stage_end()
            if stop_after == ("A", l):
                break

            with ExitStack() as ls:
                lsb = lambda name, shape, dtype=F32: ls.enter_context(nc.sbuf_tensor(name + "_L%d" % l, list(shape), dtype))
                Sst = lsb("Sst", [128, 1024])
                Df = lsb("Df", [128, 1024])
                Dbf = [lsb("Dbf%d" % i, [128, 1024], BF16) for i in range(2)]
                kt = [lsb("kt%d" % i, [128, 512], BF16) for i in range(2)]
                vt = [lsb("vt%d" % i, [128, 1024], BF16) for i in range(2)]
                it = 0
                for s in range(SEQS):
                    for dr in range(2):
                        order = [32, 33] + list(range(32)) if dr == 0 else [33, 32] + list(range(31, -1, -1))
                        Ksrc = KF_d if dr == 0 else KB_d
                        P.op("dve", lambda e: e.memset(Sst[:], 0.0), [], ["S"])
                        for c in order:
                            j = it % 2
                            rows = slice(c * 128, (c + 1) * 128)
                            P.dma("sp", kt[j][:], Ksrc[s, rows, :], [("KF", s), ("KB", s)], [("kt", j)], ("kt", j))
                            P.dma("sp", vt[j][:], V_d[s, rows, :], [("V", s)], [("vt", j)], ("vt", j))
                            for hh in range(4):
                                P.op("dve", lambda e, hh=hh, dr=dr: e.tensor_scalar(
                                    out=Df[:, hh * 256:(hh + 1) * 256], in0=Sst[:, hh * 256:(hh + 1) * 256],
                                    scalar1=GC[:, dr * 4 + hh:dr * 4 + hh + 1], scalar2=None, op0=ALU.mult), ["S", "GC"], [("Df", hh)])
                            P.op("act", lambda e, j=j: e.activation(out=Dbf[j][:], in_=Df[:], func=AF.Copy),
                                 [("Df", hh) for hh in range(4)], [("Dbf", j)])
                            P.dma("sp", DST_d[s, dr, c].rearrange("h p v -> p h v"),
                                  Dbf[j][:].rearrange("p (h v) -> p h v", v=256), [("Dbf", j)], [("DST", s, dr, c)], ("Dbf", j))

                            def mm(e, j=j):
                                for hh in range(4):
                                    r = e.matmul(PS[hh // 2][:, (hh % 2) * 256:(hh % 2) * 256 + 256],
                                                 lhsT=kt[j][:, hh * 128:(hh + 1) * 128], rhs=vt[j][:, hh * 256:(hh + 1) * 256],
                                                 start=True, stop=True)
                                return r
                            P.op("pe", mm, [("kt", j), ("vt", j)], [("ps", 0), ("ps", 1)])
                            for b2 in range(2):
                                P.op("dve", lambda e, b2=b2: e.tensor_tensor(
                                    out=Sst[:, b2 * 512:(b2 + 1) * 512], in0=PS[b2][:, :], in1=Df[:, b2 * 512:(b2 + 1) * 512], op=ALU.add),
                                    [("ps", b2), ("Df", 2 * b2), ("Df", 2 * b2 + 1)], ["S"])
                            it += 1
                stage_end()

            with ExitStack() as ls:
                lsb = lambda name, shape, dtype=F32: ls.enter_context(nc.sbuf_tensor(name + "_L%d" % l, list(shape), dtype))
                qks = [lsb("qk%d" % i, [128, 16, 128], BF16) for i in range(2)]
                vts = [lsb("vc%d" % i, [128, 1024], BF16) for i in range(2)]
                Dts = [lsb("Dt%d" % i, [128, 2, 4, 256], BF16) for i in range(2)]
                sgs = [lsb("sg%d" % i, [128, 8, 128], BF16) for i in range(2)]
                PTs = [lsb("PT%d" % i, [128, 8, 128], BF16) for i in range(2)]
                ofs = [lsb("of%d" % i, [128, 8, 128]) for i in range(2)]
                osqs = [lsb("osq%d" % i, [128, 8, 128]) for i in range(2)]
                msrs = [lsb("msr%d" % i, [128, 4, 128]) for i in range(2)]
                m2rs = [lsb("m2r%d" % i, [128, 4, 128]) for i in range(2)]
                rsrs = [lsb("rsr%d" % i, [128, 4, 128]) for i in range(2)]
                zrts = [lsb("zrt%d" % i, [128, 8, 128], BF16) for i in range(2)]
                ntl = NT if need_ctx else 32
                ctiles = [(s, c) for s in range(SEQS) for c in range(ntl)]
                fl = lambda t: t[:].rearrange("p a b -> p (a b)")

                def gen_c1(j, s, c):
                    qk, vt, Dt, sg, PT, of, osq = qks[j], vts[j], Dts[j], sgs[j], PTs[j], ofs[j], osqs[j]
                    msr, m2r, rsr, zrt = msrs[j], m2rs[j], rsrs[j], zrts[j]
                    B0 = 4 * j
                    cs = slice(c * 128, (c + 1) * 128)
                    P.dma("sp", qk[:], QKT_d[s, :, :, cs].rearrange("a p t -> p a t"), [], [("qk", j)], ("qk", j))
                    P.dma("sp", vt[:], V_d[s, cs, :], [], [("vc", j)], ("vc", j))
                    for dr in range(2):
                        P.dma("sp", Dt[:, dr], DST_d[s, dr, c].rearrange("h p v -> p h v"), [], [("Dt", j)], ("Dt", j))
                    P.dma("sp", sg[:], G_d[0][s, :, :, cs].rearrange("k p t -> p k t"), [], [("sg", j)], ("sg", j))
                    yield
                    for dr in range(2):
                        def mm(e, dr=dr):
                            for hh in range(4):
                                r = e.matmul(PS[B0 + dr][:, hh * 128:(hh + 1) * 128], lhsT=qk[:, (2 + dr) * 4 + hh, :],
                                             rhs=qk[:, dr * 4 + hh, :], start=True, stop=True)
                            return r
                        P.op("pe", mm, [("qk", j)], [("ps", B0 + dr)])
                        yield
                        P.op("dve", lambda e, dr=dr: e.tensor_tensor(
                            out=PT[:, dr * 4:(dr + 1) * 4, :], in0=PS[B0 + dr][:, :].rearrange("p (h i) -> p h i", i=128),
                            in1=masks[:, dr, :].unsqueeze(1).to_broadcast([128, 4, 128]), op=ALU.mult),
                            [("ps", B0 + dr), "masks"], [("PT", j, dr)])
                        yield
                    for b2 in range(2):
                        def mm(e, b2=b2):
                            for q in range(4):
                                ch = b2 * 4 + q
                                hh, m = ch // 2, ch % 2
                                o = PS[B0 + 2 + b2][:, q * 128:(q + 1) * 128]
                                for dr in range(2):
                                    e.matmul(o, lhsT=vt[:, hh * 256 + m * 128:hh * 256 + m * 128 + 128],
                                             rhs=PT[:, dr * 4 + hh, :], start=(dr == 0), stop=False)
                                    r = e.matmul(o, lhsT=Dt[:, dr, hh, m * 128:(m + 1) * 128],
                                                 rhs=qk[:, dr * 4 + hh, :], start=False, stop=(dr == 1))
                            return r
                        P.op("pe", mm, [("vc", j), ("PT", j, 0), ("PT", j, 1), ("Dt", j), ("qk", j)], [("ps", B0 + 2 + b2)])
                        yield
                        P.op("act", lambda e, b2=b2: e.activation(out=of[:, b2 * 4:(b2 + 1) * 4, :].rearrange("p a b -> p (a b)"),
                                                                 in_=PS[B0 + 2 + b2][:, :], func=AF.Copy), [("ps", B0 + 2 + b2)], [("of", j, b2)])
                        yield
                        P.op("act", lambda e, b2=b2: e.activation(out=osq[:, b2 * 4:(b2 + 1) * 4, :].rearrange("p a b -> p (a b)"),
                                                                 in_=PS[B0 + 2 + b2][:, :], func=AF.Square), [("ps", B0 + 2 + b2)], [("osq", j, b2)])
                        yield

                    def mmst(e):
                        for hh in range(4):
                            for m in range(2):
                                e.matmul(PS[B0][:, hh * 128:(hh + 1) * 128], lhsT=ones_f[:], rhs=of[:, hh * 2 + m, :],
                                         start=(m == 0), stop=(m == 1))
                        for hh in range(4):
                            for m in range(2):
                                r = e.matmul(PS[B0 + 1][:, hh * 128:(hh + 1) * 128], lhsT=ones_f[:], rhs=osq[:, hh * 2 + m, :],
                                             start=(m == 0), stop=(m == 1))
                        return r
                    okeys = [("of", j, 0), ("of", j, 1)]
                    P.op("pe", mmst, okeys + [("osq", j, 0), ("osq", j, 1), "ones_f"], [("ps", B0), ("ps", B0 + 1)])
                    yield
                    P.op("act", lambda e: e.activation(out=fl(msr), in_=PS[B0][:, :], func=AF.Copy), [("ps", B0)], [("msr", j)])
                    yield
                    P.op("act", lambda e: e.activation(out=fl(m2r), in_=PS[B0][:, :], func=AF.Square), [("ps", B0)], [("m2r", j)])
                    yield
                    P.op("dve", lambda e: e.tensor_tensor(out=fl(rsr), in0=PS[B0 + 1][:, :], in1=fl(m2r), op=ALU.subtract), [("ps", B0 + 1), ("m2r", j)], [("rsr", j)])
                    yield
                    P.op("act", lambda e: e.activation(out=fl(rsr), in_=fl(rsr), func=AF.Ln, bias=epsb[:, 0:1]), [("rsr", j), "epsb0"], [("rsr", j)])
                    yield
                    P.op("act", lambda e: e.activation(out=fl(rsr), in_=fl(rsr), func=AF.Exp, scale=-0.5), [("rsr", j)], [("rsr", j)])
                    yield
                    ov = of[:].rearrange("p (h m) t -> p h m t", m=2)
                    P.op("dve", lambda e: e.tensor_tensor(out=ov, in0=ov, in1=msr[:].unsqueeze(2).to_broadcast([128, 4, 2, 128]), op=ALU.subtract),
                         okeys + [("msr", j)], okeys)
                    yield
                    P.op("dve", lambda e: e.tensor_tensor(out=ov, in0=ov, in1=rsr[:].unsqueeze(2).to_broadcast([128, 4, 2, 128]), op=ALU.mult),
                         okeys + [("rsr", j)], okeys)
                    yield
                    P.op("dve", lambda e: e.tensor_tensor(out=zrt[:], in0=of[:], in1=sg[:], op=ALU.mult),
                         okeys + [("sg", j)], [("zrt", j)])
                    yield
                    P.dma("sp", ZR_d[s, :, :, cs].rearrange("k p t -> p k t"), zrt[:], [("zrt", j)], [("ZR", s, c)], ("zrt", j))
                    yield

                def stream_c1(j):
                    for (s, c) in ctiles[j::2]:
                        yield from gen_c1(j, s, c)

                interleave([stream_c1(0), stream_c1(1)], lead=9)
                stage_end()
            if stop_after == ("C1", l):
                break

            with ExitStack() as ls:
                lsb = lambda name, shape, dtype=F32: ls.enter_context(nc.sbuf_tensor(name + "_L%d" % l, list(shape), dtype))
                wro = lsb("wro", [128, 8, 1024], BF16)
                wo = lsb("wo", [128, 8, 1024], BF16)
                wpo = lsb("wpo", [128, 4, 1024], BF16)
                plw = lsb("plw", [128, 4, 128], BF16)
                with ExitStack() as ls2:
                    wst = [ls2.enter_context(nc.sbuf_tensor("wstc%d_L%d" % (i, l), [128, 8, 512], F32)) for i in range(2)]
                    load_cast(wro, w["wro"], 8, 1024, wst, "wro")
                    load_cast(wo, w["wo"], 8, 1024, wst, "wo")
                    load_cast(wpo, w["wpo"], 4, 1024, wst, "wpo")
                    P.dma("sp", wst[0][:, 0:4, 0:128], w["pool_w"].rearrange("g c d -> c g d"), [], [("wst", 0)], ("wst", 0))
                    P.op("pool", lambda e: e.tensor_copy(out=plw[:], in_=wst[0][:, 0:4, 0:128]), [("wst", 0)], ["plw"])
                    stage_end()
                Ures = lsb("Ures", [128, NT, 512], BF16)
                PMb = lsb("PMb", [128, pm_slots, 512], BF16)
                PMc = lsb("PMc", [128, 8, 256], BF16)
                P.dma("sp", PMc[:], pmc_in.rearrange("g t p o -> p (g t) o"), [], ["PMc"], "pmc")
                zr = lsb("zr", [128, 8, 512], BF16)
                spb = lsb("spb", [128, 8, 512], BF16)
                srb = lsb("srb", [128, 8, 512], BF16)
                dTb = lsb("dTb", [128, 4, 512], BF16)
                ygb = lsb("ygb", [128, 4, 512], BF16)
                mixb = lsb("mixb", [128, 8, 512], BF16)
                t1 = lsb("t1", [128, 512])
                t2 = lsb("t2", [128, 512])
                xb = lsb("xc", [128, 8, 512])
                ss = dict(zb=lsb("zb", [128, 8, 512], BF16), sq=lsb("sq", [128, 8, 512], BF16),
                          ms=lsb("ms", [128, 512]), m2=lsb("m2", [128, 512]), rs=lsb("rs", [128, 512]))
                for s in range(SEQS):
                    P.dma("sp", Ures[:], U_d[s].rearrange("(t p) c -> p t c", p=128), [("U", s)], ["Ures"], "Ures")
                    for ob, (t0, n, isctx) in enumerate(blocks):
                        if isctx and not need_ctx:
                            continue
                        tc = 2 if isctx else s
                        bsl = (slice(None), slice(None), slice(t0, t0 + n))
                        P.dma("sp", zr[:, :, :n], ZR_d[s][bsl].rearrange("k p t -> p k t"), [("ZR", s)], ["zr"], "zr")
                        P.dma("sp", spb[:, :, :n], G_d[1][s][bsl].rearrange("k p t -> p k t"), [("G", 1, s)], ["spb"], "spb")
                        P.dma("sp", srb[:, :, :n], G_d[2][s][bsl].rearrange("k p t -> p k t"), [("G", 2, s)], ["srb"], "srb")
                        P.dma("sp", xb[:, :, :n], xsrc[s][bsl].rearrange("k p t -> p k t"), [("XS", s, t0)], ["xb"], "xb")
                        if not isctx:
                            si = 0 if ob == 0 else (2 if ob == 7 else 1)
                            if ob in (0, 1, 7):
                                P.dma("sp", PMb[:], pm_in[si].rearrange("a p o -> p a o"), [], ["PMb"], "PMb")
                        for g in range(4):
                            bnk = g % 2
                            if isctx:
                                lst = [(32 + tt, PMc[:, g * 2 + tt, :]) for tt in range(2)]
                            else:
                                lst = [(4 * ob + rel, PMb[:, slot, :]) for rel, slot in pm_lists[si][g]]
                            def mm(e, lst=lst, g=g, bnk=bnk, n=n):
                                for i2, (tin, pmv) in enumerate(lst):
                                    r = e.matmul(PS[bnk][:, :n], lhsT=Ures[:, tin, g * 128:(g + 1) * 128], rhs=pmv[:, :n],
                                                 start=(i2 == 0), stop=(i2 == len(lst) - 1))
                                return r
                            P.op("pe", mm, ["Ures", "PMb", "PMc"], [("ps", bnk)])
                            P.op("act", lambda e, g=g, bnk=bnk, n=n: e.activation(out=dTb[:, g, :n], in_=PS[bnk][:, :n], func=AF.Copy),
                                 [("ps", bnk)], [("dTb", g)])
                            P.op("pe", lambda e, g=g, bnk=bnk, n=n: e.matmul(PS[2 + bnk][:, :n], lhsT=plw[:, g, :], rhs=dTb[:, g, :n], start=True, stop=True),
                                 [("dTb", g), "plw"], [("ps", 2 + bnk)])
                            P.op("act", lambda e, g=g, bnk=bnk, n=n: e.activation(out=ygb[:, g, :n], in_=PS[2 + bnk][:, :n], func=AF.Identity,
                                                                                   scale=psc[:, g:g + 1]), [("ps", 2 + bnk), "psc"], [("ygb", g)])
                        for oc in range(8):
                            ocs = slice(oc * 128, (oc + 1) * 128)
                            def mmp(e, ocs=ocs, n=n):
                                for g in range(4):
                                    r = e.matmul(PS[4][:, :n], lhsT=wpo[:, g, ocs], rhs=ygb[:, g, :n], start=(g == 0), stop=(g == 3))
                                return r
                            def mmr(e, ocs=ocs, n=n):
                                for k in range(8):
                                    r = e.matmul(PS[5][:, :n], lhsT=wro[:, k, ocs], rhs=zr[:, k, :n], start=(k == 0), stop=(k == 7))
                                return r
                            P.op("pe", mmp, [("ygb", g) for g in range(4)] + ["wpo"], [("ps", 4)])
                            P.op("pe", mmr, ["zr", "wro"], [("ps", 5)])
                            P.op("dve", lambda e, oc=oc, n=n: e.tensor_tensor(out=t1[:, :n], in0=PS[4][:, :n], in1=spb[:, oc, :n], op=ALU.mult),
                                 [("ps", 4), "spb"], ["t1"])
                            P.op("dve", lambda e, oc=oc, n=n: e.tensor_tensor(out=t2[:, :n], in0=PS[5][:, :n], in1=srb[:, oc, :n], op=ALU.mult),
                                 [("ps", 5), "srb"], ["t2"])
                            P.op("dve", lambda e, oc=oc, n=n: e.tensor_tensor(out=mixb[:, oc, :n], in0=t1[:, :n], in1=t2[:, :n], op=ALU.add),
                                 ["t1", "t2"], [("mixb", oc)])
                        for oc2 in range(8):
                            bnk = 6 + oc2 % 2
                            def mmo(e, oc2=oc2, bnk=bnk, n=n):
                                for oc in range(8):
                                    r = e.matmul(PS[bnk][:, :n], lhsT=wo[:, oc, oc2 * 128:(oc2 + 1) * 128], rhs=mixb[:, oc, :n],
                                                 start=(oc == 0), stop=(oc == 7))
                                return r
                            P.op("pe", mmo, [("mixb", oc) for oc in range(8)] + ["wo"], [("ps", bnk)])
                            P.op("dve", lambda e, oc2=oc2, bnk=bnk, n=n, tc=tc: e.scalar_tensor_tensor(
                                out=xb[:, oc2, :n], in0=PS[bnk][:, :n], scalar=modp(2, oc2, tc), in1=xb[:, oc2, :n],
                                op0=ALU.mult, op1=ALU.add), [("ps", bnk), "xb", "MOD"], ["xb"])
                        ln_block(ss, xb, n, 0, 1, 1, 0, 1)
                        P.dma("sp", XS[s][bsl].rearrange("k p t -> p k t"), xb[:, :, :n], ["xb"], [("XS", s, t0)], "xb_st")
                stage_end()
            xsrc = XS
            if stop_after == ("C2", l):
                break

            last = (li == len(layers) - 1)
            if l % 2 == 0:
                with ExitStack() as ls:
                    lsb = lambda name, shape, dtype=F32: ls.enter_context(nc.sbuf_tensor(name + "_L%d" % l, list(shape), dtype))
                    w1 = lsb("w1", [128, 8, DFF], BF16)
                    w3 = lsb("w3", [128, 8, DFF], BF16)
                    w2 = lsb("w2", [128, 22, D], BF16)
                    with ExitStack() as ls2:
                        wst = [ls2.enter_context(nc.sbuf_tensor("wstd%d_L%d" % (i, l), [128, 8, 512], F32)) for i in range(2)]
                        load_cast(w1, w["w1"], 8, DFF, wst, "w1")
                        load_cast(w3, w["w3"], 8, DFF, wst, "w3")
                        load_cast(w2, w["w2"], 22, D, wst, "w2")
                        stage_end()
                    NB = 256
                    xds = [lsb("xd%d" % i, [128, 8, NB]) for i in range(2)]
                    h2 = lsb("h2", [128, 8, NB], BF16)
                    hid = lsb("hid", [128, 22, NB], BF16)
                    sl = [lsb("sl%d" % i, [128, NB]) for i in range(2)]
                    ss = dict(zb=lsb("zbd", [128, 8, NB], BF16), sq=lsb("sqd", [128, 8, NB], BF16),
                              ms=lsb("msd", [128, NB]), m2=lsb("m2d", [128, NB]), rs=lsb("rsd", [128, NB]))
                    bi = 0
                    for s in range(SEQS):
                        ntok = T if need_ctx else L
                        for t0 in range(0, ntok, NB):
                            isctx = t0 >= L
                            tc = 2 if isctx else s
                            xb = xds[bi % 2]
                            xk = ("xd", bi % 2)
                            bsl = (slice(None), slice(None), slice(t0, t0 + NB))
                            P.dma("sp", xb[:], XS[s][bsl].rearrange("k p t -> p k t"), [("XS", s, t0)], [xk], xk)
                            for k in range(8):
                                P.op("act", lambda e, k=k, xb=xb, tc=tc: e.activation(out=h2[:, k, :], in_=xb[:, k, :], func=AF.Identity,
                                                                                      scale=modp(4, k, tc), bias=modp(3, k, tc)), [xk, "MOD"], ["h2"])
                            for ff in range(22):
                                fs = slice(ff * 128, (ff + 1) * 128)
                                b1, b3 = (ff % 2) * 2, (ff % 2) * 2 + 1
                                def mm13(e, fs=fs, b1=b1, b3=b3):
                                    for k in range(8):
                                        e.matmul(PS[b1][:, :NB], lhsT=w1[:, k, fs], rhs=h2[:, k, :], start=(k == 0), stop=(k == 7))
                                    for k in range(8):
                                        r = e.matmul(PS[b3][:, :NB], lhsT=w3[:, k, fs], rhs=h2[:, k, :], start=(k == 0), stop=(k == 7))
                                    return r
                                P.op("pe", mm13, ["h2", "w1", "w3"], [("ps", b1), ("ps", b3)])
                                P.op("act", lambda e, ff=ff, b1=b1: e.activation(out=sl[ff % 2][:], in_=PS[b1][:, :NB], func=AF.Silu), [("ps", b1)], [("sl", ff % 2)])
                                P.op("dve", lambda e, ff=ff, b3=b3: e.tensor_tensor(out=hid[:, ff, :], in0=PS[b3][:, :NB], in1=sl[ff % 2][:], op=ALU.mult),
                                     [("ps", b3), ("sl", ff % 2)], [("hid", ff)])
                            for oc in range(8):
                                bnk = 4 + oc % 4
                                def mm2_(e, oc=oc, bnk=bnk):
                                    for ff in range(22):
                                        r = e.matmul(PS[bnk][:, :NB], lhsT=w2[:, ff, oc * 128:(oc + 1) * 128], rhs=hid[:, ff, :],
                                                     start=(ff == 0), stop=(ff == 21))
                                    return r
                                P.op("pe", mm2_, [("hid", ff) for ff in range(22)] + ["w2"], [("ps", bnk)])
                                P.op("dve", lambda e, oc=oc, bnk=bnk, xb=xb, tc=tc: e.scalar_tensor_tensor(
                                    out=xb[:, oc, :], in0=PS[bnk][:, :NB], scalar=modp(5, oc, tc), in1=xb[:, oc, :],
                                    op0=ALU.mult, op1=ALU.add), [("ps", bnk), xk, "MOD"], [xk])
                            ln_block(ss, xb, NB, 2, 3, 1, 0, 1, XK=xk)
                            if last:
                                if not isctx:
                                    P.dma("sp", outT[s][bsl].rearrange("k p t -> p k t"), xb[:], [xk], [("OUT",)], ("xd_st", bi % 2))
                            else:
                                P.dma("sp", XS[s][bsl].rearrange("k p t -> p k t"), xb[:], [xk], [("XS", s, t0)], ("xd_st", bi % 2))
                            bi += 1
                    stage_end()
            else:
                with ExitStack() as ls:
                    lsb = lambda name, shape, dtype=F32: ls.enter_context(nc.sbuf_tensor(name + "_L%d" % l, list(shape), dtype))
                    NB = 512
                    wrb = lsb("wrb", [128, 8, NEXP], BF16)
                    wrf = lsb("wrf", [128, 8, NEXP])
                    P.dma("sp", wrf[:], w["wr"].rearrange("(k p) e -> p k e", p=128), [], ["wrf"], "wrf")
                    P.op("dve", lambda e: e.tensor_copy(out=wrb[:], in_=wrf[:]), ["wrf"], ["wrb"])
                    wst = [lsb("wste%d" % i, [128, 4096]) for i in range(3)]
                    wsl = [[lsb("wsl%d_%d" % (i, j), [128, 4096], BF16) for j in range(3)] for i in range(2)]
                    xe = lsb("xe", [128, 8, 1024])
                    h2 = lsb("h2e", [128, 8, 1024], BF16)
                    gw = lsb("gw", [128, NEXP, 1024])
                    hid = [lsb("hide%d" % i, [128, 4, NB], BF16) for i in range(2)]
                    sl = [lsb("sle%d" % i, [128, NB]) for i in range(2)]
                    tg = [lsb("tge%d" % i, [128, NB]) for i in range(2)]
                    lgs = lsb("lgs", [128, 8])
                    mx8 = lsb("mx8", [128, 8])
                    dd = lsb("dd", [128, 4])
                    gte = lsb("gte", [128, 2, 8])
                    gbc = lsb("gbc", [128, 8, 128])
                    ss = dict(zb=lsb("zbe", [128, 8, 128], BF16), sq=lsb("sqe", [128, 8, 128], BF16),
                              ms=lsb("mse", [128, 128]), m2=lsb("m2e", [128, 128]), rs=lsb("rse", [128, 128]))
                    sbs = [[(s, q * 1024 + b * NB, NB, s) for b in range(2)] for s in range(SEQS) for q in range(4)]
                    if need_ctx:
                        sbs.append([(0, L, NCTX, 2), (1, L, NCTX, 2)])
                    jobs = [(sbi, ex, fsl) for sbi in range(len(sbs)) for ex in range(NEXP) for fsl in range(7)]

                    def load_dma(ji):
                        sbi, ex, fsl = jobs[ji]
                        j = ji % 2
                        if sbi == 0:
                            srcs = (w["w1"][ex].rearrange("(k p) n -> p k n", p=128)[:, :, fsl * 512:(fsl + 1) * 512],
                                    w["w3"][ex].rearrange("(k p) n -> p k n", p=128)[:, :, fsl * 512:(fsl + 1) * 512],
                                    w["w2"][ex, fsl * 512:(fsl + 1) * 512, :].rearrange("(c p) n -> p c n", p=128))
                            for m3 in range(3):
                                bdim = 512 if m3 < 2 else 1024
                                P.dma("sp", wst[m3][:].rearrange("p (a b) -> p a b", b=bdim), srcs[m3], [], [("wste", m3)], ("wste", m3))

                    def load_job(ji):
                        sbi, ex, fsl = jobs[ji]
                        j = ji % 2
                        if sbi == 0:
                            for m3 in range(3):
                                P.op("act", lambda e, j=j, m3=m3: e.activation(out=wsl[j][m3][:], in_=wst[m3][:], func=AF.Copy),
                                     [("wste", m3)], [("wsl", j, m3)])
                                P.dma("sp", WC_d[ex, fsl, m3], wsl[j][m3][:], [("wsl", j, m3)], [("WC", ex, fsl, m3)], ("wc_st", j, m3))
                            if ji + 1 < len(jobs):
                                load_dma(ji + 1)
                        else:
                            for m3 in range(3):
                                P.dma("sp", wsl[j][m3][:], WC_d[ex, fsl, m3], [("WC", ex, fsl, m3)], [("wsl", j, m3)], ("wsl", j, m3))

                    hi = 0
                    load_dma(0)
                    load_job(0)
                    for ji, (sbi, ex, fsl) in enumerate(jobs):
                        blks = sbs[sbi]
                        if ex == 0 and fsl == 0:
                            off = 0
                            for bix, (s, t0, n, tc) in enumerate(blks):
                                bs = slice(off, off + n)
                                P.dma("sp", xe[:, :, bs], XS[s, :, :, t0:t0 + n].rearrange("k p t -> p k t"), [("XS", s, t0)], [("xe", bix)], ("xe", bix))
                                for k in range(8):
                                    P.op("act", lambda e, k=k, bs=bs, tc=tc: e.activation(out=h2[:, k, bs], in_=xe[:, k, bs], func=AF.Identity,
                                                                                          scale=modp(4, k, tc), bias=modp(3, k, tc)),
                                         [("xe", bix), "MOD"], [("h2e", bix)])
                                for tt in range(n // 128):
                                    ts_ = slice(off + tt * 128, off + tt * 128 + 128)
                                    def mmr(e, ts_=ts_):
                                        for k in range(8):
                                            r = e.matmul(PS[6][:, 0:8], lhsT=h2[:, k, ts_], rhs=wrb[:, k, :], start=(k == 0), stop=(k == 7))
                                        return r
                                    P.op("pe", mmr, [("h2e", bix), "wrb"], [("ps", 6)])
                                    P.op("act", lambda e: e.activation(out=lgs[:], in_=PS[6][:, 0:8], func=AF.Copy), [("ps", 6)], ["lgs"])
                                    P.op("dve", lambda e: e.max(out=mx8[:], in_=lgs[:]), ["lgs"], ["mx8"])
                                    P.op("dve", lambda e: e.tensor_tensor(out=dd[:, 0:1], in0=mx8[:, 0:1], in1=mx8[:, 1:2], op=ALU.subtract), ["mx8"], ["dd0"])
                                    P.op("act", lambda e: e.activation(out=dd[:, 1:2], in_=dd[:, 0:1], func=AF.Sigmoid), ["dd0"], ["dd1"])
                                    P.op("act", lambda e: e.activation(out=dd[:, 2:3], in_=dd[:, 0:1], func=AF.Sigmoid, scale=-1.0), ["dd0"], ["dd2"])
                                    P.op("dve", lambda e: e.tensor_scalar(out=gte[:, 0, :], in0=lgs[:], scalar1=mx8[:, 0:1], scalar2=dd[:, 1:2],
                                                                          op0=ALU.is_equal, op1=ALU.mult), ["lgs", "mx8", "dd1"], ["gte0"])
                                    P.op("dve", lambda e: e.tensor_scalar(out=gte[:, 1, :], in0=lgs[:], scalar1=mx8[:, 1:2], scalar2=dd[:, 2:3],
                                                                          op0=ALU.is_equal, op1=ALU.mult), ["lgs", "mx8", "dd2"], ["gte1"])
                                    P.op("dve", lambda e: e.tensor_tensor(out=gte[:, 0, :], in0=gte[:, 0, :], in1=gte[:, 1, :], op=ALU.add), ["gte0", "gte1"], ["gte0"])
                                    P.op("dve", lambda e: e.tensor_copy(out=gbc[:], in_=gte[:, 0, :].unsqueeze(2).to_broadcast([128, 8, 128])), ["gte0"], ["gbc"])
                                    for hb2 in range(2):
                                        def mmb(e, hb2=hb2):
                                            for q in range(4):
                                                r = e.matmul(PS[4 + hb2][:, q * 128:(q + 1) * 128], lhsT=gbc[:, hb2 * 4 + q, :], rhs=ident_f[:], start=True, stop=True)
                                            return r
                                        P.op("pe", mmb, ["gbc", "ident_f"], [("ps", 4 + hb2)])
                                        P.op("act", lambda e, hb2=hb2, ts_=ts_: e.activation(out=gw[:, hb2 * 4:(hb2 + 1) * 4, ts_],
                                                                                            in_=PS[4 + hb2][:, :].rearrange("p (a b) -> p a b", b=128), func=AF.Copy),
                                             [("ps", 4 + hb2)], [("gw", bix)])
                                off += n
                        if ji + 1 < len(jobs):
                            load_job(ji + 1)
                        j = ji % 2
                        w1s = wsl[j][0][:].rearrange("p (a b) -> p a b", b=512)
                        w3s = wsl[j][1][:].rearrange("p (a b) -> p a b", b=512)
                        w2s = wsl[j][2][:].rearrange("p (a b) -> p a b", b=1024)
                        off = 0
                        hjs = []
                        for bix, (s, t0, n, tc) in enumerate(blks):
                            bs = slice(off, off + n)
                            hj = hi % 2
                            hjs.append((hj, bs))
                            for c4 in range(4):
                                cs = slice(c4 * 128, (c4 + 1) * 128)
                                b1, b3 = (c4 % 2) * 2, (c4 % 2) * 2 + 1
                                def mm13(e, cs=cs, b1=b1, b3=b3, bs=bs, w1s=w1s, w3s=w3s, n=n):
                                    for k in range(8):
                                        e.matmul(PS[b1][:, :n], lhsT=w1s[:, k, cs], rhs=h2[:, k, bs], start=(k == 0), stop=(k == 7))
                                    for k in range(8):
                                        r = e.matmul(PS[b3][:, :n], lhsT=w3s[:, k, cs], rhs=h2[:, k, bs], start=(k == 0), stop=(k == 7))
                                    return r
                                P.op("pe", mm13, [("h2e", bix), ("wsl", j, 0), ("wsl", j, 1)], [("ps", b1), ("ps", b3)])
                                P.op("act", lambda e, c4=c4, b1=b1, n=n: e.activation(out=sl[c4 % 2][:, :n], in_=PS[b1][:, :n], func=AF.Silu), [("ps", b1)], [("sle", c4 % 2)])
                                P.op("pool", lambda e, c4=c4, ex=ex, bs=bs, n=n: e.tensor_tensor(out=tg[c4 % 2][:, :n], in0=sl[c4 % 2][:, :n], in1=gw[:, ex, bs], op=ALU.mult),
                                     [("sle", c4 % 2), ("gw", bix)], [("tge", c4 % 2)])
                                P.op("dve", lambda e, c4=c4, hj=hj, b3=b3, n=n: e.tensor_tensor(out=hid[hj][:, c4, :n], in0=PS[b3][:, :n], in1=tg[c4 % 2][:, :n], op=ALU.mult),
                                     [("ps", b3), ("tge", c4 % 2)], [("hide", hj, c4)])
                            hi += 1
                            off += n
                        for bix, (s, t0, n, tc) in enumerate(blks):
                            hj, bs = hjs[bix]
                            for oc in range(8):
                                bnk = 4 + oc % 4
                                def mm2_(e, oc=oc, bnk=bnk, hj=hj, w2s=w2s, n=n):
                                    for c4 in range(4):
                                        r = e.matmul(PS[bnk][:, :n], lhsT=w2s[:, c4, oc * 128:(oc + 1) * 128], rhs=hid[hj][:, c4, :n],
                                                     start=(c4 == 0), stop=(c4 == 3))
                                    return r
                                P.op("pe", mm2_, [("hide", hj, c4) for c4 in range(4)] + [("wsl", j, 2)], [("ps", bnk)])
                                P.op("dve", lambda e, oc=oc, bnk=bnk, bs=bs, tc=tc, n=n: e.scalar_tensor_tensor(
                                    out=xe[:, oc, bs], in0=PS[bnk][:, :n], scalar=modp(5, oc, tc), in1=xe[:, oc, bs],
                                    op0=ALU.mult, op1=ALU.add), [("ps", bnk), ("xe", bix), "MOD"], [("xe", bix)])
                        if ex == NEXP - 1 and fsl == 6:
                            off = 0
                            for bix, (s, t0, n, tc) in enumerate(blks):
                                for sub in range(n // 128):
                                    bs = slice(off + sub * 128, off + sub * 128 + 128)
                                    tt0 = t0 + sub * 128
                                    ln_block(ss, xe[:, :, bs], 128, 2, 3, 1, 0, 1, XK=("xe", bix))
                                    if last:
                                        if tc != 2:
                                            P.dma("sp", outT[s, :, :, tt0:tt0 + 128].rearrange("k p t -> p k t"), xe[:, :, bs], [("xe", bix)], [("OUT", s, tt0)], ("xe_st", bix))
                                    else:
                                        P.dma("sp", XS[s, :, :, tt0:tt0 + 128].rearrange("k p t -> p k t"), xe[:, :, bs], [("xe", bix)], [("XSo", s, tt0)], ("xe_st", bix))
                                off += n
                    stage_end()
        if stop_after is not None:
            dbg = dt("dbgXS", [SEQS, 8, 128, T], F32, kind="ExternalOutput").ap()
            for s_ in range(SEQS):
                P.dma("sp", dbg[s_], XS[s_], [], [("dbg", s_)], ("dbg", s_))
        P.barrier()
        P.emit(block)
    return nc


def _host_inputs(inputs, layers):
    f32 = np.float32
    x = np.asarray(inputs["x"], f32)
    ctx = np.asarray(inputs["ctx"], f32)
    c = np.asarray(inputs["c"], f32)
    c_ctx = np.asarray(inputs["c_ctx"], f32)
    PM, pm_lists, PMC = _pool_constants()
    cosT, sinT = _rope_tables()
    E, masks = _misc_constants()
    common = dict(cosT=cosT, sinT=sinT, etab=E, masks=masks, pm=PM, pmc=PMC, ident=np.eye(128, dtype=f32))
    for l in layers:
        common["ada_w%d" % l] = np.ascontiguousarray(inputs["ada_w"][l], f32)
        common["ada_bT%d" % l] = np.ascontiguousarray(np.asarray(inputs["ada_b"][l], f32).reshape(48, 128).T)
        common["w_in%d" % l] = np.ascontiguousarray(inputs["w_in"][l], f32)
        common["pool_w%d" % l] = np.ascontiguousarray(inputs["pool_w"][l], f32)
        common["pscT%d" % l] = np.ascontiguousarray(np.asarray(inputs["pool_scale"][l], f32).reshape(4, 128).T)
        common["w_pool_out%d" % l] = np.ascontiguousarray(inputs["w_pool_out"][l], f32)
        common["w_ret_out%d" % l] = np.ascontiguousarray(inputs["w_ret_out"][l], f32)
        common["logit_bc%d" % l] = np.ascontiguousarray(
            np.broadcast_to(np.asarray(inputs["ret_decay_logit"][l], f32).reshape(1, 8), (128, 8)))
        common["w_out%d" % l] = np.ascontiguousarray(inputs["w_out"][l], f32)
        lnT = np.stack([np.asarray(inputs[k][l], f32).reshape(8, 128).T
                        for k in ("ln_mix_g", "ln_mix_b", "ln_ffn_g", "ln_ffn_b")], axis=1)
        common["lnT%d" % l] = np.ascontiguousarray(lnT)
        if l % 2 == 0:
            common["ffn_w1_%d" % l] = np.ascontiguousarray(inputs["ffn_w1"][l // 2], f32)
            common["ffn_w3_%d" % l] = np.ascontiguousarray(inputs["ffn_w3"][l // 2], f32)
            common["ffn_w2_%d" % l] = np.ascontiguousarray(inputs["ffn_w2"][l // 2], f32)
        else:
            common["moe_router%d" % l] = np.ascontiguousarray(inputs["moe_router"][l // 2], f32)
            common["moe_w1_%d" % l] = np.ascontiguousarray(inputs["moe_w1"][l // 2], f32)
            common["moe_w3_%d" % l] = np.ascontiguousarray(inputs["moe_w3"][l // 2], f32)
            common["moe_w2_%d" % l] = np.ascontiguousarray(inputs["moe_w2"][l // 2], f32)
    in_maps = []
    for core in range(NCORES):
        m = dict(common)
        xs = []
        for s in range(SEQS):
            b = core * SEQS + s
            xt = np.concatenate([x[b].T, ctx[b].T], axis=1)
            xs.append(xt.reshape(8, 128, T))
        m["xT"] = np.ascontiguousarray(np.stack(xs))
        cs = np.stack([c[core * SEQS], c[core * SEQS + 1], c_ctx], axis=1)
        m["cT"] = np.ascontiguousarray(cs.reshape(8, 128, 3).transpose(1, 0, 2))
        in_maps.append(m)
    return in_maps, pm_lists, PM.shape[1]


def kernel(**inputs):
    layers = (0, 1, 2, 3)
    in_maps, pm_lists, pm_slots = _host_inputs(inputs, layers)
    nc = build(layers, None, pm_lists, pm_slots)
    res = run_bass_kernel_spmd(nc, in_maps, core_ids=list(range(NCORES)))
    out = np.empty((NCORES * SEQS, L, D), np.float32)
    for core in range(NCORES):
        o = res.results[core]["outT"]
        for s in range(SEQS):
            out[core * SEQS + s] = o[s].reshape(D, L).T
    return out
```
